# Optimizing a Trainium2 kernel written in Bass

```python
import math
import jax
import jax.numpy as jnp
from jax import lax
import numpy as np

D_MODEL = 1024
BATCH = 32
SEQ = 2048
DEPTH = 4

N_EVEN = (DEPTH + 1) // 2
N_ODD = DEPTH // 2
EPS = 1e-6
POOL_WINDOWS = (2, 4, 8, 16)
N_POOL_GROUPS = 4
POOL_GROUP = D_MODEL // 8
POOL_WIDTH = N_POOL_GROUPS * POOL_GROUP
CONV_WIDTH = D_MODEL // 2
CONV_K = 31
AB_IN = POOL_WIDTH + 2 * CONV_WIDTH
AB_MIX = POOL_WIDTH + CONV_WIDTH
DN_HEADS = 8
DN_HEAD_DIM = 128
DN_WIDTH = DN_HEADS * DN_HEAD_DIM
DN_CONV_K = 4
DN_CHUNK = 64
DN_IN = 4 * DN_WIDTH + 2 * DN_HEADS
N_GROUPS = 4
EXPERTS_PER_GROUP = 8
N_EXPERTS = N_GROUPS * EXPERTS_PER_GROUP
TOP_K = 2
D_EXPERT = D_MODEL // 4
MOE_BLOCK = 128

kernel_name = 'hybrid_pool_conv_deltanet_hmoe'


def rmsnorm(x, gain):
    xf = x.astype(jnp.float32)
    y = xf * lax.rsqrt(jnp.mean(xf * xf, axis=-1, keepdims=True) + EPS)
    return (y * gain.astype(jnp.float32)).astype(x.dtype)


def layernorm(x, gain, bias):
    xf = x.astype(jnp.float32)
    mu = jnp.mean(xf, axis=-1, keepdims=True)
    var = jnp.mean(jnp.square(xf - mu), axis=-1, keepdims=True)
    y = (xf - mu) * lax.rsqrt(var + EPS)
    return (y * gain.astype(jnp.float32) + bias.astype(jnp.float32)).astype(x.dtype)


def l2norm(x):
    return x * lax.rsqrt(jnp.sum(x * x, axis=-1, keepdims=True) + EPS)


def causal_depthwise_conv(x, w):
    k_width, ch = w.shape
    return lax.conv_general_dilated(
        x, w[:, None, :].astype(x.dtype), window_strides=(1,), padding=[(k_width - 1, 0)],
        dimension_numbers=('NWC', 'WIO', 'NWC'), feature_group_count=ch)


def multiscale_causal_pool(u):
    seq = u.shape[1]
    uf = u.astype(jnp.float32)
    cs = jnp.cumsum(uf, axis=1)
    pos = jnp.arange(1, seq + 1, dtype=jnp.float32)
    outs = []
    for j, w in enumerate(POOL_WINDOWS):
        csj = cs[:, :, j]
        win = csj - jnp.pad(csj, ((0, 0), (w, 0), (0, 0)))[:, :seq]
        cnt = jnp.minimum(pos, float(w))[None, :, None]
        outs.append(win / cnt - uf[:, :, j])
    return jnp.stack(outs, axis=2)


def pool_conv_mixer(h, w_in, pool_w, pool_scale, conv_w, conv_b, ln_g, ln_b, w_out):
    bsz, seq, _ = h.shape
    u = h @ w_in
    ua = u[..., :POOL_WIDTH].reshape(bsz, seq, N_POOL_GROUPS, POOL_GROUP)
    pooled = multiscale_causal_pool(ua).astype(h.dtype)
    ya = jnp.einsum('bsgc,gcd->bsgd', pooled, pool_w).reshape(bsz, seq, POOL_WIDTH) * pool_scale
    val = u[..., POOL_WIDTH:POOL_WIDTH + CONV_WIDTH]
    gate = u[..., POOL_WIDTH + CONV_WIDTH:]
    yb = causal_depthwise_conv(val * jax.nn.sigmoid(gate), conv_w) + conv_b
    yb = jax.nn.silu(layernorm(yb, ln_g, ln_b))
    return jnp.concatenate([ya, yb], axis=-1) @ w_out


def chunk_gated_delta_rule(q, k, v, beta, g):
    bsz, seq, nh, dk = q.shape
    dv = v.shape[-1]
    c = DN_CHUNK
    n = seq // c

    def to_chunks(t):
        return t.reshape(bsz, n, c, nh, -1).transpose(0, 3, 1, 2, 4)

    q = to_chunks(q) * (dk ** -0.5)
    k = to_chunks(k)
    v = to_chunks(v)
    beta = to_chunks(beta[..., None])[..., 0]
    g = jnp.cumsum(to_chunks(g[..., None])[..., 0], axis=-1)
    causal = jnp.tril(jnp.ones((c, c), dtype=bool))
    strict = jnp.tril(jnp.ones((c, c), dtype=bool), -1)
    decay = jnp.where(causal, jnp.exp(jnp.where(causal, g[..., :, None] - g[..., None, :], 0.0)), 0.0)
    kb = k * beta[..., None]
    lower = jnp.where(strict, jnp.einsum('bhnck,bhnmk->bhncm', kb, k) * decay, 0.0)
    eye = jnp.eye(c, dtype=jnp.float32)
    t_mat = lax.linalg.triangular_solve(lower + eye, jnp.broadcast_to(eye, lower.shape),
                                        left_side=True, lower=True)
    u = t_mat @ (v * beta[..., None])
    w = t_mat @ (kb * jnp.exp(g)[..., None])
    a_intra = jnp.where(causal, jnp.einsum('bhnck,bhnmk->bhncm', q, k) * decay, 0.0)
    g_last = g[..., -1:]
    qg = q * jnp.exp(g)[..., None]
    kd = k * jnp.exp(g_last - g)[..., None]
    d_last = jnp.exp(g_last[..., 0])

    def step(state, xs):
        qg_i, kd_i, u_i, w_i, a_i, d_i = xs
        v_new = u_i - jnp.einsum('bhck,bhkv->bhcv', w_i, state)
        o_i = jnp.einsum('bhck,bhkv->bhcv', qg_i, state) + jnp.einsum('bhcm,bhmv->bhcv', a_i, v_new)
        state = state * d_i[..., None, None] + jnp.einsum('bhck,bhcv->bhkv', kd_i, v_new)
        return state, o_i

    xs = tuple(jnp.moveaxis(t, 2, 0) for t in (qg, kd, u, w, a_intra, d_last))
    state0 = jnp.zeros((bsz, nh, dk, dv), jnp.float32)
    _, o = lax.scan(step, state0, xs)
    return o.transpose(1, 0, 3, 2, 4).reshape(bsz, seq, nh, dv)


def gated_deltanet_mixer(h, w_in, conv_w, a_log, dt_bias, onorm_g, w_out):
    bsz, seq, _ = h.shape
    u = h @ w_in
    qkv = jax.nn.silu(causal_depthwise_conv(u[..., :3 * DN_WIDTH], conv_w)).astype(jnp.float32)
    q, k, v = [t.reshape(bsz, seq, DN_HEADS, DN_HEAD_DIM) for t in jnp.split(qkv, 3, axis=-1)]
    z = u[..., 3 * DN_WIDTH:4 * DN_WIDTH].reshape(bsz, seq, DN_HEADS, DN_HEAD_DIM).astype(jnp.float32)
    b_raw = u[..., 4 * DN_WIDTH:4 * DN_WIDTH + DN_HEADS].astype(jnp.float32)
    a_raw = u[..., 4 * DN_WIDTH + DN_HEADS:].astype(jnp.float32)
    beta = jax.nn.sigmoid(b_raw)
    g = -jnp.exp(a_log.astype(jnp.float32)) * jax.nn.softplus(a_raw + dt_bias.astype(jnp.float32))
    o = chunk_gated_delta_rule(l2norm(q), l2norm(k), v, beta, g)
    o = o * lax.rsqrt(jnp.mean(o * o, axis=-1, keepdims=True) + EPS) * onorm_g.astype(jnp.float32) * jax.nn.silu(z)
    return o.reshape(bsz, seq, DN_WIDTH).astype(h.dtype) @ w_out


def hierarchical_moe(h, w_grp, b_grp, w_exp, b_exp, w_up, w_down):
    bsz, seq, d = h.shape
    n_tok = bsz * seq
    xt = h.reshape(n_tok, d)
    grp_prob = jax.nn.softmax((xt @ w_grp).astype(jnp.float32) + b_grp.astype(jnp.float32), axis=-1)
    gp, gi = lax.top_k(grp_prob, 1)
    e_logits = ((xt @ w_exp).astype(jnp.float32) + b_exp.astype(jnp.float32)).reshape(n_tok, N_GROUPS, EXPERTS_PER_GROUP)
    sel = e_logits[jnp.arange(n_tok), gi[:, 0]]
    ew, ei = lax.top_k(jax.nn.softmax(sel, axis=-1), TOP_K)
    gate = gp * (ew / jnp.sum(ew, axis=-1, keepdims=True))
    eid = (gi * EXPERTS_PER_GROUP + ei).reshape(-1)
    tok = jnp.repeat(jnp.arange(n_tok, dtype=jnp.int32), TOP_K)
    wts = gate.reshape(-1)
    m = n_tok * TOP_K
    order = jnp.argsort(eid)
    se = eid[order]
    counts = jnp.bincount(eid, length=N_EXPERTS)
    starts = jnp.cumsum(counts) - counts
    padded = (counts + MOE_BLOCK - 1) // MOE_BLOCK * MOE_BLOCK
    pend = jnp.cumsum(padded)
    pstart = pend - padded
    dest = pstart[se] + jnp.arange(m) - starts[se]
    p_rows = (m + N_EXPERTS * (MOE_BLOCK - 1) + MOE_BLOCK - 1) // MOE_BLOCK * MOE_BLOCK
    n_blocks = p_rows // MOE_BLOCK
    buf_tok = jnp.zeros((p_rows,), jnp.int32).at[dest].set(tok[order])
    buf_w = jnp.zeros((p_rows,), jnp.float32).at[dest].set(wts[order])
    blk_e = jnp.minimum(jnp.searchsorted(pend, jnp.arange(n_blocks) * MOE_BLOCK, side='right'), N_EXPERTS - 1)

    def expert_block(args):
        bt, bw, be = args
        xb = xt[bt]
        gu = xb @ w_up[be]
        y = (jax.nn.silu(gu[:, :D_EXPERT]) * gu[:, D_EXPERT:]) @ w_down[be]
        return y * bw[:, None].astype(y.dtype)

    yb = lax.map(expert_block, (buf_tok.reshape(n_blocks, MOE_BLOCK), buf_w.reshape(n_blocks, MOE_BLOCK), blk_e))
    out = jnp.zeros_like(xt).at[buf_tok].add(yb.reshape(p_rows, d))
    return out.reshape(bsz, seq, d)


def setup_inputs(seed: int = 0) -> dict:
    key = jax.random.key(seed)
    ks = iter(jax.random.split(key, 40))

    def nrm(shape, scale):
        return jax.random.normal(next(ks), shape, jnp.float32) * scale

    dt = jnp.exp(jax.random.uniform(next(ks), (N_ODD, DN_HEADS), jnp.float32, math.log(1e-3), math.log(1e-1)))
    return {
        'x': nrm((BATCH, SEQ, D_MODEL), 1.0),
        'c': nrm((BATCH, D_MODEL), 1.0),
        'mod_w': nrm((DEPTH, D_MODEL, 6 * D_MODEL), 0.5 * D_MODEL ** -0.5),
        'mod_b': nrm((DEPTH, 6 * D_MODEL), 0.01),
        'norm_mix': 1.0 + nrm((DEPTH, D_MODEL), 0.02),
        'norm_ffn': 1.0 + nrm((DEPTH, D_MODEL), 0.02),
        'ab_w_in': nrm((N_EVEN, D_MODEL, AB_IN), D_MODEL ** -0.5),
        'pool_w': nrm((N_EVEN, N_POOL_GROUPS, POOL_GROUP, POOL_GROUP), POOL_GROUP ** -0.5),
        'pool_scale': 1.0 + nrm((N_EVEN, POOL_WIDTH), 0.02),
        'conv_w': nrm((N_EVEN, CONV_K, CONV_WIDTH), CONV_K ** -0.5),
        'conv_b': nrm((N_EVEN, CONV_WIDTH), 0.01),
        'conv_ln_g': 1.0 + nrm((N_EVEN, CONV_WIDTH), 0.02),
        'conv_ln_b': nrm((N_EVEN, CONV_WIDTH), 0.01),
        'ab_w_out': nrm((N_EVEN, AB_MIX, D_MODEL), AB_MIX ** -0.5),
        'dn_w_in': nrm((N_ODD, D_MODEL, DN_IN), D_MODEL ** -0.5),
        'dn_conv_w': nrm((N_ODD, DN_CONV_K, 3 * DN_WIDTH), DN_CONV_K ** -0.5),
        'dn_a_log': jnp.log(jax.random.uniform(next(ks), (N_ODD, DN_HEADS), jnp.float32, 1.0, 16.0)),
        'dn_dt_bias': dt + jnp.log(-jnp.expm1(-dt)),
        'dn_onorm': 1.0 + nrm((N_ODD, DN_HEAD_DIM), 0.02),
        'dn_w_out': nrm((N_ODD, DN_WIDTH, D_MODEL), DN_WIDTH ** -0.5),
        'moe_w_grp': nrm((DEPTH, D_MODEL, N_GROUPS), D_MODEL ** -0.5),
        'moe_b_grp': nrm((DEPTH, N_GROUPS), 0.01),
        'moe_w_exp': nrm((DEPTH, D_MODEL, N_EXPERTS), D_MODEL ** -0.5),
        'moe_b_exp': nrm((DEPTH, N_EXPERTS), 0.01),
        'moe_w_up': nrm((DEPTH, N_EXPERTS, D_MODEL, 2 * D_EXPERT), D_MODEL ** -0.5),
        'moe_w_down': nrm((DEPTH, N_EXPERTS, D_EXPERT, D_MODEL), D_EXPERT ** -0.5),
        'final_norm': 1.0 + nrm((D_MODEL,), 0.02),
    }


def reference(x, c, mod_w, mod_b, norm_mix, norm_ffn, ab_w_in, pool_w, pool_scale, conv_w, conv_b,
              conv_ln_g, conv_ln_b, ab_w_out, dn_w_in, dn_conv_w, dn_a_log, dn_dt_bias, dn_onorm, dn_w_out,
              moe_w_grp, moe_b_grp, moe_w_exp, moe_b_exp, moe_w_up, moe_w_down, final_norm):
    c_act = jax.nn.silu(c)
    for l in range(DEPTH):
        mod = (c_act @ mod_w[l] + mod_b[l])[:, None, :]
        sh_m, sc_m, g_m, sh_f, sc_f, g_f = jnp.split(mod, 6, axis=-1)
        hn = rmsnorm(x, norm_mix[l]) * (1.0 + sc_m) + sh_m
        i = l // 2
        if l % 2 == 0:
            y = pool_conv_mixer(hn, ab_w_in[i], pool_w[i], pool_scale[i], conv_w[i], conv_b[i],
                                conv_ln_g[i], conv_ln_b[i], ab_w_out[i])
        else:
            y = gated_deltanet_mixer(hn, dn_w_in[i], dn_conv_w[i], dn_a_log[i], dn_dt_bias[i],
                                     dn_onorm[i], dn_w_out[i])
        x = x + g_m * y
        hn = rmsnorm(x, norm_ffn[l]) * (1.0 + sc_f) + sh_f
        x = x + g_f * hierarchical_moe(hn, moe_w_grp[l], moe_b_grp[l], moe_w_exp[l], moe_b_exp[l],
                                       moe_w_up[l], moe_w_down[l])
    return rmsnorm(x, final_norm)
```

```python
import numpy as np
from contextlib import ExitStack
import concourse.bass as bass
import concourse.mybir as mybir
from concourse.bass_utils import run_bass_kernel_spmd

F32 = mybir.dt.float32
BF16 = mybir.dt.bfloat16
AF = mybir.ActivationFunctionType
ALU = mybir.AluOpType

D = 1024
KD = 8
EPS = 1e-6
NEXP = 32
FE = 256
NDSEM = 8
GS = 512
NEG = -30000.0
INTERLEAVE = True

CFG = dict(NSEQ=4, S=2048, DEPTH=4, NCORES=8)


class Buf:
    __slots__ = ("t", "w", "r", "name")

    def __init__(self, t, name):
        self.t = t
        self.w = {}
        self.r = {}
        self.name = name

    def __getitem__(self, k):
        return self.t[k]


class _Proxy:
    def __init__(self):
        self.call = None

    def __getattr__(self, name):
        def f(*a, **kw):
            self.call = (name, a, kw)
            return None
        return f


class K:
    def __init__(self, nc, es):
        self.nc = nc
        self.es = es
        self.E = {"pe": nc.tensor, "act": nc.scalar, "dve": nc.vector, "pool": nc.gpsimd, "sp": nc.sync}
        self.sem = {e: es.enter_context(nc.semaphore("s_" + e)) for e in self.E}
        self.cnt = {e: 0 for e in self.E}
        self.waited = {e: {} for e in self.E}
        self.dq = {}
        for q in ("sp", "pool"):
            self.dq[q] = [[es.enter_context(nc.semaphore("d_%s%d" % (q, i))), 0, "d_%s%d" % (q, i)] for i in range(NDSEM)]
        self.dqi = {"sp": 0, "pool": 0}
        self.uid = 0
        self.nins = 0
        self.rec = None

    def sb(self, shape, dt, es=None, name=None):
        self.uid += 1
        name = (name or "t") + "_%d" % self.uid
        t = (es or self.es).enter_context(self.nc.sbuf_tensor(name, list(shape), dt))
        return Buf(t, name)

    def psb(self, shape, dt, name):
        t = self.es.enter_context(self.nc.psum_tensor(name, list(shape), dt))
        return Buf(t, name)

    def _wait(self, eng, tok):
        key, sem, val, peng = tok
        if peng == "pe" and eng == "pe":
            return
        if self.waited[eng].get(key, 0) >= val:
            return
        self.E[eng].wait_ge(sem, val)
        self.nins += 1
        self.waited[eng][key] = val

    def _deps(self, eng, reads, writes):
        for b in reads:
            for tok in b.w.values():
                self._wait(eng, tok)
        for b in writes:
            for tok in b.w.values():
                self._wait(eng, tok)
            for tok in b.r.values():
                self._wait(eng, tok)

    def _mark(self, tok, reads, writes):
        for b in reads:
            b.r[tok[0]] = tok
        for b in writes:
            b.w = {tok[0]: tok}
            b.r = {}

    def op(self, eng, fn, reads=(), writes=(), inc=True):
        if self.rec is not None:
            pr = _Proxy()
            fn(pr)
            self.rec.append(("op", eng, pr.call, tuple(reads), tuple(writes)))
            return
        self._deps(eng, reads, writes)
        ins = fn(self.E[eng])
        self.nins += 1
        if inc:
            self.cnt[eng] += 1
            ins.then_inc(self.sem[eng], 1)
            tok = (eng, self.sem[eng], self.cnt[eng], eng)
        else:
            tok = (eng, self.sem[eng], self.cnt[eng] + 1, eng)
        self._mark(tok, reads, writes)

    def dma(self, q, out, in_, reads=(), writes=()):
        if self.rec is not None:
            self.rec.append(("dma", q, (out, in_), tuple(reads), tuple(writes)))
            return
        self._deps(q, reads, writes)
        slot = self.dq[q][self.dqi[q] % NDSEM]
        self.dqi[q] += 1
        if slot[1] > 0:
            self._wait(q, (slot[2], slot[0], slot[1], "dma"))
        ins = self.E[q].dma_start(out=out, in_=in_)
        self.nins += 1
        slot[1] += 16
        ins.then_inc(slot[0], 16)
        tok = (slot[2], slot[0], slot[1], "dma")
        self._mark(tok, reads, writes)

    def record(self, fn):
        self.rec = []
        fn()
        r, self.rec = self.rec, None
        return r

    def replay(self, items):
        for it in items:
            if it[0] == "op":
                _, eng, call, reads, writes = it
                self.op(eng, lambda e, c=call: getattr(e, c[0])(*c[1], **c[2]), reads=reads, writes=writes)
            else:
                _, q, (out, in_), reads, writes = it
                self.dma(q, out, in_, reads=reads, writes=writes)

    @staticmethod
    def interleave(a, b):
        out = []
        i = j = 0
        while i < len(a) or j < len(b):
            if j >= len(b) or (i < len(a) and i * len(b) <= j * len(a)):
                out.append(a[i])
                i += 1
            else:
                out.append(b[j])
                j += 1
        return out

    def barrier(self, engines=None):
        engs = list(self.E) if engines is None else engines
        for e in engs:
            for f in self.E:
                if self.cnt[f] > 0:
                    self._wait(e, (f, self.sem[f], self.cnt[f], "x"))
            for q in self.dq:
                for slot in self.dq[q]:
                    if slot[1] > 0:
                        self._wait(e, (slot[2], slot[0], slot[1], "dma"))


def _prm_layout(L):
    off = {}
    n = 0

    def add(name, cnt):
        nonlocal n
        off[name] = n
        n += cnt

    add("nm", L * 8)
    add("nf", L * 8)
    add("fn", 8)
    add("modb", L * 48)
    add("pscale", 2 * 4)
    add("convw", 2 * 4 * 31)
    add("convb", 2 * 4)
    add("lng", 2 * 4)
    add("lnb", 2 * 4)
    add("dnconv", 2 * 24 * 4)
    add("onorm", 2)
    add("alog", 2)
    add("dtb", 2)
    add("mlo", 1)
    add("mhi", 1)
    add("rbias", L * 36)
    return off, n


def _fm(v):
    v = np.asarray(v, np.float32)
    lead = v.shape[:-1]
    n = v.shape[-1] // 128
    a = v.reshape(lead + (n, 128))
    a = np.moveaxis(a, -1, 0)
    return np.ascontiguousarray(a.reshape(128, -1))


def _build_prm(inp, L):
    off, n = _prm_layout(L)
    P = np.zeros((128, n), np.float32)

    def put(name, arr):
        arr = np.asarray(arr, np.float32)
        P[:arr.shape[0], off[name]:off[name] + arr.shape[1]] = arr

    put("nm", _fm(inp["norm_mix"][:L]))
    put("nf", _fm(inp["norm_ffn"][:L]))
    put("fn", _fm(inp["final_norm"]))
    put("modb", _fm(inp["mod_b"][:L]))
    put("pscale", _fm(inp["pool_scale"]))
    cw = np.asarray(inp["conv_w"], np.float32)
    cw = cw.reshape(2, 31, 4, 128).transpose(3, 0, 2, 1)
    put("convw", cw.reshape(128, -1))
    put("convb", _fm(inp["conv_b"]))
    put("lng", _fm(inp["conv_ln_g"]))
    put("lnb", _fm(inp["conv_ln_b"]))
    dcw = np.asarray(inp["dn_conv_w"], np.float32)
    dcw = dcw.reshape(2, 4, 24, 128).transpose(3, 0, 2, 1)
    put("dnconv", dcw.reshape(128, -1))
    put("onorm", np.asarray(inp["dn_onorm"], np.float32).T)
    al = np.zeros((16, 2), np.float32)
    al[8:16, :] = np.asarray(inp["dn_a_log"], np.float32).T
    put("alog", al)
    db = np.zeros((16, 2), np.float32)
    db[8:16, :] = np.asarray(inp["dn_dt_bias"], np.float32).T
    put("dtb", db)
    mlo = np.zeros((16, 1), np.float32)
    mlo[0:8] = 1.0
    put("mlo", mlo)
    put("mhi", 1.0 - mlo)
    rb = np.concatenate([np.asarray(inp["moe_b_grp"], np.float32)[:L], np.asarray(inp["moe_b_exp"], np.float32)[:L]], axis=1)
    put("rbias", np.broadcast_to(rb.reshape(1, -1), (128, L * 36)))
    return P, off


def _build_cst():
    c = np.zeros((128, 640), np.float32)
    c[:, 0:128] = np.eye(128, dtype=np.float32)
    c[:, 128:256] = 1.0
    c[127, 256:384] = 1.0
    m = np.arange(128)[:, None]
    cc = np.arange(128)[None, :]
    c[:, 384:512] = np.where(cc > m, 0.0, NEG)
    c[:, 512:640] = np.where(cc >= m, 0.0, NEG)
    return c


def _build_mk():
    mk = np.zeros((128, 7, 2, 128), np.float32)
    r = np.arange(128)[:, None]
    c = np.arange(128)[None, :]
    for k_ in range(7):
        b = 1 << k_
        same = (r // (2 * b)) == (c // (2 * b))
        mk[:, k_, 0, :] = same & ((r % (2 * b)) >= b) & ((c % (2 * b)) < b)
        mk[:, k_, 1, :] = same & ((c % (2 * b)) >= b) & ((r % (2 * b)) < b)
    return np.ascontiguousarray(mk.reshape(128, -1))


def build_program(cfg):
    NSEQ, S, L = cfg["NSEQ"], cfg["S"], cfg["DEPTH"]
    NG = S // GS
    NT = S // 128
    off, NP = _prm_layout(L)
    nc = bass.Bass("TRN2", target_bir_lowering=False)

    def din(name, shape):
        return nc.dram_tensor(name, list(shape), F32, kind="ExternalInput").ap()

    x_d = din("x", [NSEQ * S, D])
    cT_d = din("cT", [128, 8 * NSEQ])
    cst_d = din("cst", [128, 640])
    prm_d = din("prm", [128, NP])
    mk_d = din("mk", [128, 7 * 2 * 128])
    modw_d = din("mod_w", [L, D, 6 * D])
    abin_d = din("ab_w_in", [2, D, 1536])
    poolw_d = din("pool_w", [2, 4, 128, 128])
    about_d = din("ab_w_out", [2, D, D])
    dnin_d = din("dn_w_in_h", [2, D, 4096])
    dnbg_d = din("dn_w_bg", [2, D, 16])
    dnout_d = din("dn_w_out", [2, D, D])
    moer_d = din("moe_w_r", [L, D, 36])
    moeup_d = din("moe_w_up", [L, NEXP, D, 2 * FE])
    moedn_d = din("moe_w_down", [L, NEXP, FE, D])
    out_d = nc.dram_tensor("out", [NSEQ * S, D], F32, kind="ExternalOutput").ap()

    es = ExitStack()
    k = K(nc, es)
    op, dma = k.op, k.dma

    xT = k.sb([128, KD, S], F32, name="xT")
    hnT = k.sb([128, KD, S], BF16, name="hnT")
    cst = k.sb([128, 640], F32, name="cst")
    identb = k.sb([128, 128], BF16, name="identb")
    onesb = k.sb([128, 128], BF16, name="onesb")
    prm = k.sb([128, NP], F32, name="prm")
    cact = k.sb([128, 8 * NSEQ], F32, name="cact")
    modT = k.sb([128, L, 48, NSEQ], F32, name="modT")
    modA = k.sb([128, L, 2, 8, NSEQ], F32, name="modA")
    wpool = [k.sb([128, KD, 512], BF16, name="wp%d" % i) for i in range(2)]
    wpi = [0]
    banks = [k.psb([128, 512], F32, "bank%d" % i) for i in range(7)]
    bankb = k.psb([128, 1024], BF16, "bankb")
    bi = [0]

    def nb():
        b = banks[bi[0] % 7]
        bi[0] += 1
        return b

    def nw():
        w = wpool[wpi[0] % 2]
        wpi[0] += 1
        return w

    ident = cst.t[:, 0:128]
    ones = cst.t[:, 128:256]
    sel127 = cst.t[:, 256:384]
    negm2 = cst.t[:, 384:640]

    def P(name, i=0, n=1, rows=128):
        o = off[name] + i
        return prm.t[0:rows, o:o + n]

    dma("sp", cst.t[:, :], cst_d[:, :], writes=[cst])
    dma("sp", prm.t[:, :], prm_d[:, :], writes=[prm])
    dma("sp", cact.t[:, :], cT_d[:, :], writes=[cact])
    dma("pool", identb.t[:, :], cst_d[:, 0:128], writes=[identb])
    dma("pool", onesb.t[:, :], cst_d[:, 128:256], writes=[onesb])
    op("act", lambda e: e.activation(out=cact.t[:, :], in_=cact.t[:, :], func=AF.Silu), reads=[cact], writes=[cact])

    with ExitStack() as pes:
        mw = [k.sb([128, KD, 512], F32, es=pes, name="mw%d" % i) for i in range(2)]
        mi = 0
        for l in range(L):
            for pc in range(12):
                w = mw[mi % 2]
                mi += 1
                dma("sp", w.t[:, :, :], modw_d[l, :, pc * 512:(pc + 1) * 512].rearrange("(k p) n -> p k n", p=128), writes=[w])
                pb = nb()
                for c4 in range(4):
                    for kk in range(KD):
                        op("pe", lambda e, c4=c4, kk=kk, w=w, pb=pb: e.matmul(
                            pb.t[:, c4 * NSEQ:(c4 + 1) * NSEQ], w.t[:, kk, c4 * 128:(c4 + 1) * 128],
                            cact.t[:, kk * NSEQ:(kk + 1) * NSEQ], start=(kk == 0), stop=(kk == KD - 1)),
                           reads=[w, cact], writes=[pb], inc=(kk == KD - 1))
                for c4 in range(4):
                    ch = pc * 4 + c4
                    op("dve", lambda e, c4=c4, ch=ch, pb=pb, l=l: e.tensor_scalar(
                        out=modT.t[:, l, ch, :], in0=pb.t[:, c4 * NSEQ:(c4 + 1) * NSEQ],
                        scalar1=P("modb", l * 48 + ch), scalar2=None, op0=ALU.add),
                       reads=[pb, prm], writes=[modT])
            for j, (nname, cbase) in enumerate((("nm", 8), ("nf", 32))):
                for kk in range(KD):
                    op("dve", lambda e, j=j, kk=kk, nname=nname, cbase=cbase, l=l: e.tensor_scalar(
                        out=modA.t[:, l, j, kk, :], in0=modT.t[:, l, cbase + kk, :],
                        scalar1=1.0, scalar2=P(nname, l * 8 + kk), op0=ALU.add, op1=ALU.mult),
                       reads=[modT, prm], writes=[modA])
    k.barrier()

    def rms_rstd(pes, g, scale_n):
        cols = slice(g * GS, (g + 1) * GS)
        pb = nb()
        for kk in range(KD):
            sq = scr["sqb"][kk % 2]
            op("act", lambda e, kk=kk, sq=sq: e.activation(out=sq.t[:, :], in_=xT.t[:, kk, cols], func=AF.Square),
               reads=[xT], writes=[sq])
            op("pe", lambda e, kk=kk, sq=sq, pb=pb: e.matmul(pb.t[:, :], ones, sq.t[:, :], start=(kk == 0), stop=(kk == KD - 1)),
               reads=[sq, cst], writes=[pb])
        rs = rsb[g % 2]
        op("act", lambda e: e.activation(out=rs.t[:, :], in_=pb.t[:, :], func=AF.Ln, scale=1.0 / scale_n, bias=EPS), reads=[pb], writes=[rs])
        op("act", lambda e: e.activation(out=rs.t[:, :], in_=rs.t[:, :], func=AF.Exp, scale=-0.5), reads=[rs], writes=[rs])
        return rs

    def make_hn(l, which, sl, g, hn32=None, rs=None):
        cols = slice(g * GS, (g + 1) * GS)
        if rs is None:
            rs = rms_rstd(None, g, float(D))
        shbase = 0 if which == 0 else 24
        for kk in range(KD):
            tmp = scr["tmpb"][kk % 2]
            op("dve", lambda e, kk=kk, tmp=tmp: e.tensor_tensor(out=tmp.t[:, :], in0=xT.t[:, kk, cols], in1=rs.t[:, :], op=ALU.mult),
               reads=[xT, rs], writes=[tmp])
            op("act", lambda e, kk=kk, tmp=tmp: e.activation(
                out=hnT.t[:, kk, cols], in_=tmp.t[:, :], func=AF.Identity,
                bias=modT.t[:, l, shbase + kk, sl:sl + 1], scale=modA.t[:, l, which, kk, sl:sl + 1]),
               reads=[tmp, modT, modA], writes=[hnT])
            if hn32 is not None:
                op("act", lambda e, kk=kk, tmp=tmp: e.activation(
                    out=hn32.t[:, kk, :], in_=tmp.t[:, :], func=AF.Identity,
                    bias=modT.t[:, l, shbase + kk, sl:sl + 1], scale=modA.t[:, l, which, kk, sl:sl + 1]),
                   reads=[tmp, modT, modA], writes=[hn32])

    def hn_pipeline(l, which, sl, hn32=None, after=None):
        rs_next = rms_rstd(None, 0, float(D))
        for g in range(NG):
            rs = rs_next
            if g + 1 < NG:
                rs_next = rms_rstd(None, g + 1, float(D))
            make_hn(l, which, sl, g, hn32=hn32, rs=rs)
            if after is not None:
                after(g)

    def load_w(src_ap, ncols=512):
        w = nw()
        dma("pool", w.t[:, :, 0:ncols], src_ap.rearrange("(k p) n -> p k n", p=128), writes=[w])
        return w

    def proj(w, wc0, g, pb, pcols=None):
        cols = slice(g * GS, (g + 1) * GS)
        for kk in range(KD):
            op("pe", lambda e, kk=kk: e.matmul(pb.t[:, :], w.t[:, kk, wc0:wc0 + 128], hnT.t[:, kk, cols],
                                               start=(kk == 0), stop=(kk == KD - 1)),
               reads=[w, hnT], writes=[pb], inc=(kk == KD - 1))

    def resid_add(pb, l, gate_chunk_base, kk, sl, cols):
        op("dve", lambda e: e.scalar_tensor_tensor(
            out=xT.t[:, kk, cols], in0=pb.t[:, :], scalar=modT.t[:, l, gate_chunk_base + kk, sl:sl + 1],
            in1=xT.t[:, kk, cols], op0=ALU.mult, op1=ALU.add), reads=[pb, modT, xT], writes=[xT])

    rsb = [k.sb([128, GS], F32, name="rs%d" % i) for i in range(2)]
    scr = {}

    def alloc_scr(es_):
        scr["sqb"] = [k.sb([128, GS], F32, es=es_, name="sq%d" % i) for i in range(2)]
        scr["tmpb"] = [k.sb([128, GS], F32, es=es_, name="tmp%d" % i) for i in range(2)]

    for sl in range(NSEQ):
        with ExitStack() as pes:
            xin = [k.sb([128, D], F32, es=pes, name="xin%d" % i) for i in range(2)]
            for i in range(NT):
                xi = xin[i % 2]
                dma("sp", xi.t[:, :], x_d[sl * S + i * 128: sl * S + (i + 1) * 128, :], writes=[xi])
                for half in range(2):
                    pb = nb()
                    for c4 in range(4):
                        kk = half * 4 + c4
                        op("pe", lambda e, kk=kk, c4=c4, pb=pb, xi=xi: e.transpose(
                            pb.t[:, c4 * 128:(c4 + 1) * 128], xi.t[:, kk * 128:(kk + 1) * 128], ident),
                           reads=[xi, cst], writes=[pb], inc=(c4 == 3))
                    eng = "act" if half == 0 else "dve"
                    if eng == "act":
                        op("act", lambda e, half=half, pb=pb, i=i: e.activation(
                            out=xT.t[:, half * 4:(half + 1) * 4, i * 128:(i + 1) * 128],
                            in_=pb.t[:, :].rearrange("p (a b) -> p a b", a=4), func=AF.Copy), reads=[pb], writes=[xT])
                    else:
                        op("dve", lambda e, half=half, pb=pb, i=i: e.tensor_copy(
                            out=xT.t[:, half * 4:(half + 1) * 4, i * 128:(i + 1) * 128],
                            in_=pb.t[:, :].rearrange("p (a b) -> p a b", a=4)), reads=[pb], writes=[xT])
        k.barrier()

        for l in range(L):
            li = l // 2
            with ExitStack() as hs:
                alloc_scr(hs)
                hn_pipeline(l, 0, sl)
                k.barrier()
            if l % 2 == 0:
                even_mixer(k, nc, locals())
            else:
                odd_mixer(k, nc, locals())
            k.barrier()
            moe_layer(k, nc, locals())
            k.barrier()

        with ExitStack() as pes:
            alloc_scr(pes)
            xn = [k.sb([128, KD, GS], F32, es=pes, name="xn%d" % i) for i in range(2)]
            ost = [k.sb([128, D], F32, es=pes, name="ost%d" % i) for i in range(2)]
            for g in range(NG):
                cols = slice(g * GS, (g + 1) * GS)
                rs = rms_rstd(None, g, float(D))
                xg = xn[g % 2]
                for kk in range(KD):
                    op("dve", lambda e, kk=kk: e.scalar_tensor_tensor(
                        out=xg.t[:, kk, :], in0=xT.t[:, kk, cols], scalar=P("fn", kk), in1=rs.t[:, :],
                        op0=ALU.mult, op1=ALU.mult), reads=[xT, prm, rs], writes=[xg])
                for ti in range(GS // 128):
                    i = g * (GS // 128) + ti
                    o = ost[i % 2]
                    for half in range(2):
                        pb = nb()
                        for c4 in range(4):
                            kk = half * 4 + c4
                            op("pe", lambda e, kk=kk, c4=c4, pb=pb: e.transpose(
                                pb.t[:, c4 * 128:(c4 + 1) * 128], xg.t[:, kk, ti * 128:(ti + 1) * 128], ident),
                               reads=[xg, cst], writes=[pb], inc=(c4 == 3))
                        if half == 0:
                            op("act", lambda e, pb=pb, o=o: e.activation(out=o.t[:, 0:512], in_=pb.t[:, :], func=AF.Copy),
                               reads=[pb], writes=[o])
                        else:
                            op("dve", lambda e, pb=pb, o=o: e.tensor_copy(out=o.t[:, 512:1024], in_=pb.t[:, :]),
                               reads=[pb], writes=[o])
                    dma("sp", out_d[sl * S + i * 128: sl * S + (i + 1) * 128, :], o.t[:, :], reads=[o])
        k.barrier()

    k.barrier(["sp"])
    es.close()
    return nc, k


def even_mixer(k, nc, env):
    op, dma = k.op, k.dma
    xT, hnT, modT, prm, cst = env["xT"], env["hnT"], env["modT"], env["prm"], env["cst"]
    nb, load_w, proj, resid_add, P = env["nb"], env["load_w"], env["proj"], env["resid_add"], env["P"]
    l, li, sl, S, NG = env["l"], env["li"], env["sl"], env["S"], env["NG"]
    abin_d, poolw_d, about_d = env["abin_d"], env["poolw_d"], env["about_d"]
    ones = env["ones"]
    rsb = env["rsb"]
    WIN = (2, 4, 8, 16)
    with ExitStack() as pes:
        env["alloc_scr"](pes)
        sqb, tmpb = env["scr"]["sqb"], env["scr"]["tmpb"]
        ua = [k.sb([128, 4, 16 + GS], F32, es=pes, name="ua%d" % i) for i in range(2)]
        tl = [k.sb([128, 16 + GS], F32, es=pes, name="tl%d" % i) for i in range(2)]
        glu = [k.sb([128, 4, 30 + GS], F32, es=pes, name="glu%d" % i) for i in range(2)]
        acc = k.sb([128, 4, GS], F32, es=pes, name="cacc")
        pooled = k.sb([128, 4, GS], BF16, es=pes, name="pooled")
        pw = k.sb([128, 4, 128], BF16, es=pes, name="poolw")
        sig = [k.sb([128, GS], F32, es=pes, name="sig%d" % i) for i in range(2)]
        ct = k.sb([128, 8, GS], BF16, es=pes, name="cat")
        mu, rstd = rsb[0], rsb[1]
        dma("pool", pw.t[:, :, :], poolw_d[li].rearrange("j c d -> c j d"), writes=[pw])
        for g in range(NG):
            cols = slice(g * GS, (g + 1) * GS)
            u_, g_ = ua[g % 2], glu[g % 2]
            if g == 0:
                op("pool", lambda e: e.memset(u_.t[:, :, 0:16], 0.0), writes=[u_])
                op("pool", lambda e: e.memset(g_.t[:, :, 0:30], 0.0), writes=[g_])
            else:
                up, gp = ua[(g - 1) % 2], glu[(g - 1) % 2]
                op("pool", lambda e: e.tensor_copy(out=u_.t[:, :, 0:16], in_=up.t[:, :, GS:GS + 16]), reads=[up], writes=[u_])
                op("pool", lambda e: e.tensor_copy(out=g_.t[:, :, 0:30], in_=gp.t[:, :, GS:GS + 30]), reads=[gp], writes=[g_])
            w0 = load_w(abin_d[li, :, 0:512])
            for j in range(4):
                pb = nb()
                proj(w0, j * 128, g, pb)
                op("act", lambda e, j=j, pb=pb: e.activation(out=u_.t[:, j, 16:16 + GS], in_=pb.t[:, :], func=AF.Copy), reads=[pb], writes=[u_])
            wv = load_w(abin_d[li, :, 512:1024])
            wg = load_w(abin_d[li, :, 1024:1536])
            for j in range(4):
                pg = nb()
                proj(wg, j * 128, g, pg)
                sg = sig[j % 2]
                op("act", lambda e, pg=pg, sg=sg: e.activation(out=sg.t[:, :], in_=pg.t[:, :], func=AF.Sigmoid), reads=[pg], writes=[sg])
                pv = nb()
                proj(wv, j * 128, g, pv)
                op("dve", lambda e, j=j, pv=pv, sg=sg: e.tensor_tensor(out=g_.t[:, j, 30:30 + GS], in0=pv.t[:, :], in1=sg.t[:, :], op=ALU.mult),
                   reads=[pv, sg], writes=[g_])
            for j in range(4):
                w_ = WIN[j]
                prev_ap = u_.t[:, j, :]
                prev_buf = u_
                for lev in range(j + 1):
                    sh = 1 << lev
                    c0 = (2 << lev) - 1
                    dst = tl[lev % 2]
                    op("dve", lambda e, dst=dst, prev_ap=prev_ap, sh=sh, c0=c0: e.tensor_tensor(
                        out=dst.t[:, c0:16 + GS], in0=prev_ap[:, c0:16 + GS], in1=prev_ap[:, c0 - sh:16 + GS - sh], op=ALU.add),
                       reads=[prev_buf], writes=[dst])
                    prev_ap = dst.t[:, :]
                    prev_buf = dst
                if g == 0:
                    for t in range(w_ - 1):
                        op("dve", lambda e, t=t, prev_ap=prev_ap, w_=w_: e.tensor_scalar_mul(
                            out=prev_ap[:, 16 + t:17 + t], in0=prev_ap[:, 16 + t:17 + t], scalar1=float(w_) / (t + 1)),
                           reads=[prev_buf], writes=[prev_buf])
                op("dve", lambda e, j=j, prev_ap=prev_ap, w_=w_: e.scalar_tensor_tensor(
                    out=pooled.t[:, j, :], in0=prev_ap[:, 16:16 + GS], scalar=1.0 / w_, in1=u_.t[:, j, 16:16 + GS],
                    op0=ALU.mult, op1=ALU.subtract), reads=[prev_buf, u_], writes=[pooled])
            for j in range(4):
                eng = "dve"
                for tap in range(31):
                    wcol = P("convw", (li * 4 + j) * 31 + tap)
                    srcv = g_.t[:, j, tap:tap + GS]
                    if tap == 0:
                        op(eng, lambda e, j=j, srcv=srcv, wcol=wcol: e.tensor_scalar(
                            out=acc.t[:, j, :], in0=srcv, scalar1=wcol, scalar2=P("convb", li * 4 + j),
                            op0=ALU.mult, op1=ALU.add), reads=[g_, prm], writes=[acc])
                    else:
                        op(eng, lambda e, j=j, srcv=srcv, wcol=wcol: e.scalar_tensor_tensor(
                            out=acc.t[:, j, :], in0=srcv, scalar=wcol, in1=acc.t[:, j, :],
                            op0=ALU.mult, op1=ALU.add), reads=[g_, prm, acc], writes=[acc])
            for j in range(4):
                pb = nb()
                op("pe", lambda e, j=j, pb=pb: e.matmul(pb.t[:, :], pw.t[:, j, :], pooled.t[:, j, :], start=True, stop=True),
                   reads=[pw, pooled], writes=[pb])
                op("act", lambda e, j=j, pb=pb: e.activation(out=ct.t[:, j, :], in_=pb.t[:, :], func=AF.Copy,
                                                             scale=P("pscale", li * 4 + j)), reads=[pb, prm], writes=[ct])
            pm = nb()
            pq = nb()
            for j in range(4):
                s2 = sqb[j % 2]
                op("act", lambda e, j=j, s2=s2: e.activation(out=s2.t[:, :], in_=acc.t[:, j, :], func=AF.Square), reads=[acc], writes=[s2])
                op("pe", lambda e, j=j: e.matmul(pm.t[:, :], ones, acc.t[:, j, :], start=(j == 0), stop=(j == 3)),
                   reads=[acc, cst], writes=[pm])
                op("pe", lambda e, s2=s2, j=j: e.matmul(pq.t[:, :], ones, s2.t[:, :], start=(j == 0), stop=(j == 3)),
                   reads=[s2, cst], writes=[pq])
            op("act", lambda e: e.activation(out=mu.t[:, :], in_=pm.t[:, :], func=AF.Copy, scale=1.0 / 512), reads=[pm], writes=[mu])
            op("dve", lambda e: e.tensor_tensor(out=rstd.t[:, :], in0=mu.t[:, :], in1=mu.t[:, :], op=ALU.mult), reads=[mu], writes=[rstd])
            op("dve", lambda e: e.scalar_tensor_tensor(out=rstd.t[:, :], in0=pq.t[:, :], scalar=1.0 / 512, in1=rstd.t[:, :],
                                                       op0=ALU.mult, op1=ALU.subtract), reads=[pq, rstd], writes=[rstd])
            op("act", lambda e: e.activation(out=rstd.t[:, :], in_=rstd.t[:, :], func=AF.Ln, bias=EPS), reads=[rstd], writes=[rstd])
            op("act", lambda e: e.activation(out=rstd.t[:, :], in_=rstd.t[:, :], func=AF.Exp, scale=-0.5), reads=[rstd], writes=[rstd])
            for j in range(4):
                t_ = tmpb[j % 2]
                op("dve", lambda e, j=j, t_=t_: e.tensor_tensor(out=t_.t[:, :], in0=acc.t[:, j, :], in1=mu.t[:, :], op=ALU.subtract),
                   reads=[acc, mu], writes=[t_])
                op("dve", lambda e, t_=t_: e.tensor_tensor(out=t_.t[:, :], in0=t_.t[:, :], in1=rstd.t[:, :], op=ALU.mult),
                   reads=[t_, rstd], writes=[t_])
                op("act", lambda e, j=j, t_=t_: e.activation(
                    out=ct.t[:, 4 + j, :], in_=t_.t[:, :], func=AF.Silu, bias=P("lnb", li * 4 + j), scale=P("lng", li * 4 + j)),
                   reads=[t_, prm], writes=[ct])
            wo = [load_w(about_d[li, :, h * 512:(h + 1) * 512]) for h in range(2)]
            for oc in range(8):
                pb = nb()
                w = wo[oc // 4]
                for kk in range(8):
                    op("pe", lambda e, kk=kk, oc=oc, w=w, pb=pb: e.matmul(
                        pb.t[:, :], w.t[:, kk, (oc % 4) * 128:(oc % 4 + 1) * 128], ct.t[:, kk, :], start=(kk == 0), stop=(kk == 7)),
                       reads=[w, ct], writes=[pb], inc=(kk == 7))
                resid_add(pb, l, 16, oc, sl, cols)


def odd_mixer(k, nc, env):
    op, dma = k.op, k.dma
    xT, hnT, modT, prm, cst = env["xT"], env["hnT"], env["modT"], env["prm"], env["cst"]
    nb, load_w, proj, resid_add, P = env["nb"], env["load_w"], env["proj"], env["resid_add"], env["P"]
    l, li, sl, S, NG, NT = env["l"], env["li"], env["sl"], env["S"], env["NG"], env["NT"]
    dnin_d, dnbg_d, dnout_d = env["dnin_d"], env["dnbg_d"], env["dnout_d"]
    ones, ident, sel127, negm2, identb, bankb = env["ones"], env["ident"], env["sel127"], env["negm2"], env["identb"], env["bankb"]
    onesb = env["onesb"]
    TG = GS // 128
    with ExitStack() as pes:
        def sbt(shape, dt, name):
            return k.sb(shape, dt, es=pes, name=name)
        BG = sbt([16, S], F32, "BG")
        TM = sbt([128, NT, 16], F32, "TM")
        egc = sbt([128, NT, 16], F32, "egc")
        bexp = sbt([128, NT, 8], F32, "bexp")
        kdec = sbt([128, NT, 16], F32, "kdec")
        gl = sbt([128, NT, 16], F32, "gl")
        dl = gl
        wbg = sbt([128, KD, 16], BF16, "wbg")
        nea = sbt([16, 1], F32, "nea")
        res2 = ExitStack()
        r1 = [k.sb([16, GS], F32, es=res2, name="r1_%d" % i) for i in range(2)]
        r2 = [k.sb([16, GS], F32, es=res2, name="r2_%d" % i) for i in range(2)]
        dma("pool", wbg.t[:, :, :], dnbg_d[li].rearrange("(k p) n -> p k n", p=128), writes=[wbg])
        op("act", lambda e: e.activation(out=nea.t[:, :], in_=P("alog", li, rows=16), func=AF.Exp), reads=[prm], writes=[nea])
        op("dve", lambda e: e.tensor_scalar_mul(out=nea.t[:, :], in0=nea.t[:, :], scalar1=-1.0), reads=[nea], writes=[nea])
        for g in range(NG):
            cols = slice(g * GS, (g + 1) * GS)
            pb = nb()
            for kk in range(KD):
                op("pe", lambda e, kk=kk, pb=pb: e.matmul(pb.t[0:16, :], wbg.t[:, kk, :], hnT.t[:, kk, cols],
                                                        start=(kk == 0), stop=(kk == KD - 1)),
                   reads=[wbg, hnT], writes=[pb], inc=(kk == KD - 1))
            a, b = r1[0], r1[1]
            c_, d_ = r2[0], r2[1]
            op("act", lambda e, pb=pb: e.activation(out=a.t[:, :], in_=pb.t[0:16, :], func=AF.Sigmoid), reads=[pb], writes=[a])
            op("act", lambda e, pb=pb: e.activation(out=b.t[:, :], in_=pb.t[0:16, :], func=AF.Exp, bias=P("dtb", li, rows=16)),
               reads=[pb, prm], writes=[b])
            op("act", lambda e: e.activation(out=b.t[:, :], in_=b.t[:, :], func=AF.Ln, bias=1.0), reads=[b], writes=[b])
            op("dve", lambda e: e.tensor_scalar_mul(out=b.t[:, :], in0=b.t[:, :], scalar1=nea.t[:, 0:1]), reads=[b, nea], writes=[b])
            src, dst = b, c_
            for lev in range(7):
                sh = 1 << lev
                sv = src.t[:, :].rearrange("p (a t) -> p a t", t=128)
                dv = dst.t[:, :].rearrange("p (a t) -> p a t", t=128)
                op("dve", lambda e, sv=sv, dv=dv, sh=sh: e.tensor_tensor(out=dv[:, :, sh:128], in0=sv[:, :, sh:128],
                                                                         in1=sv[:, :, 0:128 - sh], op=ALU.add),
                   reads=[src], writes=[dst])
                op("dve", lambda e, sv=sv, dv=dv, sh=sh: e.tensor_copy(out=dv[:, :, 0:sh], in_=sv[:, :, 0:sh]),
                   reads=[src], writes=[dst])
                src, dst = dst, src
            op("dve", lambda e: e.tensor_scalar_mul(out=d_.t[:, :], in0=a.t[:, :], scalar1=P("mlo", rows=16)), reads=[a, prm], writes=[d_])
            op("dve", lambda e, src=src: e.scalar_tensor_tensor(out=BG.t[:, cols], in0=src.t[:, :], scalar=P("mhi", rows=16), in1=d_.t[:, :],
                                                                op0=ALU.mult, op1=ALU.add), reads=[src, d_, prm], writes=[BG])
        pb = nb()
        for i in range(NT):
            op("pe", lambda e, i=i, pb=pb: e.transpose(pb.t[:, i * 16:(i + 1) * 16], BG.t[:, i * 128:(i + 1) * 128], ident[0:16, 0:16]),
               reads=[BG, cst], writes=[pb], inc=(i == NT - 1))
        op("act", lambda e, pb=pb: e.activation(out=TM.t[:, :, :], in_=pb.t[:, 0:NT * 16].rearrange("p (a b) -> p a b", b=16), func=AF.Copy),
           reads=[pb], writes=[TM])
        pb2 = nb()
        op("pe", lambda e: e.matmul(pb2.t[:, 0:NT * 16], sel127, TM.t[:, :, :].rearrange("p a b -> p (a b)"), start=True, stop=True),
           reads=[TM, cst], writes=[pb2])
        op("act", lambda e: e.activation(out=gl.t[:, :, :], in_=pb2.t[:, 0:NT * 16].rearrange("p (a b) -> p a b", b=16), func=AF.Copy),
           reads=[pb2], writes=[gl])
        op("act", lambda e: e.activation(out=egc.t[:, :, :], in_=TM.t[:, :, :], func=AF.Exp), reads=[TM], writes=[egc])
        op("dve", lambda e: e.tensor_tensor(out=bexp.t[:, :, :], in0=TM.t[:, :, 0:8], in1=egc.t[:, :, 8:16], op=ALU.mult),
           reads=[TM, egc], writes=[bexp])
        op("dve", lambda e: e.tensor_tensor(out=kdec.t[:, :, :], in0=gl.t[:, :, :], in1=TM.t[:, :, :], op=ALU.subtract),
           reads=[gl, TM], writes=[kdec])
        op("act", lambda e: e.activation(out=kdec.t[:, :, :], in_=kdec.t[:, :, :], func=AF.Exp), reads=[kdec], writes=[kdec])
        op("act", lambda e: e.activation(out=dl.t[:, :, :], in_=gl.t[:, :, :], func=AF.Exp), reads=[gl], writes=[dl])

        k.barrier()
        res2.close()
        uraw = [sbt([128, 3 + GS], F32, "uraw%d" % q) for q in range(3)]
        cacc = [sbt([128, GS], F32, "cacc%d" % q) for q in range(3)]
        rn = env["rsb"]
        qn = sbt([128, GS], F32, "qn")
        kn = sbt([128, GS], F32, "kn")
        rowm = [sbt([16, GS], F32, "rowm0")] * 2
        beta_b = sbt([128, GS], F32, "beta_b")
        gc_b = sbt([128, GS], F32, "gc_b")
        eg_b = beta_b
        kT = sbt([128, GS], BF16, "kT")
        nkbT = sbt([128, GS], BF16, "nkbT")
        nkT = sbt([128, GS], BF16, "nkT")
        qT = sbt([128, GS], BF16, "qT")
        qgTs = [sbt([128, GS], BF16, "qgT%d" % i) for i in range(2)]
        zss = [sbt([128, GS], BF16, "zs%d" % i) for i in range(2)]
        tmp1 = sbt([128, TG, 128], F32, "dtmp1")
        tmp2 = sbt([128, TG, 2, 128], F32, "dtmp2")
        X2s = [sbt([128, TG, 2, 128], BF16, "X2_%d" % i) for i in range(2)]
        NLb = sbt([128, TG, 2, 128], BF16, "NLb")
        NCks = [sbt([128, 2, 2, 128], BF16, "NCk%d" % i) for i in range(2)]
        Ybs = [sbt([128, 2, 2, 128], BF16, "Yb%d" % i) for i in range(2)]
        Tbs = [[sbt([128, 2, 2, 128], BF16, "Tb%d_%d" % (i, j)) for j in range(2)] for i in range(2)]
        mkb = sbt([128, 7, 2, 128], BF16, "mkb")
        dma("pool", mkb.t[:, :, :, :], env["mk_d"][:, :].rearrange("p (a b c) -> p a b c", a=7, b=2), writes=[mkb])
        kbgs = [sbt([128, TG, 128], BF16, "kbg%d" % i) for i in range(2)]
        kds = [sbt([128, TG, 128], BF16, "kd%d" % i) for i in range(2)]
        vbs = [sbt([128, TG, 128], BF16, "vb%d" % i) for i in range(2)]
        nwT = sbt([128, TG, 128], BF16, "nwT")
        vnew = [sbt([128, 128], BF16, "vnew%d" % i) for i in range(2)]
        S32 = sbt([128, 128], F32, "S32")
        Sbf = sbt([128, 128], BF16, "Sbf")
        o32 = sbt([128, GS], F32, "o32")
        og = o32
        og2 = sbt([128, GS], BF16, "og2")
        wo = sbt([128, D], BF16, "wo_h")

        U = 8 * NG

        banks_ = env["banks"]
        bctr = [0, 0]

        def nb1():
            bctr[0] += 1
            return banks_[(bctr[0] - 1) % 4]

        def nb2():
            bctr[1] += 1
            return banks_[4 + (bctr[1] - 1) % 3]

        def stage1(u):
            h, g = divmod(u, NG)
            p = u % 2
            X2, kbg, kd, vb, qgT, zs = X2s[p], kbgs[p], kds[p], vbs[p], qgTs[p], zss[p]
            if g == 0:
                wcur[0] = load_w(dnin_d[li, :, h * 512:(h + 1) * 512])
            w = wcur[0]
            cols = slice(g * GS, (g + 1) * GS)
            for q in range(3):
                ur = uraw[q]
                if g == 0:
                    op("pool", lambda e, ur=ur: e.memset(ur.t[:, 0:3], 0.0), writes=[ur])
                else:
                    op("pool", lambda e, ur=ur: e.tensor_copy(out=ur.t[:, 0:3], in_=ur.t[:, GS:GS + 3]), reads=[ur], writes=[ur])
                pb = nb1()
                proj(w, q * 128, g, pb)
                op("act", lambda e, ur=ur, pb=pb: e.activation(out=ur.t[:, 3:3 + GS], in_=pb.t[:, :], func=AF.Copy), reads=[pb], writes=[ur])
            pb = nb1()
            proj(w, 3 * 128, g, pb)
            op("act", lambda e, pb=pb: e.activation(out=zs.t[:, :], in_=pb.t[:, :], func=AF.Silu), reads=[pb], writes=[zs])
            for q in range(3):
                ur = uraw[q]
                ca = cacc[q]
                cb = (li * 24 + q * 8 + h) * 4
                op("pool", lambda e, ur=ur, ca=ca, cb=cb: e.tensor_scalar_mul(out=ca.t[:, :], in0=ur.t[:, 3:3 + GS], scalar1=P("dnconv", cb + 3)),
                   reads=[ur, prm], writes=[ca])
                for tap in range(3):
                    op("dve", lambda e, ur=ur, ca=ca, cb=cb, tap=tap: e.scalar_tensor_tensor(
                        out=ca.t[:, :], in0=ur.t[:, tap:tap + GS], scalar=P("dnconv", cb + tap), in1=ca.t[:, :],
                        op0=ALU.mult, op1=ALU.add), reads=[ur, prm, ca], writes=[ca])
                op("act", lambda e, ca=ca: e.activation(out=ca.t[:, :], in_=ca.t[:, :], func=AF.Silu), reads=[ca], writes=[ca])
            for q in range(2):
                ca = cacc[q]
                sqs = (qT, qgT)[q]
                op("act", lambda e, ca=ca, sqs=sqs: e.activation(out=sqs.t[:, :], in_=ca.t[:, :], func=AF.Square), reads=[ca], writes=[sqs])
                pb = nb1()
                op("pe", lambda e, pb=pb, sqs=sqs: e.matmul(pb.t[:, :], onesb.t[:, :], sqs.t[:, :], start=True, stop=True),
                   reads=[sqs, onesb], writes=[pb])
                op("act", lambda e, pb=pb: e.activation(out=rn[0].t[:, :], in_=pb.t[:, :], func=AF.Ln, bias=EPS), reads=[pb], writes=[rn[0]])
                op("act", lambda e: e.activation(out=rn[0].t[:, :], in_=rn[0].t[:, :], func=AF.Exp, scale=-0.5), reads=[rn[0]], writes=[rn[0]])
                if q == 0:
                    op("dve", lambda e: e.scalar_tensor_tensor(out=qn.t[:, :], in0=cacc[0].t[:, :], scalar=128.0 ** -0.5, in1=rn[0].t[:, :],
                                                               op0=ALU.mult, op1=ALU.mult), reads=[cacc[0], rn[0]], writes=[qn])
                else:
                    op("pool", lambda e: e.tensor_tensor(out=kn.t[:, :], in0=cacc[1].t[:, :], in1=rn[0].t[:, :], op=ALU.mult),
                       reads=[cacc[1], rn[0]], writes=[kn])
            for qi, (row, dstb) in enumerate(((h, beta_b), (8 + h, gc_b))):
                rm = rowm[qi]
                op("dve", lambda e, rm=rm, row=row: e.tensor_scalar_mul(out=rm.t[:, :], in0=BG.t[:, cols], scalar1=ident[0:16, row:row + 1]),
                   reads=[BG, cst], writes=[rm])
                pb = nb1()
                op("pe", lambda e, rm=rm, pb=pb: e.matmul(pb.t[:, :], ones[0:16, :], rm.t[:, :], start=True, stop=True),
                   reads=[rm, cst], writes=[pb])
                op("act", lambda e, pb=pb, dstb=dstb: e.activation(out=dstb.t[:, :], in_=pb.t[:, :], func=AF.Copy), reads=[pb], writes=[dstb])
            op("act", lambda e: e.activation(out=kT.t[:, :], in_=kn.t[:, :], func=AF.Copy), reads=[kn], writes=[kT])
            op("act", lambda e: e.activation(out=nkT.t[:, :], in_=kn.t[:, :], func=AF.Copy, scale=-1.0), reads=[kn], writes=[nkT])
            op("pool", lambda e: e.tensor_tensor(out=nkbT.t[:, :], in0=kn.t[:, :], in1=beta_b.t[:, :], op=ALU.mult),
               reads=[kn, beta_b], writes=[nkbT])
            op("act", lambda e: e.activation(out=eg_b.t[:, :], in_=gc_b.t[:, :], func=AF.Exp), reads=[gc_b], writes=[eg_b])
            op("act", lambda e: e.activation(out=qT.t[:, :], in_=qn.t[:, :], func=AF.Copy), reads=[qn], writes=[qT])
            op("pool", lambda e: e.tensor_tensor(out=qgT.t[:, :], in0=qn.t[:, :], in1=eg_b.t[:, :], op=ALU.mult),
               reads=[qn, eg_b], writes=[qgT])
            pk = nb1()
            pv = nb1()
            for t in range(TG):
                tc_ = slice(t * 128, (t + 1) * 128)
                op("pe", lambda e, t=t, tc_=tc_: e.transpose(pk.t[:, tc_], kn.t[:, tc_], ident), reads=[kn, cst], writes=[pk], inc=(t == TG - 1))
            for t in range(TG):
                tc_ = slice(t * 128, (t + 1) * 128)
                op("pe", lambda e, t=t, tc_=tc_: e.transpose(pv.t[:, tc_], cacc[2].t[:, tc_], ident), reads=[cacc[2], cst], writes=[pv], inc=(t == TG - 1))
            pm = [nb1(), nb1()]
            for t in range(TG):
                ti = g * TG + t
                tc_ = slice(t * 128, (t + 1) * 128)
                op("act", lambda e, t=t, ti=ti, tc_=tc_: e.activation(out=kbg.t[:, t, :], in_=pk.t[:, tc_], func=AF.Copy, scale=bexp.t[:, ti, h:h + 1]),
                   reads=[pk, bexp], writes=[kbg])
                op("act", lambda e, t=t, ti=ti, tc_=tc_: e.activation(out=kd.t[:, t, :], in_=pk.t[:, tc_], func=AF.Copy, scale=kdec.t[:, ti, 8 + h:9 + h]),
                   reads=[pk, kdec], writes=[kd])
                op("act", lambda e, t=t, ti=ti, tc_=tc_: e.activation(out=vb.t[:, t, :], in_=pv.t[:, tc_], func=AF.Copy, scale=TM.t[:, ti, h:h + 1]),
                   reads=[pv, TM], writes=[vb])
                op("dve", lambda e, t=t, ti=ti, tc_=tc_: e.tensor_scalar(out=tmp1.t[:, t, :], in0=gc_b.t[:, tc_], scalar1=TM.t[:, ti, 8 + h:9 + h],
                                                                         scalar2=0.0, op0=ALU.subtract, op1=ALU.min), reads=[gc_b, TM], writes=[tmp1])
                op("dve", lambda e, t=t: e.tensor_tensor(out=tmp2.t[:, t, :, :], in0=tmp1.t[:, t, :].unsqueeze(1).to_broadcast([128, 2, 128]),
                                                          in1=negm2.rearrange("p (a b) -> p a b", a=2), op=ALU.add), reads=[tmp1, cst], writes=[tmp2])
                pmm = pm[t // 2]
                o0 = (t % 2) * 256
                op("pe", lambda e, tc_=tc_, pmm=pmm, o0=o0: e.matmul(pmm.t[:, o0:o0 + 128], nkT.t[:, tc_], nkbT.t[:, tc_], start=True, stop=True),
                   reads=[nkT, nkbT], writes=[pmm], inc=False)
                op("pe", lambda e, tc_=tc_, pmm=pmm, o0=o0: e.matmul(pmm.t[:, o0 + 128:o0 + 256], kT.t[:, tc_], qT.t[:, tc_], start=True, stop=True),
                   reads=[kT, qT], writes=[pmm])
            op("act", lambda e: e.activation(out=tmp2.t[:, :, :, :], in_=tmp2.t[:, :, :, :], func=AF.Exp), reads=[tmp2], writes=[tmp2])
            for hf in range(2):
                op("dve", lambda e, hf=hf: e.tensor_tensor(
                    out=X2.t[:, 2 * hf:2 * hf + 2, :, :], in0=pm[hf].t[:, :].rearrange("p (a b c) -> p a b c", a=2, b=2),
                    in1=tmp2.t[:, 2 * hf:2 * hf + 2, :, :], op=ALU.mult), reads=[pm[hf], tmp2], writes=[X2])

        def stage2(u):
            h, g = divmod(u, NG)
            p = u % 2
            X2, kbg, kd, vb, qgT, zs = X2s[p], kbgs[p], kds[p], vbs[p], qgTs[p], zss[p]
            cols = slice(g * GS, (g + 1) * GS)
            if g == 0:
                dma("pool", wo.t[:, :], dnout_d[li, h * 128:(h + 1) * 128, :], writes=[wo])
                op("dve", lambda e: e.memset(S32.t[:, :], 0.0), writes=[S32])
                op("dve", lambda e: e.memset(Sbf.t[:, :], 0.0), writes=[Sbf])
            for t in range(TG):
                op("pe", lambda e, t=t: e.transpose(bankb.t[:, t * 128:(t + 1) * 128], X2.t[:, t, 0, :], identb.t[:, :]),
                   reads=[X2, identb], writes=[bankb], inc=(t == TG - 1))
            op("act", lambda e: e.activation(out=NLb.t[:, :, 0, :], in_=bankb.t[:, 0:TG * 128].rearrange("p (a b) -> p a b", a=TG), func=AF.Copy),
               reads=[bankb], writes=[NLb])
            op("act", lambda e: e.activation(out=NLb.t[:, :, 1, :], in_=X2.t[:, :, 0, :], func=AF.Copy), reads=[X2], writes=[NLb])
            for hf in range(2):
                op("pool", lambda e, hf=hf: e.tensor_tensor(out=NCks[hf].t[:, :, :, :], in0=NLb.t[:, 2 * hf:2 * hf + 2, :, :],
                                                           in1=mkb.t[:, 0, :, :].unsqueeze(1).to_broadcast([128, 2, 2, 128]), op=ALU.mult),
                   reads=[NLb, mkb], writes=[NCks[hf]])
            Tc = Tbs[0]
            for hf in range(2):
                op("pool", lambda e, hf=hf, Tc=Tc: e.tensor_tensor(out=Tc[hf].t[:, :, :, :], in0=NCks[hf].t[:, :, :, :],
                                                                   in1=identb.t[:, :].unsqueeze(1).unsqueeze(1).to_broadcast([128, 2, 2, 128]), op=ALU.add),
                   reads=[NCks[hf], identb], writes=[Tc[hf]])
            for lev in range(1, 7):
                last = (lev == 6)
                for hf in range(2):
                    op("pool", lambda e, hf=hf, lev=lev: e.tensor_tensor(out=NCks[hf].t[:, :, :, :], in0=NLb.t[:, 2 * hf:2 * hf + 2, :, :],
                                                                        in1=mkb.t[:, lev, :, :].unsqueeze(1).to_broadcast([128, 2, 2, 128]), op=ALU.mult),
                       reads=[NLb, mkb], writes=[NCks[hf]])
                py = [nb2(), nb2()]
                for t in range(TG):
                    hf, tt_ = t // 2, t % 2
                    ppp = py[hf]
                    o0 = tt_ * 256
                    if not last:
                        op("pe", lambda e, hf=hf, tt_=tt_, ppp=ppp, o0=o0, Tc=Tc: e.matmul(ppp.t[:, o0:o0 + 128], NCks[hf].t[:, tt_, 1, :], Tc[hf].t[:, tt_, 0, :], start=True, stop=True),
                           reads=[NCks[hf], Tc[hf]], writes=[ppp], inc=False)
                    op("pe", lambda e, hf=hf, tt_=tt_, ppp=ppp, o0=o0, Tc=Tc: e.matmul(ppp.t[:, o0 + 128:o0 + 256], NCks[hf].t[:, tt_, 0, :], Tc[hf].t[:, tt_, 1, :], start=True, stop=True),
                       reads=[NCks[hf], Tc[hf]], writes=[ppp])
                for hf in range(2):
                    src4 = py[hf].t[:, :].rearrange("p (a b c) -> p a b c", a=2, b=2)
                    if last:
                        src, dst = src4[:, :, 1, :], Ybs[hf].t[:, :, 1, :]
                    else:
                        src, dst = src4, Ybs[hf].t[:, :, :, :]
                    if hf == 0:
                        op("act", lambda e, src=src, dst=dst: e.activation(out=dst, in_=src, func=AF.Copy), reads=[py[hf]], writes=[Ybs[hf]])
                    else:
                        op("dve", lambda e, src=src, dst=dst: e.tensor_copy(out=dst, in_=src), reads=[py[hf]], writes=[Ybs[hf]])
                pz = [nb2(), nb2()]
                for t in range(TG):
                    hf, tt_ = t // 2, t % 2
                    pzz = pz[hf]
                    o0 = tt_ * 256
                    if not last:
                        op("pe", lambda e, hf=hf, tt_=tt_, pzz=pzz, o0=o0, Tc=Tc: e.matmul(pzz.t[:, o0:o0 + 128], Tc[hf].t[:, tt_, 1, :], Ybs[hf].t[:, tt_, 0, :], start=True, stop=True),
                           reads=[Tc[hf], Ybs[hf]], writes=[pzz], inc=False)
                    op("pe", lambda e, hf=hf, tt_=tt_, pzz=pzz, o0=o0, Tc=Tc: e.matmul(pzz.t[:, o0 + 128:o0 + 256], Tc[hf].t[:, tt_, 0, :], Ybs[hf].t[:, tt_, 1, :], start=True, stop=True),
                       reads=[Tc[hf], Ybs[hf]], writes=[pzz])
                Tn = Tbs[lev % 2]
                for hf in range(2):
                    src4 = pz[hf].t[:, :].rearrange("p (a b c) -> p a b c", a=2, b=2)
                    if last:
                        op("dve", lambda e, hf=hf, src4=src4, Tn=Tn, Tc=Tc: e.tensor_tensor(
                            out=Tn[hf].t[:, :, 1, :], in0=src4[:, :, 1, :], in1=Tc[hf].t[:, :, 1, :], op=ALU.add), reads=[pz[hf], Tc[hf]], writes=[Tn[hf]])
                    else:
                        op("dve", lambda e, hf=hf, src4=src4, Tn=Tn, Tc=Tc: e.tensor_tensor(
                            out=Tn[hf].t[:, :, :, :], in0=src4, in1=Tc[hf].t[:, :, :, :], op=ALU.add), reads=[pz[hf], Tc[hf]], writes=[Tn[hf]])
                Tc = Tn
            TT = Tc
            pw_ = nb2()
            for t in range(TG):
                op("pe", lambda e, t=t: e.matmul(pw_.t[:, t * 128:(t + 1) * 128], kbg.t[:, t, :], TT[t // 2].t[:, t % 2, 1, :], start=True, stop=True),
                   reads=[kbg, TT[t // 2]], writes=[pw_], inc=(t == TG - 1))
            op("act", lambda e: e.activation(out=nwT.t[:, :, :], in_=pw_.t[:, :].rearrange("p (a b) -> p a b", a=TG), func=AF.Copy, scale=-1.0),
               reads=[pw_], writes=[nwT])
            po = nb2()
            pvns = [nb2(), nb2()]
            for t in range(TG):
                ti = g * TG + t
                tc_ = slice(t * 128, (t + 1) * 128)
                vn = vnew[t % 2]
                pvn = pvns[t % 2]
                op("pe", lambda e, t=t, pvn=pvn: e.matmul(pvn.t[:, 0:128], TT[t // 2].t[:, t % 2, 1, :], vb.t[:, t, :], start=True, stop=False),
                   reads=[TT[t // 2], vb], writes=[pvn], inc=False)
                op("pe", lambda e, t=t, pvn=pvn: e.matmul(pvn.t[:, 0:128], nwT.t[:, t, :], Sbf.t[:, :], start=False, stop=True),
                   reads=[nwT, Sbf], writes=[pvn])
                op("act", lambda e, pvn=pvn, vn=vn: e.activation(out=vn.t[:, :], in_=pvn.t[:, 0:128], func=AF.Copy), reads=[pvn], writes=[vn])
                op("pe", lambda e, tc_=tc_: e.matmul(po.t[:, tc_], Sbf.t[:, :], qgT.t[:, tc_], start=True, stop=False),
                   reads=[Sbf, qgT], writes=[po], inc=False)
                op("pe", lambda e, t=t, tc_=tc_, vn=vn: e.matmul(po.t[:, tc_], vn.t[:, :], X2.t[:, t, 1, :], start=False, stop=True),
                   reads=[vn, X2], writes=[po], inc=False)
                op("pe", lambda e, t=t, pvn=pvn, vn=vn: e.matmul(pvn.t[:, 128:256], kd.t[:, t, :], vn.t[:, :], start=True, stop=True),
                   reads=[kd, vn], writes=[pvn])
                op("dve", lambda e, pvn=pvn, ti=ti: e.scalar_tensor_tensor(out=Sbf.t[:, :], in0=S32.t[:, :], scalar=dl.t[:, ti, 8 + h:9 + h],
                                                                           in1=pvn.t[:, 128:256], op0=ALU.mult, op1=ALU.add),
                   reads=[S32, dl, pvn], writes=[Sbf])
                op("dve", lambda e, pvn=pvn, ti=ti: e.scalar_tensor_tensor(out=S32.t[:, :], in0=S32.t[:, :], scalar=dl.t[:, ti, 8 + h:9 + h],
                                                                           in1=pvn.t[:, 128:256], op0=ALU.mult, op1=ALU.add),
                   reads=[S32, dl, pvn], writes=[S32])
            op("act", lambda e: e.activation(out=o32.t[:, :], in_=po.t[:, :], func=AF.Copy), reads=[po], writes=[o32])
            op("act", lambda e: e.activation(out=og2.t[:, :], in_=o32.t[:, :], func=AF.Square), reads=[o32], writes=[og2])
            pb = nb2()
            op("pe", lambda e, pb=pb: e.matmul(pb.t[:, :], onesb.t[:, :], og2.t[:, :], start=True, stop=True), reads=[og2, onesb], writes=[pb])
            op("act", lambda e, pb=pb: e.activation(out=rn[1].t[:, :], in_=pb.t[:, :], func=AF.Ln, scale=1.0 / 128, bias=EPS), reads=[pb], writes=[rn[1]])
            op("act", lambda e: e.activation(out=rn[1].t[:, :], in_=rn[1].t[:, :], func=AF.Exp, scale=-0.5), reads=[rn[1]], writes=[rn[1]])
            op("dve", lambda e: e.tensor_tensor(out=og.t[:, :], in0=o32.t[:, :], in1=rn[1].t[:, :], op=ALU.mult), reads=[o32, rn[1]], writes=[og])
            op("dve", lambda e: e.scalar_tensor_tensor(out=og2.t[:, :], in0=og.t[:, :], scalar=P("onorm", li), in1=zs.t[:, :],
                                                       op0=ALU.mult, op1=ALU.mult), reads=[og, prm, zs], writes=[og2])
            for oc in range(8):
                pb = nb2()
                op("pe", lambda e, oc=oc, pb=pb: e.matmul(pb.t[:, :], wo.t[:, oc * 128:(oc + 1) * 128], og2.t[:, :], start=True, stop=True),
                   reads=[wo, og2], writes=[pb])
                resid_add(pb, l, 16, oc, sl, cols)


        wcur = [None]
        k.replay(k.record(lambda: stage1(0)))
        for u in range(U):
            ra = k.record(lambda: stage2(u))
            rb = k.record(lambda: stage1(u + 1)) if u + 1 < U else []
            k.replay(k.interleave(ra, rb) if INTERLEAVE else (ra + rb))


def moe_layer(k, nc, env):
    op, dma = k.op, k.dma
    xT, hnT, modT, prm, cst = env["xT"], env["hnT"], env["modT"], env["prm"], env["cst"]
    nb, nw, resid_add, P, make_hn = env["nb"], env["nw"], env["resid_add"], env["P"], env["make_hn"]
    l, sl, S, NG, NT = env["l"], env["sl"], env["S"], env["NG"], env["NT"]
    moer_d, moeup_d, moedn_d = env["moer_d"], env["moeup_d"], env["moedn_d"]
    ones, ident = env["ones"], env["ident"]
    wpool = env["wpool"]
    TG = GS // 128
    with ExitStack() as pes:
        GT = k.sb([32, S], F32, es=pes, name="GT")
        with ExitStack() as res:
            def sbt(shape, dt, name):
                return k.sb(shape, dt, es=res, name=name)
            env["alloc_scr"](res)
            h32 = sbt([128, KD, GS], F32, "hn32")
            wr = sbt([128, KD, 36], F32, "wr")
            lg = sbt([128, 36], F32, "lg")
            sm = {n: sbt([128, 36], F32, "sm_" + n) for n in ("mx", "oh", "pen", "ml", "m1", "k1", "ml2", "m2", "k2", "ex", "sm", "gp", "r", "den", "w1", "w2", "G", "nmx")}
            dma("sp", wr.t[:, :, :], moer_d[l].rearrange("(k p) n -> p k n", p=128), writes=[wr])

            def dv(fn, reads, writes):
                op("dve", fn, reads=reads, writes=writes)

            def route(g):
                for t in range(TG):
                    ti = g * TG + t
                    pb = nb()
                    for kk in range(KD):
                        op("pe", lambda e, kk=kk, t=t, pb=pb: e.matmul(pb.t[:, 0:36], h32.t[:, kk, t * 128:(t + 1) * 128], wr.t[:, kk, :],
                                                                     start=(kk == 0), stop=(kk == KD - 1)),
                           reads=[h32, wr], writes=[pb], inc=(kk == KD - 1))
                    dv(lambda e, pb=pb: e.tensor_tensor(out=lg.t[:, :], in0=pb.t[:, 0:36], in1=P("rbias", l * 36, 36), op=ALU.add), [pb, prm], [lg])
                    dv(lambda e: e.tensor_reduce(out=sm["mx"].t[:, 0:1], in_=lg.t[:, 0:4], axis=mybir.AxisListType.X, op=ALU.max), [lg], [sm["mx"]])
                    dv(lambda e: e.tensor_scalar(out=sm["oh"].t[:, 0:4], in0=lg.t[:, 0:4], scalar1=sm["mx"].t[:, 0:1], scalar2=None, op0=ALU.is_ge), [lg, sm["mx"]], [sm["oh"]])
                    dv(lambda e: e.tensor_scalar_mul(out=sm["nmx"].t[:, 0:1], in0=sm["mx"].t[:, 0:1], scalar1=-1.0), [sm["mx"]], [sm["nmx"]])
                    op("act", lambda e: e.activation(out=sm["ex"].t[:, 0:4], in_=lg.t[:, 0:4], func=AF.Exp, bias=sm["nmx"].t[:, 0:1]), reads=[lg, sm["nmx"]], writes=[sm["ex"]])
                    dv(lambda e: e.tensor_reduce(out=sm["sm"].t[:, 0:1], in_=sm["ex"].t[:, 0:4], axis=mybir.AxisListType.X, op=ALU.add), [sm["ex"]], [sm["sm"]])
                    dv(lambda e: e.reciprocal(out=sm["gp"].t[:, 0:1], in_=sm["sm"].t[:, 0:1]), [sm["sm"]], [sm["gp"]])
                    dv(lambda e: e.tensor_scalar(out=sm["pen"].t[:, 0:4], in0=sm["oh"].t[:, 0:4], scalar1=-1.0, scalar2=1.0e4, op0=ALU.add, op1=ALU.mult), [sm["oh"]], [sm["pen"]])
                    dv(lambda e: e.tensor_tensor(out=sm["ml"].t[:, 0:32].rearrange("p (a b) -> p a b", a=4), in0=lg.t[:, 4:36].rearrange("p (a b) -> p a b", a=4),
                                                 in1=sm["pen"].t[:, 0:4].unsqueeze(2).to_broadcast([128, 4, 8]), op=ALU.add), [lg, sm["pen"]], [sm["ml"]])
                    dv(lambda e: e.tensor_reduce(out=sm["m1"].t[:, 0:1], in_=sm["ml"].t[:, 0:32], axis=mybir.AxisListType.X, op=ALU.max), [sm["ml"]], [sm["m1"]])
                    dv(lambda e: e.tensor_scalar(out=sm["k1"].t[:, 0:32], in0=sm["ml"].t[:, 0:32], scalar1=sm["m1"].t[:, 0:1], scalar2=None, op0=ALU.is_ge), [sm["ml"], sm["m1"]], [sm["k1"]])
                    dv(lambda e: e.scalar_tensor_tensor(out=sm["ml2"].t[:, 0:32], in0=sm["k1"].t[:, 0:32], scalar=-1.0e4, in1=sm["ml"].t[:, 0:32], op0=ALU.mult, op1=ALU.add),
                       [sm["k1"], sm["ml"]], [sm["ml2"]])
                    dv(lambda e: e.tensor_reduce(out=sm["m2"].t[:, 0:1], in_=sm["ml2"].t[:, 0:32], axis=mybir.AxisListType.X, op=ALU.max), [sm["ml2"]], [sm["m2"]])
                    dv(lambda e: e.tensor_scalar(out=sm["k2"].t[:, 0:32], in0=sm["ml2"].t[:, 0:32], scalar1=sm["m2"].t[:, 0:1], scalar2=None, op0=ALU.is_ge), [sm["ml2"], sm["m2"]], [sm["k2"]])
                    dv(lambda e: e.tensor_tensor(out=sm["r"].t[:, 0:1], in0=sm["m2"].t[:, 0:1], in1=sm["m1"].t[:, 0:1], op=ALU.subtract), [sm["m2"], sm["m1"]], [sm["r"]])
                    op("act", lambda e: e.activation(out=sm["r"].t[:, 0:1], in_=sm["r"].t[:, 0:1], func=AF.Exp), reads=[sm["r"]], writes=[sm["r"]])
                    dv(lambda e: e.tensor_scalar_add(out=sm["den"].t[:, 0:1], in0=sm["r"].t[:, 0:1], scalar1=1.0), [sm["r"]], [sm["den"]])
                    dv(lambda e: e.reciprocal(out=sm["den"].t[:, 0:1], in_=sm["den"].t[:, 0:1]), [sm["den"]], [sm["den"]])
                    dv(lambda e: e.tensor_tensor(out=sm["w1"].t[:, 0:1], in0=sm["gp"].t[:, 0:1], in1=sm["den"].t[:, 0:1], op=ALU.mult), [sm["gp"], sm["den"]], [sm["w1"]])
                    dv(lambda e: e.tensor_tensor(out=sm["w2"].t[:, 0:1], in0=sm["w1"].t[:, 0:1], in1=sm["r"].t[:, 0:1], op=ALU.mult), [sm["w1"], sm["r"]], [sm["w2"]])
                    dv(lambda e: e.tensor_scalar_mul(out=sm["G"].t[:, 0:32], in0=sm["k1"].t[:, 0:32], scalar1=sm["w1"].t[:, 0:1]), [sm["k1"], sm["w1"]], [sm["G"]])
                    dv(lambda e: e.scalar_tensor_tensor(out=sm["G"].t[:, 0:32], in0=sm["k2"].t[:, 0:32], scalar=sm["w2"].t[:, 0:1], in1=sm["G"].t[:, 0:32], op0=ALU.mult, op1=ALU.add),
                       [sm["k2"], sm["w2"], sm["G"]], [sm["G"]])
                    pt = nb()
                    op("pe", lambda e, pt=pt: e.transpose(pt.t[0:32, 0:128], sm["G"].t[:, 0:32], ident), reads=[sm["G"], cst], writes=[pt])
                    op("act", lambda e, pt=pt, ti=ti: e.activation(out=GT.t[:, ti * 128:(ti + 1) * 128], in_=pt.t[0:32, 0:128], func=AF.Copy), reads=[pt], writes=[GT])
            env["hn_pipeline"](l, 1, sl, hn32=h32, after=route)
            k.barrier()

        def sbt(shape, dt, name):
            return k.sb(shape, dt, es=pes, name=name)
        selall = sbt([32, NEXP // 2, 128], F32, "selall")
        wup = list(wpool) + [sbt([128, KD, 512], BF16, "wupx%d" % i) for i in range(2)]
        wdn = [sbt([128, 2, D], BF16, "wdn%d" % i) for i in range(4)]
        gsb = [sbt([128, GS], F32, "gsb%d" % i) for i in range(2)]
        sgb = [sbt([128, GS], F32, "sgb%d" % i) for i in range(2)]
        tb = sgb
        hb = [[sbt([128, 2, GS], BF16, "hb%d_%d" % (i, j)) for j in range(2)] for i in range(2)]
        def build_sel(half):
            for ex_ in range(NEXP // 2):
                exg = half * (NEXP // 2) + ex_
                op("dve", lambda e, ex_=ex_, exg=exg: e.tensor_copy(out=selall.t[:, ex_, :], in_=ident[0:32, exg:exg + 1].to_broadcast([32, 128])),
                   reads=[cst], writes=[selall])

        build_sel(0)
        def fetch_pair(ep_):
            for ei_ in range(2):
                ex_ = 2 * ep_ + ei_
                wu_ = wup[ex_ % 4]
                dma("pool", wu_.t[:, :, :], moeup_d[l, ex_].rearrange("(k p) n -> p k n", p=128), writes=[wu_])
                wd_ = wdn[ex_ % 4]
                dma("pool", wd_.t[:, :, :], moedn_d[l, ex_].rearrange("(k p) n -> p k n", p=128), writes=[wd_])

        fetch_pair(0)
        for ep in range(NEXP // 2):
            if ep + 1 < NEXP // 2:
                fetch_pair(ep + 1)
            if ep == NEXP // 4:
                build_sel(1)
            wus = [wup[(2 * ep + ei) % 4] for ei in range(2)]
            wds = [wdn[(2 * ep + ei) % 4] for ei in range(2)]
            def phase1(g):
                cols = slice(g * GS, (g + 1) * GS)
                for ei in range(2):
                    ex = 2 * ep + ei
                    wu = wus[ei]
                    pg_ = nb()
                    op("pe", lambda e, ex=ex, pg_=pg_: e.matmul(pg_.t[:, :], selall.t[:, ex % (NEXP // 2), :], GT.t[:, cols], start=True, stop=True),
                       reads=[selall, GT], writes=[pg_])
                    gs_ = gsb[ei]
                    op("dve", lambda e, pg_=pg_, gs_=gs_: e.tensor_copy(out=gs_.t[:, :], in_=pg_.t[:, :]), reads=[pg_], writes=[gs_])
                    hh = hb[ei][g % 2]
                    for j in range(2):
                        pgt = nb()
                        put = nb()
                        for kk in range(KD):
                            op("pe", lambda e, kk=kk, j=j, pgt=pgt, wu=wu: e.matmul(pgt.t[:, :], wu.t[:, kk, j * 128:(j + 1) * 128], hnT.t[:, kk, cols],
                                                                                 start=(kk == 0), stop=(kk == KD - 1)), reads=[wu, hnT], writes=[pgt], inc=(kk == KD - 1))
                        for kk in range(KD):
                            op("pe", lambda e, kk=kk, j=j, put=put, wu=wu: e.matmul(put.t[:, :], wu.t[:, kk, FE + j * 128:FE + (j + 1) * 128], hnT.t[:, kk, cols],
                                                                                 start=(kk == 0), stop=(kk == KD - 1)), reads=[wu, hnT], writes=[put], inc=(kk == KD - 1))
                        sg = sgb[j]
                        tt = tb[j]
                        op("act", lambda e, pgt=pgt, sg=sg: e.activation(out=sg.t[:, :], in_=pgt.t[:, :], func=AF.Silu), reads=[pgt], writes=[sg])
                        op("dve", lambda e, put=put, sg=sg, tt=tt: e.tensor_tensor(out=tt.t[:, :], in0=put.t[:, :], in1=sg.t[:, :], op=ALU.mult),
                           reads=[put, sg], writes=[tt])
                        op("pool", lambda e, j=j, tt=tt, hh=hh, gs_=gs_: e.tensor_tensor(out=hh.t[:, j, :], in0=tt.t[:, :], in1=gs_.t[:, :], op=ALU.mult),
                           reads=[tt, gs_], writes=[hh])
            def phase2(g):
                cols = slice(g * GS, (g + 1) * GS)
                for oc in range(8):
                    py = nb()
                    n = 0
                    for ei in range(2):
                        hh = hb[ei][g % 2]
                        wd = wds[ei]
                        for j in range(2):
                            op("pe", lambda e, j=j, oc=oc, py=py, wd=wd, hh=hh, n=n: e.matmul(py.t[:, :], wd.t[:, j, oc * 128:(oc + 1) * 128], hh.t[:, j, :],
                                                                                          start=(n == 0), stop=(n == 3)), reads=[wd, hh], writes=[py], inc=(n == 3))
                            n += 1
                    resid_add(py, l, 40, oc, sl, cols)

            for g in range(NG):
                phase1(g)
                if g > 0:
                    phase2(g - 1)
            phase2(NG - 1)


_CACHE = {}


def prepare_inputs(inputs, cfg):
    NSEQ, S, L, NCO = cfg["NSEQ"], cfg["S"], cfg["DEPTH"], cfg["NCORES"]
    f = lambda a: np.ascontiguousarray(np.asarray(a, np.float32))
    prm, _ = _build_prm(inputs, L)
    cst = _build_cst()
    dnin = f(inputs["dn_w_in"])
    qkvz = dnin[:, :, :4096].reshape(2, D, 4, 8, 128).transpose(0, 1, 3, 2, 4).reshape(2, D, 4096)
    shared = {
        "cst": cst, "prm": prm, "mk": _build_mk(),
        "mod_w": f(inputs["mod_w"])[:L], "ab_w_in": f(inputs["ab_w_in"]), "pool_w": f(inputs["pool_w"]),
        "ab_w_out": f(inputs["ab_w_out"]), "dn_w_in_h": np.ascontiguousarray(qkvz),
        "dn_w_bg": np.ascontiguousarray(dnin[:, :, 4096:4112]), "dn_w_out": f(inputs["dn_w_out"]),
        "moe_w_r": np.ascontiguousarray(np.concatenate([f(inputs["moe_w_grp"])[:L], f(inputs["moe_w_exp"])[:L]], axis=2)),
        "moe_w_up": f(inputs["moe_w_up"])[:L], "moe_w_down": f(inputs["moe_w_down"])[:L],
    }
    x = f(inputs["x"])
    c = f(inputs["c"])
    maps = []
    for core in range(NCO):
        xs = x[core * NSEQ:(core + 1) * NSEQ].reshape(NSEQ * S, D)
        cs = c[core * NSEQ:(core + 1) * NSEQ]
        cT = cs.reshape(NSEQ, 8, 128).transpose(2, 1, 0).reshape(128, 8 * NSEQ)
        m = dict(shared)
        m["x"] = np.ascontiguousarray(xs)
        m["cT"] = np.ascontiguousarray(cT)
        maps.append(m)
    return maps


def kernel(**inputs):
    cfg = CFG
    key = tuple(sorted(cfg.items()))
    if key not in _CACHE:
        _CACHE[key] = build_program(cfg)[0]
    nc = _CACHE[key]
    maps = prepare_inputs(inputs, cfg)
    res = run_bass_kernel_spmd(nc, maps, core_ids=list(range(cfg["NCORES"])))
    outs = [np.asarray(r["out"], np.float32).reshape(cfg["NSEQ"], cfg["S"], D) for r in res.results]
    return np.concatenate(outs, axis=0)
```

```python
import numpy as np
from contextlib import ExitStack
import concourse.bass as bass
import concourse.mybir as mybir
from concourse.bass_utils import run_bass_kernel_spmd

F32 = mybir.dt.float32
BF16 = mybir.dt.bfloat16
AF = mybir.ActivationFunctionType
ALU = mybir.AluOpType

D = 1024
KD = 8
EPS = 1e-6
NEXP = 32
FE = 256
NDSEM = 8
GS = 512
NEG = -30000.0
INTERLEAVE = True

CFG = dict(NSEQ=4, S=2048, DEPTH=4, NCORES=8)


class Buf:
    __slots__ = ("t", "w", "r", "name")

    def __init__(self, t, name):
        self.t = t
        self.w = {}
        self.r = {}
        self.name = name

    def __getitem__(self, k):
        return self.t[k]


class _Proxy:
    def __init__(self):
        self.call = None

    def __getattr__(self, name):
        def f(*a, **kw):
            self.call = (name, a, kw)
            return None
        return f


class K:
    def __init__(self, nc, es):
        self.nc = nc
        self.es = es
        self.E = {"pe": nc.tensor, "act": nc.scalar, "dve": nc.vector, "pool": nc.gpsimd, "sp": nc.sync}
        self.sem = {e: es.enter_context(nc.semaphore("s_" + e)) for e in self.E}
        self.cnt = {e: 0 for e in self.E}
        self.waited = {e: {} for e in self.E}
        self.dq = {}
        for q in ("sp", "pool"):
            self.dq[q] = [[es.enter_context(nc.semaphore("d_%s%d" % (q, i))), 0, "d_%s%d" % (q, i)] for i in range(NDSEM)]
        self.dqi = {"sp": 0, "pool": 0}
        self.uid = 0
        self.nins = 0
        self.rec = None

    def sb(self, shape, dt, es=None, name=None):
        self.uid += 1
        name = (name or "t") + "_%d" % self.uid
        t = (es or self.es).enter_context(self.nc.sbuf_tensor(name, list(shape), dt))
        return Buf(t, name)

    def psb(self, shape, dt, name):
        t = self.es.enter_context(self.nc.psum_tensor(name, list(shape), dt))
        return Buf(t, name)

    def _wait(self, eng, tok):
        key, sem, val, peng = tok
        if peng == "pe" and eng == "pe":
            return
        if self.waited[eng].get(key, 0) >= val:
            return
        self.E[eng].wait_ge(sem, val)
        self.nins += 1
        self.waited[eng][key] = val

    def _deps(self, eng, reads, writes):
        for b in reads:
            for tok in b.w.values():
                self._wait(eng, tok)
        for b in writes:
            for tok in b.w.values():
                self._wait(eng, tok)
            for tok in b.r.values():
                self._wait(eng, tok)

    def _mark(self, tok, reads, writes):
        for b in reads:
            b.r[tok[0]] = tok
        for b in writes:
            b.w = {tok[0]: tok}
            b.r = {}

    def op(self, eng, fn, reads=(), writes=(), inc=True):
        if self.rec is not None:
            pr = _Proxy()
            fn(pr)
            self.rec.append(("op", eng, pr.call, tuple(reads), tuple(writes)))
            return
        self._deps(eng, reads, writes)
        ins = fn(self.E[eng])
        self.nins += 1
        if inc:
            self.cnt[eng] += 1
            ins.then_inc(self.sem[eng], 1)
            tok = (eng, self.sem[eng], self.cnt[eng], eng)
        else:
            tok = (eng, self.sem[eng], self.cnt[eng] + 1, eng)
        self._mark(tok, reads, writes)

    def dma(self, q, out, in_, reads=(), writes=()):
        if self.rec is not None:
            self.rec.append(("dma", q, (out, in_), tuple(reads), tuple(writes)))
            return
        self._deps(q, reads, writes)
        slot = self.dq[q][self.dqi[q] % NDSEM]
        self.dqi[q] += 1
        if slot[1] > 0:
            self._wait(q, (slot[2], slot[0], slot[1], "dma"))
        ins = self.E[q].dma_start(out=out, in_=in_)
        self.nins += 1
        slot[1] += 16
        ins.then_inc(slot[0], 16)
        tok = (slot[2], slot[0], slot[1], "dma")
        self._mark(tok, reads, writes)

    def record(self, fn):
        self.rec = []
        fn()
        r, self.rec = self.rec, None
        return r

    def replay(self, items):
        for it in items:
            if it[0] == "op":
                _, eng, call, reads, writes = it
                self.op(eng, lambda e, c=call: getattr(e, c[0])(*c[1], **c[2]), reads=reads, writes=writes)
            else:
                _, q, (out, in_), reads, writes = it
                self.dma(q, out, in_, reads=reads, writes=writes)

    @staticmethod
    def interleave(a, b):
        out = []
        i = j = 0
        while i < len(a) or j < len(b):
            if j >= len(b) or (i < len(a) and i * len(b) <= j * len(a)):
                out.append(a[i])
                i += 1
            else:
                out.append(b[j])
                j += 1
        return out

    def barrier(self, engines=None):
        engs = list(self.E) if engines is None else engines
        for e in engs:
            for f in self.E:
                if self.cnt[f] > 0:
                    self._wait(e, (f, self.sem[f], self.cnt[f], "x"))
            for q in self.dq:
                for slot in self.dq[q]:
                    if slot[1] > 0:
                        self._wait(e, (slot[2], slot[0], slot[1], "dma"))


def _prm_layout(L):
    off = {}
    n = 0

    def add(name, cnt):
        nonlocal n
        off[name] = n
        n += cnt

    add("nm", L * 8)
    add("nf", L * 8)
    add("fn", 8)
    add("modb", L * 48)
    add("pscale", 2 * 4)
    add("convw", 2 * 4 * 31)
    add("convb", 2 * 4)
    add("lng", 2 * 4)
    add("lnb", 2 * 4)
    add("dnconv", 2 * 24 * 4)
    add("onorm", 2)
    add("alog", 2)
    add("dtb", 2)
    add("mlo", 1)
    add("mhi", 1)
    add("rbias", L * 36)
    return off, n


def _fm(v):
    v = np.asarray(v, np.float32)
    lead = v.shape[:-1]
    n = v.shape[-1] // 128
    a = v.reshape(lead + (n, 128))
    a = np.moveaxis(a, -1, 0)
    return np.ascontiguousarray(a.reshape(128, -1))


def _build_prm(inp, L):
    off, n = _prm_layout(L)
    P = np.zeros((128, n), np.float32)

    def put(name, arr):
        arr = np.asarray(arr, np.float32)
        P[:arr.shape[0], off[name]:off[name] + arr.shape[1]] = arr

    put("nm", _fm(inp["norm_mix"][:L]))
    put("nf", _fm(inp["norm_ffn"][:L]))
    put("fn", _fm(inp["final_norm"]))
    put("modb", _fm(inp["mod_b"][:L]))
    put("pscale", _fm(inp["pool_scale"]))
    cw = np.asarray(inp["conv_w"], np.float32)
    cw = cw.reshape(2, 31, 4, 128).transpose(3, 0, 2, 1)
    put("convw", cw.reshape(128, -1))
    put("convb", _fm(inp["conv_b"]))
    put("lng", _fm(inp["conv_ln_g"]))
    put("lnb", _fm(inp["conv_ln_b"]))
    dcw = np.asarray(inp["dn_conv_w"], np.float32)
    dcw = dcw.reshape(2, 4, 24, 128).transpose(3, 0, 2, 1)
    put("dnconv", dcw.reshape(128, -1))
    put("onorm", np.asarray(inp["dn_onorm"], np.float32).T)
    al = np.zeros((16, 2), np.float32)
    al[8:16, :] = np.asarray(inp["dn_a_log"], np.float32).T
    put("alog", al)
    db = np.zeros((16, 2), np.float32)
    db[8:16, :] = np.asarray(inp["dn_dt_bias"], np.float32).T
    put("dtb", db)
    mlo = np.zeros((16, 1), np.float32)
    mlo[0:8] = 1.0
    put("mlo", mlo)
    put("mhi", 1.0 - mlo)
    rb = np.concatenate([np.asarray(inp["moe_b_grp"], np.float32)[:L], np.asarray(inp["moe_b_exp"], np.float32)[:L]], axis=1)
    put("rbias", np.broadcast_to(rb.reshape(1, -1), (128, L * 36)))
    return P, off


def _build_cst():
    c = np.zeros((128, 640), np.float32)
    c[:, 0:128] = np.eye(128, dtype=np.float32)
    c[:, 128:256] = 1.0
    c[127, 256:384] = 1.0
    m = np.arange(128)[:, None]
    cc = np.arange(128)[None, :]
    c[:, 384:512] = np.where(cc > m, 0.0, NEG)
    c[:, 512:640] = np.where(cc >= m, 0.0, NEG)
    return c


def _build_mk():
    mk = np.zeros((128, 7, 2, 128), np.float32)
    r = np.arange(128)[:, None]
    c = np.arange(128)[None, :]
    for k_ in range(7):
        b = 1 << k_
        same = (r // (2 * b)) == (c // (2 * b))
        mk[:, k_, 0, :] = same & ((r % (2 * b)) >= b) & ((c % (2 * b)) < b)
        mk[:, k_, 1, :] = same & ((c % (2 * b)) >= b) & ((r % (2 * b)) < b)
    return np.ascontiguousarray(mk.reshape(128, -1))


def build_program(cfg):
    NSEQ, S, L = cfg["NSEQ"], cfg["S"], cfg["DEPTH"]
    NG = S // GS
    NT = S // 128
    off, NP = _prm_layout(L)
    nc = bass.Bass("TRN2", target_bir_lowering=False)

    def din(name, shape):
        return nc.dram_tensor(name, list(shape), F32, kind="ExternalInput").ap()

    x_d = din("x", [NSEQ * S, D])
    cT_d = din("cT", [128, 8 * NSEQ])
    cst_d = din("cst", [128, 640])
    prm_d = din("prm", [128, NP])
    mk_d = din("mk", [128, 7 * 2 * 128])
    modw_d = din("mod_w", [L, D, 6 * D])
    abin_d = din("ab_w_in", [2, D, 1536])
    poolw_d = din("pool_w", [2, 4, 128, 128])
    about_d = din("ab_w_out", [2, D, D])
    dnin_d = din("dn_w_in_h", [2, D, 4096])
    dnbg_d = din("dn_w_bg", [2, D, 16])
    dnout_d = din("dn_w_out", [2, D, D])
    moer_d = din("moe_w_r", [L, D, 36])
    moeup_d = din("moe_w_up", [L, NEXP, D, 2 * FE])
    moedn_d = din("moe_w_down", [L, NEXP, FE, D])
    out_d = nc.dram_tensor("out", [NSEQ * S, D], F32, kind="ExternalOutput").ap()

    es = ExitStack()
    k = K(nc, es)
    op, dma = k.op, k.dma

    xT = k.sb([128, KD, S], F32, name="xT")
    hnT = k.sb([128, KD, S], BF16, name="hnT")
    cst = k.sb([128, 640], F32, name="cst")
    identb = k.sb([128, 128], BF16, name="identb")
    onesb = k.sb([128, 128], BF16, name="onesb")
    prm = k.sb([128, NP], F32, name="prm")
    cact = k.sb([128, 8 * NSEQ], F32, name="cact")
    modT = k.sb([128, L, 48, NSEQ], F32, name="modT")
    modA = k.sb([128, L, 2, 8, NSEQ], F32, name="modA")
    wpool = [k.sb([128, KD, 512], BF16, name="wp%d" % i) for i in range(2)]
    wpi = [0]
    banks = [k.psb([128, 512], F32, "bank%d" % i) for i in range(7)]
    bankb = k.psb([128, 1024], BF16, "bankb")
    bi = [0]

    def nb():
        b = banks[bi[0] % 7]
        bi[0] += 1
        return b

    def nw():
        w = wpool[wpi[0] % 2]
        wpi[0] += 1
        return w

    ident = cst.t[:, 0:128]
    ones = cst.t[:, 128:256]
    sel127 = cst.t[:, 256:384]
    negm2 = cst.t[:, 384:640]

    def P(name, i=0, n=1, rows=128):
        o = off[name] + i
        return prm.t[0:rows, o:o + n]

    dma("sp", cst.t[:, :], cst_d[:, :], writes=[cst])
    dma("sp", prm.t[:, :], prm_d[:, :], writes=[prm])
    dma("sp", cact.t[:, :], cT_d[:, :], writes=[cact])
    dma("pool", identb.t[:, :], cst_d[:, 0:128], writes=[identb])
    dma("pool", onesb.t[:, :], cst_d[:, 128:256], writes=[onesb])
    op("act", lambda e: e.activation(out=cact.t[:, :], in_=cact.t[:, :], func=AF.Silu), reads=[cact], writes=[cact])

    with ExitStack() as pes:
        mw = [k.sb([128, KD, 512], F32, es=pes, name="mw%d" % i) for i in range(2)]
        mi = 0
        for l in range(L):
            for pc in range(12):
                w = mw[mi % 2]
                mi += 1
                dma("sp", w.t[:, :, :], modw_d[l, :, pc * 512:(pc + 1) * 512].rearrange("(k p) n -> p k n", p=128), writes=[w])
                pb = nb()
                for c4 in range(4):
                    for kk in range(KD):
                        op("pe", lambda e, c4=c4, kk=kk, w=w, pb=pb: e.matmul(
                            pb.t[:, c4 * NSEQ:(c4 + 1) * NSEQ], w.t[:, kk, c4 * 128:(c4 + 1) * 128],
                            cact.t[:, kk * NSEQ:(kk + 1) * NSEQ], start=(kk == 0), stop=(kk == KD - 1)),
                           reads=[w, cact], writes=[pb], inc=(kk == KD - 1))
                for c4 in range(4):
                    ch = pc * 4 + c4
                    op("dve", lambda e, c4=c4, ch=ch, pb=pb, l=l: e.tensor_scalar(
                        out=modT.t[:, l, ch, :], in0=pb.t[:, c4 * NSEQ:(c4 + 1) * NSEQ],
                        scalar1=P("modb", l * 48 + ch), scalar2=None, op0=ALU.add),
                       reads=[pb, prm], writes=[modT])
            for j, (nname, cbase) in enumerate((("nm", 8), ("nf", 32))):
                for kk in range(KD):
                    op("dve", lambda e, j=j, kk=kk, nname=nname, cbase=cbase, l=l: e.tensor_scalar(
                        out=modA.t[:, l, j, kk, :], in0=modT.t[:, l, cbase + kk, :],
                        scalar1=1.0, scalar2=P(nname, l * 8 + kk), op0=ALU.add, op1=ALU.mult),
                       reads=[modT, prm], writes=[modA])
    k.barrier()

    def rms_rstd(pes, g, scale_n):
        cols = slice(g * GS, (g + 1) * GS)
        pb = nb()
        for kk in range(KD):
            sq = scr["sqb"][kk % 2]
            op("act", lambda e, kk=kk, sq=sq: e.activation(out=sq.t[:, :], in_=xT.t[:, kk, cols], func=AF.Square),
               reads=[xT], writes=[sq])
            op("pe", lambda e, kk=kk, sq=sq, pb=pb: e.matmul(pb.t[:, :], ones, sq.t[:, :], start=(kk == 0), stop=(kk == KD - 1)),
               reads=[sq, cst], writes=[pb])
        rs = rsb[g % 2]
        op("act", lambda e: e.activation(out=rs.t[:, :], in_=pb.t[:, :], func=AF.Ln, scale=1.0 / scale_n, bias=EPS), reads=[pb], writes=[rs])
        op("act", lambda e: e.activation(out=rs.t[:, :], in_=rs.t[:, :], func=AF.Exp, scale=-0.5), reads=[rs], writes=[rs])
        return rs

    def make_hn(l, which, sl, g, hn32=None):
        cols = slice(g * GS, (g + 1) * GS)
        rs = rms_rstd(None, g, float(D))
        shbase = 0 if which == 0 else 24
        for kk in range(KD):
            tmp = scr["tmpb"][kk % 2]
            op("dve", lambda e, kk=kk, tmp=tmp: e.tensor_tensor(out=tmp.t[:, :], in0=xT.t[:, kk, cols], in1=rs.t[:, :], op=ALU.mult),
               reads=[xT, rs], writes=[tmp])
            op("act", lambda e, kk=kk, tmp=tmp: e.activation(
                out=hnT.t[:, kk, cols], in_=tmp.t[:, :], func=AF.Identity,
                bias=modT.t[:, l, shbase + kk, sl:sl + 1], scale=modA.t[:, l, which, kk, sl:sl + 1]),
               reads=[tmp, modT, modA], writes=[hnT])
            if hn32 is not None:
                op("act", lambda e, kk=kk, tmp=tmp: e.activation(
                    out=hn32.t[:, kk, :], in_=tmp.t[:, :], func=AF.Identity,
                    bias=modT.t[:, l, shbase + kk, sl:sl + 1], scale=modA.t[:, l, which, kk, sl:sl + 1]),
                   reads=[tmp, modT, modA], writes=[hn32])

    def load_w(src_ap, ncols=512):
        w = nw()
        dma("pool", w.t[:, :, 0:ncols], src_ap.rearrange("(k p) n -> p k n", p=128), writes=[w])
        return w

    def proj(w, wc0, g, pb, pcols=None):
        cols = slice(g * GS, (g + 1) * GS)
        for kk in range(KD):
            op("pe", lambda e, kk=kk: e.matmul(pb.t[:, :], w.t[:, kk, wc0:wc0 + 128], hnT.t[:, kk, cols],
                                               start=(kk == 0), stop=(kk == KD - 1)),
               reads=[w, hnT], writes=[pb], inc=(kk == KD - 1))

    def resid_add(pb, l, gate_chunk_base, kk, sl, cols):
        op("dve", lambda e: e.scalar_tensor_tensor(
            out=xT.t[:, kk, cols], in0=pb.t[:, :], scalar=modT.t[:, l, gate_chunk_base + kk, sl:sl + 1],
            in1=xT.t[:, kk, cols], op0=ALU.mult, op1=ALU.add), reads=[pb, modT, xT], writes=[xT])

    rsb = [k.sb([128, GS], F32, name="rs%d" % i) for i in range(2)]
    scr = {}

    def alloc_scr(es_):
        scr["sqb"] = [k.sb([128, GS], F32, es=es_, name="sq%d" % i) for i in range(2)]
        scr["tmpb"] = [k.sb([128, GS], F32, es=es_, name="tmp%d" % i) for i in range(2)]

    for sl in range(NSEQ):
        with ExitStack() as pes:
            xin = [k.sb([128, D], F32, es=pes, name="xin%d" % i) for i in range(2)]
            for i in range(NT):
                xi = xin[i % 2]
                dma("sp", xi.t[:, :], x_d[sl * S + i * 128: sl * S + (i + 1) * 128, :], writes=[xi])
                for half in range(2):
                    pb = nb()
                    for c4 in range(4):
                        kk = half * 4 + c4
                        op("pe", lambda e, kk=kk, c4=c4, pb=pb, xi=xi: e.transpose(
                            pb.t[:, c4 * 128:(c4 + 1) * 128], xi.t[:, kk * 128:(kk + 1) * 128], ident),
                           reads=[xi, cst], writes=[pb], inc=(c4 == 3))
                    eng = "act" if half == 0 else "dve"
                    if eng == "act":
                        op("act", lambda e, half=half, pb=pb, i=i: e.activation(
                            out=xT.t[:, half * 4:(half + 1) * 4, i * 128:(i + 1) * 128],
                            in_=pb.t[:, :].rearrange("p (a b) -> p a b", a=4), func=AF.Copy), reads=[pb], writes=[xT])
                    else:
                        op("dve", lambda e, half=half, pb=pb, i=i: e.tensor_copy(
                            out=xT.t[:, half * 4:(half + 1) * 4, i * 128:(i + 1) * 128],
                            in_=pb.t[:, :].rearrange("p (a b) -> p a b", a=4)), reads=[pb], writes=[xT])
        k.barrier()

        for l in range(L):
            li = l // 2
            with ExitStack() as hs:
                alloc_scr(hs)
                for g in range(NG):
                    make_hn(l, 0, sl, g)
                k.barrier()
            if l % 2 == 0:
                even_mixer(k, nc, locals())
            else:
                odd_mixer(k, nc, locals())
            k.barrier()
            moe_layer(k, nc, locals())
            k.barrier()

        with ExitStack() as pes:
            alloc_scr(pes)
            xn = [k.sb([128, KD, GS], F32, es=pes, name="xn%d" % i) for i in range(2)]
            ost = [k.sb([128, D], F32, es=pes, name="ost%d" % i) for i in range(2)]
            for g in range(NG):
                cols = slice(g * GS, (g + 1) * GS)
                rs = rms_rstd(None, g, float(D))
                xg = xn[g % 2]
                for kk in range(KD):
                    op("dve", lambda e, kk=kk: e.scalar_tensor_tensor(
                        out=xg.t[:, kk, :], in0=xT.t[:, kk, cols], scalar=P("fn", kk), in1=rs.t[:, :],
                        op0=ALU.mult, op1=ALU.mult), reads=[xT, prm, rs], writes=[xg])
                for ti in range(GS // 128):
                    i = g * (GS // 128) + ti
                    o = ost[i % 2]
                    for half in range(2):
                        pb = nb()
                        for c4 in range(4):
                            kk = half * 4 + c4
                            op("pe", lambda e, kk=kk, c4=c4, pb=pb: e.transpose(
                                pb.t[:, c4 * 128:(c4 + 1) * 128], xg.t[:, kk, ti * 128:(ti + 1) * 128], ident),
                               reads=[xg, cst], writes=[pb], inc=(c4 == 3))
                        if half == 0:
                            op("act", lambda e, pb=pb, o=o: e.activation(out=o.t[:, 0:512], in_=pb.t[:, :], func=AF.Copy),
                               reads=[pb], writes=[o])
                        else:
                            op("dve", lambda e, pb=pb, o=o: e.tensor_copy(out=o.t[:, 512:1024], in_=pb.t[:, :]),
                               reads=[pb], writes=[o])
                    dma("sp", out_d[sl * S + i * 128: sl * S + (i + 1) * 128, :], o.t[:, :], reads=[o])
        k.barrier()

    k.barrier(["sp"])
    es.close()
    return nc, k


def even_mixer(k, nc, env):
    op, dma = k.op, k.dma
    xT, hnT, modT, prm, cst = env["xT"], env["hnT"], env["modT"], env["prm"], env["cst"]
    nb, load_w, proj, resid_add, P = env["nb"], env["load_w"], env["proj"], env["resid_add"], env["P"]
    l, li, sl, S, NG = env["l"], env["li"], env["sl"], env["S"], env["NG"]
    abin_d, poolw_d, about_d = env["abin_d"], env["poolw_d"], env["about_d"]
    ones = env["ones"]
    rsb = env["rsb"]
    WIN = (2, 4, 8, 16)
    with ExitStack() as pes:
        env["alloc_scr"](pes)
        sqb, tmpb = env["scr"]["sqb"], env["scr"]["tmpb"]
        ua = [k.sb([128, 4, 16 + GS], F32, es=pes, name="ua%d" % i) for i in range(2)]
        tl = [k.sb([128, 16 + GS], F32, es=pes, name="tl%d" % i) for i in range(2)]
        glu = [k.sb([128, 4, 30 + GS], F32, es=pes, name="glu%d" % i) for i in range(2)]
        accs = [k.sb([128, 4, GS], F32, es=pes, name="cacc%d" % i) for i in range(2)]
        pooleds = [k.sb([128, 4, GS], BF16, es=pes, name="pooled%d" % i) for i in range(2)]
        pw = k.sb([128, 4, 128], BF16, es=pes, name="poolw")
        sig = [k.sb([128, GS], F32, es=pes, name="sig0")] * 2
        cts = [k.sb([128, 8, GS], BF16, es=pes, name="cat0")]
        mu, rstd = rsb[0], rsb[1]
        dma("pool", pw.t[:, :, :], poolw_d[li].rearrange("j c d -> c j d"), writes=[pw])
        def stageA(g):
            cols = slice(g * GS, (g + 1) * GS)
            u_, g_ = ua[g % 2], glu[g % 2]
            acc = accs[g % 2]
            pooled = pooleds[g % 2]
            if g == 0:
                op("pool", lambda e: e.memset(u_.t[:, :, 0:16], 0.0), writes=[u_])
                op("pool", lambda e: e.memset(g_.t[:, :, 0:30], 0.0), writes=[g_])
            else:
                up, gp = ua[(g - 1) % 2], glu[(g - 1) % 2]
                op("pool", lambda e: e.tensor_copy(out=u_.t[:, :, 0:16], in_=up.t[:, :, GS:GS + 16]), reads=[up], writes=[u_])
                op("pool", lambda e: e.tensor_copy(out=g_.t[:, :, 0:30], in_=gp.t[:, :, GS:GS + 30]), reads=[gp], writes=[g_])
            w0 = load_w(abin_d[li, :, 0:512])
            for j in range(4):
                pb = nb()
                proj(w0, j * 128, g, pb)
                op("act", lambda e, j=j, pb=pb: e.activation(out=u_.t[:, j, 16:16 + GS], in_=pb.t[:, :], func=AF.Copy), reads=[pb], writes=[u_])
            wv = load_w(abin_d[li, :, 512:1024])
            wg = load_w(abin_d[li, :, 1024:1536])
            for j in range(4):
                pg = nb()
                proj(wg, j * 128, g, pg)
                sg = sig[j % 2]
                op("act", lambda e, pg=pg, sg=sg: e.activation(out=sg.t[:, :], in_=pg.t[:, :], func=AF.Sigmoid), reads=[pg], writes=[sg])
                pv = nb()
                proj(wv, j * 128, g, pv)
                op("dve", lambda e, j=j, pv=pv, sg=sg: e.tensor_tensor(out=g_.t[:, j, 30:30 + GS], in0=pv.t[:, :], in1=sg.t[:, :], op=ALU.mult),
                   reads=[pv, sg], writes=[g_])
            for j in range(4):
                w_ = WIN[j]
                prev_ap = u_.t[:, j, :]
                prev_buf = u_
                for lev in range(j + 1):
                    sh = 1 << lev
                    c0 = (2 << lev) - 1
                    dst = tl[lev % 2]
                    op("dve", lambda e, dst=dst, prev_ap=prev_ap, sh=sh, c0=c0: e.tensor_tensor(
                        out=dst.t[:, c0:16 + GS], in0=prev_ap[:, c0:16 + GS], in1=prev_ap[:, c0 - sh:16 + GS - sh], op=ALU.add),
                       reads=[prev_buf], writes=[dst])
                    prev_ap = dst.t[:, :]
                    prev_buf = dst
                if g == 0:
                    for t in range(w_ - 1):
                        op("dve", lambda e, t=t, prev_ap=prev_ap, w_=w_: e.tensor_scalar_mul(
                            out=prev_ap[:, 16 + t:17 + t], in0=prev_ap[:, 16 + t:17 + t], scalar1=float(w_) / (t + 1)),
                           reads=[prev_buf], writes=[prev_buf])
                op("dve", lambda e, j=j, prev_ap=prev_ap, w_=w_: e.scalar_tensor_tensor(
                    out=pooled.t[:, j, :], in0=prev_ap[:, 16:16 + GS], scalar=1.0 / w_, in1=u_.t[:, j, 16:16 + GS],
                    op0=ALU.mult, op1=ALU.subtract), reads=[prev_buf, u_], writes=[pooled])
            for j in range(4):
                eng = "dve"
                for tap in range(31):
                    wcol = P("convw", (li * 4 + j) * 31 + tap)
                    srcv = g_.t[:, j, tap:tap + GS]
                    if tap == 0:
                        op(eng, lambda e, j=j, srcv=srcv, wcol=wcol: e.tensor_scalar(
                            out=acc.t[:, j, :], in0=srcv, scalar1=wcol, scalar2=P("convb", li * 4 + j),
                            op0=ALU.mult, op1=ALU.add), reads=[g_, prm], writes=[acc])
                    else:
                        op(eng, lambda e, j=j, srcv=srcv, wcol=wcol: e.scalar_tensor_tensor(
                            out=acc.t[:, j, :], in0=srcv, scalar=wcol, in1=acc.t[:, j, :],
                            op0=ALU.mult, op1=ALU.add), reads=[g_, prm, acc], writes=[acc])
        def stageB(g):
            cols = slice(g * GS, (g + 1) * GS)
            acc, ct = accs[g % 2], cts[0]
            pooled = pooleds[g % 2]
            for j in range(4):
                pb = nb()
                op("pe", lambda e, j=j, pb=pb: e.matmul(pb.t[:, :], pw.t[:, j, :], pooled.t[:, j, :], start=True, stop=True),
                   reads=[pw, pooled], writes=[pb])
                op("act", lambda e, j=j, pb=pb: e.activation(out=ct.t[:, j, :], in_=pb.t[:, :], func=AF.Copy,
                                                             scale=P("pscale", li * 4 + j)), reads=[pb, prm], writes=[ct])
            pm = nb()
            pq = nb()
            for j in range(4):
                s2 = sqb[j % 2]
                op("act", lambda e, j=j, s2=s2: e.activation(out=s2.t[:, :], in_=acc.t[:, j, :], func=AF.Square), reads=[acc], writes=[s2])
                op("pe", lambda e, j=j: e.matmul(pm.t[:, :], ones, acc.t[:, j, :], start=(j == 0), stop=(j == 3)),
                   reads=[acc, cst], writes=[pm])
                op("pe", lambda e, s2=s2, j=j: e.matmul(pq.t[:, :], ones, s2.t[:, :], start=(j == 0), stop=(j == 3)),
                   reads=[s2, cst], writes=[pq])
            op("act", lambda e: e.activation(out=mu.t[:, :], in_=pm.t[:, :], func=AF.Copy, scale=1.0 / 512), reads=[pm], writes=[mu])
            op("dve", lambda e: e.tensor_tensor(out=rstd.t[:, :], in0=mu.t[:, :], in1=mu.t[:, :], op=ALU.mult), reads=[mu], writes=[rstd])
            op("dve", lambda e: e.scalar_tensor_tensor(out=rstd.t[:, :], in0=pq.t[:, :], scalar=1.0 / 512, in1=rstd.t[:, :],
                                                       op0=ALU.mult, op1=ALU.subtract), reads=[pq, rstd], writes=[rstd])
            op("act", lambda e: e.activation(out=rstd.t[:, :], in_=rstd.t[:, :], func=AF.Ln, bias=EPS), reads=[rstd], writes=[rstd])
            op("act", lambda e: e.activation(out=rstd.t[:, :], in_=rstd.t[:, :], func=AF.Exp, scale=-0.5), reads=[rstd], writes=[rstd])
            for j in range(4):
                t_ = tmpb[j % 2]
                op("dve", lambda e, j=j, t_=t_: e.tensor_tensor(out=t_.t[:, :], in0=acc.t[:, j, :], in1=mu.t[:, :], op=ALU.subtract),
                   reads=[acc, mu], writes=[t_])
                op("dve", lambda e, t_=t_: e.tensor_tensor(out=t_.t[:, :], in0=t_.t[:, :], in1=rstd.t[:, :], op=ALU.mult),
                   reads=[t_, rstd], writes=[t_])
                op("act", lambda e, j=j, t_=t_: e.activation(
                    out=ct.t[:, 4 + j, :], in_=t_.t[:, :], func=AF.Silu, bias=P("lnb", li * 4 + j), scale=P("lng", li * 4 + j)),
                   reads=[t_, prm], writes=[ct])
            wo = [load_w(about_d[li, :, h * 512:(h + 1) * 512]) for h in range(2)]
            for oc in range(8):
                pb = nb()
                w = wo[oc // 4]
                for kk in range(8):
                    op("pe", lambda e, kk=kk, oc=oc, w=w, pb=pb: e.matmul(
                        pb.t[:, :], w.t[:, kk, (oc % 4) * 128:(oc % 4 + 1) * 128], ct.t[:, kk, :], start=(kk == 0), stop=(kk == 7)),
                       reads=[w, ct], writes=[pb], inc=(kk == 7))
                resid_add(pb, l, 16, oc, sl, cols)

        stageA(0)
        for g in range(NG):
            if g + 1 < NG:
                stageA(g + 1)
            stageB(g)


def odd_mixer(k, nc, env):
    op, dma = k.op, k.dma
    xT, hnT, modT, prm, cst = env["xT"], env["hnT"], env["modT"], env["prm"], env["cst"]
    nb, load_w, proj, resid_add, P = env["nb"], env["load_w"], env["proj"], env["resid_add"], env["P"]
    l, li, sl, S, NG, NT = env["l"], env["li"], env["sl"], env["S"], env["NG"], env["NT"]
    dnin_d, dnbg_d, dnout_d = env["dnin_d"], env["dnbg_d"], env["dnout_d"]
    ones, ident, sel127, negm2, identb, bankb = env["ones"], env["ident"], env["sel127"], env["negm2"], env["identb"], env["bankb"]
    onesb = env["onesb"]
    TG = GS // 128
    with ExitStack() as pes:
        def sbt(shape, dt, name):
            return k.sb(shape, dt, es=pes, name=name)
        BG = sbt([16, S], F32, "BG")
        TM = sbt([128, NT, 16], F32, "TM")
        egc = sbt([128, NT, 16], F32, "egc")
        bexp = sbt([128, NT, 8], F32, "bexp")
        kdec = sbt([128, NT, 16], F32, "kdec")
        gl = sbt([128, NT, 16], F32, "gl")
        dl = gl
        wbg = sbt([128, KD, 16], BF16, "wbg")
        nea = sbt([16, 1], F32, "nea")
        res2 = ExitStack()
        r1 = [k.sb([16, GS], F32, es=res2, name="r1_%d" % i) for i in range(2)]
        r2 = [k.sb([16, GS], F32, es=res2, name="r2_%d" % i) for i in range(2)]
        dma("pool", wbg.t[:, :, :], dnbg_d[li].rearrange("(k p) n -> p k n", p=128), writes=[wbg])
        op("act", lambda e: e.activation(out=nea.t[:, :], in_=P("alog", li, rows=16), func=AF.Exp), reads=[prm], writes=[nea])
        op("dve", lambda e: e.tensor_scalar_mul(out=nea.t[:, :], in0=nea.t[:, :], scalar1=-1.0), reads=[nea], writes=[nea])
        for g in range(NG):
            cols = slice(g * GS, (g + 1) * GS)
            pb = nb()
            for kk in range(KD):
                op("pe", lambda e, kk=kk, pb=pb: e.matmul(pb.t[0:16, :], wbg.t[:, kk, :], hnT.t[:, kk, cols],
                                                        start=(kk == 0), stop=(kk == KD - 1)),
                   reads=[wbg, hnT], writes=[pb], inc=(kk == KD - 1))
            a, b = r1[0], r1[1]
            c_, d_ = r2[0], r2[1]
            op("act", lambda e, pb=pb: e.activation(out=a.t[:, :], in_=pb.t[0:16, :], func=AF.Sigmoid), reads=[pb], writes=[a])
            op("act", lambda e, pb=pb: e.activation(out=b.t[:, :], in_=pb.t[0:16, :], func=AF.Exp, bias=P("dtb", li, rows=16)),
               reads=[pb, prm], writes=[b])
            op("act", lambda e: e.activation(out=b.t[:, :], in_=b.t[:, :], func=AF.Ln, bias=1.0), reads=[b], writes=[b])
            op("dve", lambda e: e.tensor_scalar_mul(out=b.t[:, :], in0=b.t[:, :], scalar1=nea.t[:, 0:1]), reads=[b, nea], writes=[b])
            src, dst = b, c_
            for lev in range(7):
                sh = 1 << lev
                sv = src.t[:, :].rearrange("p (a t) -> p a t", t=128)
                dv = dst.t[:, :].rearrange("p (a t) -> p a t", t=128)
                op("dve", lambda e, sv=sv, dv=dv, sh=sh: e.tensor_tensor(out=dv[:, :, sh:128], in0=sv[:, :, sh:128],
                                                                         in1=sv[:, :, 0:128 - sh], op=ALU.add),
                   reads=[src], writes=[dst])
                op("dve", lambda e, sv=sv, dv=dv, sh=sh: e.tensor_copy(out=dv[:, :, 0:sh], in_=sv[:, :, 0:sh]),
                   reads=[src], writes=[dst])
                src, dst = dst, src
            op("dve", lambda e: e.tensor_scalar_mul(out=d_.t[:, :], in0=a.t[:, :], scalar1=P("mlo", rows=16)), reads=[a, prm], writes=[d_])
            op("dve", lambda e, src=src: e.scalar_tensor_tensor(out=BG.t[:, cols], in0=src.t[:, :], scalar=P("mhi", rows=16), in1=d_.t[:, :],
                                                                op0=ALU.mult, op1=ALU.add), reads=[src, d_, prm], writes=[BG])
        pb = nb()
        for i in range(NT):
            op("pe", lambda e, i=i, pb=pb: e.transpose(pb.t[:, i * 16:(i + 1) * 16], BG.t[:, i * 128:(i + 1) * 128], ident[0:16, 0:16]),
               reads=[BG, cst], writes=[pb], inc=(i == NT - 1))
        op("act", lambda e, pb=pb: e.activation(out=TM.t[:, :, :], in_=pb.t[:, 0:NT * 16].rearrange("p (a b) -> p a b", b=16), func=AF.Copy),
           reads=[pb], writes=[TM])
        pb2 = nb()
        op("pe", lambda e: e.matmul(pb2.t[:, 0:NT * 16], sel127, TM.t[:, :, :].rearrange("p a b -> p (a b)"), start=True, stop=True),
           reads=[TM, cst], writes=[pb2])
        op("act", lambda e: e.activation(out=gl.t[:, :, :], in_=pb2.t[:, 0:NT * 16].rearrange("p (a b) -> p a b", b=16), func=AF.Copy),
           reads=[pb2], writes=[gl])
        op("act", lambda e: e.activation(out=egc.t[:, :, :], in_=TM.t[:, :, :], func=AF.Exp), reads=[TM], writes=[egc])
        op("dve", lambda e: e.tensor_tensor(out=bexp.t[:, :, :], in0=TM.t[:, :, 0:8], in1=egc.t[:, :, 8:16], op=ALU.mult),
           reads=[TM, egc], writes=[bexp])
        op("dve", lambda e: e.tensor_tensor(out=kdec.t[:, :, :], in0=gl.t[:, :, :], in1=TM.t[:, :, :], op=ALU.subtract),
           reads=[gl, TM], writes=[kdec])
        op("act", lambda e: e.activation(out=kdec.t[:, :, :], in_=kdec.t[:, :, :], func=AF.Exp), reads=[kdec], writes=[kdec])
        op("act", lambda e: e.activation(out=dl.t[:, :, :], in_=gl.t[:, :, :], func=AF.Exp), reads=[gl], writes=[dl])

        k.barrier()
        res2.close()
        uraw = [sbt([128, 3 + GS], F32, "uraw%d" % q) for q in range(3)]
        cacc = [sbt([128, GS], F32, "cacc%d" % q) for q in range(3)]
        rn = env["rsb"]
        qn = sbt([128, GS], F32, "qn")
        kn = sbt([128, GS], F32, "kn")
        rowm = [sbt([16, GS], F32, "rowm0")] * 2
        beta_b = sbt([128, GS], F32, "beta_b")
        gc_b = sbt([128, GS], F32, "gc_b")
        eg_b = beta_b
        kT = sbt([128, GS], BF16, "kT")
        nkbT = sbt([128, GS], BF16, "nkbT")
        nkT = sbt([128, GS], BF16, "nkT")
        qT = sbt([128, GS], BF16, "qT")
        qgTs = [sbt([128, GS], BF16, "qgT%d" % i) for i in range(2)]
        zss = [sbt([128, GS], BF16, "zs%d" % i) for i in range(2)]
        tmp1 = sbt([128, TG, 128], F32, "dtmp1")
        tmp2 = sbt([128, TG, 2, 128], F32, "dtmp2")
        X2s = [sbt([128, TG, 2, 128], BF16, "X2_%d" % i) for i in range(2)]
        NLb = sbt([128, TG, 2, 128], BF16, "NLb")
        NCks = [sbt([128, 2, 2, 128], BF16, "NCk%d" % i) for i in range(2)]
        Ybs = [sbt([128, 2, 2, 128], BF16, "Yb%d" % i) for i in range(2)]
        Tbs = [[sbt([128, 2, 2, 128], BF16, "Tb%d_%d" % (i, j)) for j in range(2)] for i in range(2)]
        mkb = sbt([128, 7, 2, 128], BF16, "mkb")
        dma("pool", mkb.t[:, :, :, :], env["mk_d"][:, :].rearrange("p (a b c) -> p a b c", a=7, b=2), writes=[mkb])
        kbgs = [sbt([128, TG, 128], BF16, "kbg%d" % i) for i in range(2)]
        kds = [sbt([128, TG, 128], BF16, "kd%d" % i) for i in range(2)]
        vbs = [sbt([128, TG, 128], BF16, "vb%d" % i) for i in range(2)]
        nwT = sbt([128, TG, 128], BF16, "nwT")
        vnew = [sbt([128, 128], BF16, "vnew%d" % i) for i in range(2)]
        S32 = sbt([128, 128], F32, "S32")
        Sbf = sbt([128, 128], BF16, "Sbf")
        o32 = sbt([128, GS], F32, "o32")
        og = o32
        og2 = sbt([128, GS], BF16, "og2")
        wo = sbt([128, D], BF16, "wo_h")

        U = 8 * NG

        banks_ = env["banks"]
        bctr = [0, 0]

        def nb1():
            bctr[0] += 1
            return banks_[(bctr[0] - 1) % 4]

        def nb2():
            bctr[1] += 1
            return banks_[4 + (bctr[1] - 1) % 3]

        def stage1(u):
            h, g = divmod(u, NG)
            p = u % 2
            X2, kbg, kd, vb, qgT, zs = X2s[p], kbgs[p], kds[p], vbs[p], qgTs[p], zss[p]
            if g == 0:
                wcur[0] = load_w(dnin_d[li, :, h * 512:(h + 1) * 512])
            w = wcur[0]
            cols = slice(g * GS, (g + 1) * GS)
            for q in range(3):
                ur = uraw[q]
                if g == 0:
                    op("pool", lambda e, ur=ur: e.memset(ur.t[:, 0:3], 0.0), writes=[ur])
                else:
                    op("pool", lambda e, ur=ur: e.tensor_copy(out=ur.t[:, 0:3], in_=ur.t[:, GS:GS + 3]), reads=[ur], writes=[ur])
                pb = nb1()
                proj(w, q * 128, g, pb)
                op("act", lambda e, ur=ur, pb=pb: e.activation(out=ur.t[:, 3:3 + GS], in_=pb.t[:, :], func=AF.Copy), reads=[pb], writes=[ur])
            pb = nb1()
            proj(w, 3 * 128, g, pb)
            op("act", lambda e, pb=pb: e.activation(out=zs.t[:, :], in_=pb.t[:, :], func=AF.Silu), reads=[pb], writes=[zs])
            for q in range(3):
                ur = uraw[q]
                ca = cacc[q]
                cb = (li * 24 + q * 8 + h) * 4
                op("dve", lambda e, ur=ur, ca=ca, cb=cb: e.tensor_scalar_mul(out=ca.t[:, :], in0=ur.t[:, 3:3 + GS], scalar1=P("dnconv", cb + 3)),
                   reads=[ur, prm], writes=[ca])
                for tap in range(3):
                    op("dve", lambda e, ur=ur, ca=ca, cb=cb, tap=tap: e.scalar_tensor_tensor(
                        out=ca.t[:, :], in0=ur.t[:, tap:tap + GS], scalar=P("dnconv", cb + tap), in1=ca.t[:, :],
                        op0=ALU.mult, op1=ALU.add), reads=[ur, prm, ca], writes=[ca])
                op("act", lambda e, ca=ca: e.activation(out=ca.t[:, :], in_=ca.t[:, :], func=AF.Silu), reads=[ca], writes=[ca])
            for q in range(2):
                ca = cacc[q]
                sqs = (qT, qgT)[q]
                op("act", lambda e, ca=ca, sqs=sqs: e.activation(out=sqs.t[:, :], in_=ca.t[:, :], func=AF.Square), reads=[ca], writes=[sqs])
                pb = nb1()
                op("pe", lambda e, pb=pb, sqs=sqs: e.matmul(pb.t[:, :], onesb.t[:, :], sqs.t[:, :], start=True, stop=True),
                   reads=[sqs, onesb], writes=[pb])
                op("act", lambda e, pb=pb: e.activation(out=rn[0].t[:, :], in_=pb.t[:, :], func=AF.Ln, bias=EPS), reads=[pb], writes=[rn[0]])
                op("act", lambda e: e.activation(out=rn[0].t[:, :], in_=rn[0].t[:, :], func=AF.Exp, scale=-0.5), reads=[rn[0]], writes=[rn[0]])
                if q == 0:
                    op("dve", lambda e: e.scalar_tensor_tensor(out=qn.t[:, :], in0=cacc[0].t[:, :], scalar=128.0 ** -0.5, in1=rn[0].t[:, :],
                                                               op0=ALU.mult, op1=ALU.mult), reads=[cacc[0], rn[0]], writes=[qn])
                else:
                    op("dve", lambda e: e.tensor_tensor(out=kn.t[:, :], in0=cacc[1].t[:, :], in1=rn[0].t[:, :], op=ALU.mult),
                       reads=[cacc[1], rn[0]], writes=[kn])
            for qi, (row, dstb) in enumerate(((h, beta_b), (8 + h, gc_b))):
                rm = rowm[qi]
                op("dve", lambda e, rm=rm, row=row: e.tensor_scalar_mul(out=rm.t[:, :], in0=BG.t[:, cols], scalar1=ident[0:16, row:row + 1]),
                   reads=[BG, cst], writes=[rm])
                pb = nb1()
                op("pe", lambda e, rm=rm, pb=pb: e.matmul(pb.t[:, :], ones[0:16, :], rm.t[:, :], start=True, stop=True),
                   reads=[rm, cst], writes=[pb])
                op("act", lambda e, pb=pb, dstb=dstb: e.activation(out=dstb.t[:, :], in_=pb.t[:, :], func=AF.Copy), reads=[pb], writes=[dstb])
            op("act", lambda e: e.activation(out=kT.t[:, :], in_=kn.t[:, :], func=AF.Copy), reads=[kn], writes=[kT])
            op("act", lambda e: e.activation(out=nkT.t[:, :], in_=kn.t[:, :], func=AF.Copy, scale=-1.0), reads=[kn], writes=[nkT])
            op("pool", lambda e: e.tensor_tensor(out=nkbT.t[:, :], in0=kn.t[:, :], in1=beta_b.t[:, :], op=ALU.mult),
               reads=[kn, beta_b], writes=[nkbT])
            op("act", lambda e: e.activation(out=eg_b.t[:, :], in_=gc_b.t[:, :], func=AF.Exp), reads=[gc_b], writes=[eg_b])
            op("act", lambda e: e.activation(out=qT.t[:, :], in_=qn.t[:, :], func=AF.Copy), reads=[qn], writes=[qT])
            op("pool", lambda e: e.tensor_tensor(out=qgT.t[:, :], in0=qn.t[:, :], in1=eg_b.t[:, :], op=ALU.mult),
               reads=[qn, eg_b], writes=[qgT])
            pk = nb1()
            pv = nb1()
            for t in range(TG):
                tc_ = slice(t * 128, (t + 1) * 128)
                op("pe", lambda e, t=t, tc_=tc_: e.transpose(pk.t[:, tc_], kn.t[:, tc_], ident), reads=[kn, cst], writes=[pk], inc=(t == TG - 1))
            for t in range(TG):
                tc_ = slice(t * 128, (t + 1) * 128)
                op("pe", lambda e, t=t, tc_=tc_: e.transpose(pv.t[:, tc_], cacc[2].t[:, tc_], ident), reads=[cacc[2], cst], writes=[pv], inc=(t == TG - 1))
            pm = [nb1(), nb1()]
            for t in range(TG):
                ti = g * TG + t
                tc_ = slice(t * 128, (t + 1) * 128)
                op("act", lambda e, t=t, ti=ti, tc_=tc_: e.activation(out=kbg.t[:, t, :], in_=pk.t[:, tc_], func=AF.Copy, scale=bexp.t[:, ti, h:h + 1]),
                   reads=[pk, bexp], writes=[kbg])
                op("act", lambda e, t=t, ti=ti, tc_=tc_: e.activation(out=kd.t[:, t, :], in_=pk.t[:, tc_], func=AF.Copy, scale=kdec.t[:, ti, 8 + h:9 + h]),
                   reads=[pk, kdec], writes=[kd])
                op("act", lambda e, t=t, ti=ti, tc_=tc_: e.activation(out=vb.t[:, t, :], in_=pv.t[:, tc_], func=AF.Copy, scale=TM.t[:, ti, h:h + 1]),
                   reads=[pv, TM], writes=[vb])
                op("dve", lambda e, t=t, ti=ti, tc_=tc_: e.tensor_scalar(out=tmp1.t[:, t, :], in0=gc_b.t[:, tc_], scalar1=TM.t[:, ti, 8 + h:9 + h],
                                                                         scalar2=0.0, op0=ALU.subtract, op1=ALU.min), reads=[gc_b, TM], writes=[tmp1])
                op("dve", lambda e, t=t: e.tensor_tensor(out=tmp2.t[:, t, :, :], in0=tmp1.t[:, t, :].unsqueeze(1).to_broadcast([128, 2, 128]),
                                                          in1=negm2.rearrange("p (a b) -> p a b", a=2), op=ALU.add), reads=[tmp1, cst], writes=[tmp2])
                pmm = pm[t // 2]
                o0 = (t % 2) * 256
                op("pe", lambda e, tc_=tc_, pmm=pmm, o0=o0: e.matmul(pmm.t[:, o0:o0 + 128], nkT.t[:, tc_], nkbT.t[:, tc_], start=True, stop=True),
                   reads=[nkT, nkbT], writes=[pmm], inc=False)
                op("pe", lambda e, tc_=tc_, pmm=pmm, o0=o0: e.matmul(pmm.t[:, o0 + 128:o0 + 256], kT.t[:, tc_], qT.t[:, tc_], start=True, stop=True),
                   reads=[kT, qT], writes=[pmm])
            op("act", lambda e: e.activation(out=tmp2.t[:, :, :, :], in_=tmp2.t[:, :, :, :], func=AF.Exp), reads=[tmp2], writes=[tmp2])
            for hf in range(2):
                op("dve", lambda e, hf=hf: e.tensor_tensor(
                    out=X2.t[:, 2 * hf:2 * hf + 2, :, :], in0=pm[hf].t[:, :].rearrange("p (a b c) -> p a b c", a=2, b=2),
                    in1=tmp2.t[:, 2 * hf:2 * hf + 2, :, :], op=ALU.mult), reads=[pm[hf], tmp2], writes=[X2])

        def stage2(u):
            h, g = divmod(u, NG)
            p = u % 2
            X2, kbg, kd, vb, qgT, zs = X2s[p], kbgs[p], kds[p], vbs[p], qgTs[p], zss[p]
            cols = slice(g * GS, (g + 1) * GS)
            if g == 0:
                dma("pool", wo.t[:, :], dnout_d[li, h * 128:(h + 1) * 128, :], writes=[wo])
                op("dve", lambda e: e.memset(S32.t[:, :], 0.0), writes=[S32])
                op("dve", lambda e: e.memset(Sbf.t[:, :], 0.0), writes=[Sbf])
            for t in range(TG):
                op("pe", lambda e, t=t: e.transpose(bankb.t[:, t * 128:(t + 1) * 128], X2.t[:, t, 0, :], identb.t[:, :]),
                   reads=[X2, identb], writes=[bankb], inc=(t == TG - 1))
            op("act", lambda e: e.activation(out=NLb.t[:, :, 0, :], in_=bankb.t[:, 0:TG * 128].rearrange("p (a b) -> p a b", a=TG), func=AF.Copy),
               reads=[bankb], writes=[NLb])
            op("act", lambda e: e.activation(out=NLb.t[:, :, 1, :], in_=X2.t[:, :, 0, :], func=AF.Copy), reads=[X2], writes=[NLb])
            for hf in range(2):
                op("dve", lambda e, hf=hf: e.tensor_tensor(out=NCks[hf].t[:, :, :, :], in0=NLb.t[:, 2 * hf:2 * hf + 2, :, :],
                                                           in1=mkb.t[:, 0, :, :].unsqueeze(1).to_broadcast([128, 2, 2, 128]), op=ALU.mult),
                   reads=[NLb, mkb], writes=[NCks[hf]])
            Tc = Tbs[0]
            for hf in range(2):
                op("pool", lambda e, hf=hf, Tc=Tc: e.tensor_tensor(out=Tc[hf].t[:, :, :, :], in0=NCks[hf].t[:, :, :, :],
                                                                   in1=identb.t[:, :].unsqueeze(1).unsqueeze(1).to_broadcast([128, 2, 2, 128]), op=ALU.add),
                   reads=[NCks[hf], identb], writes=[Tc[hf]])
            for lev in range(1, 7):
                last = (lev == 6)
                for hf in range(2):
                    op("dve", lambda e, hf=hf, lev=lev: e.tensor_tensor(out=NCks[hf].t[:, :, :, :], in0=NLb.t[:, 2 * hf:2 * hf + 2, :, :],
                                                                        in1=mkb.t[:, lev, :, :].unsqueeze(1).to_broadcast([128, 2, 2, 128]), op=ALU.mult),
                       reads=[NLb, mkb], writes=[NCks[hf]])
                py = [nb2(), nb2()]
                for t in range(TG):
                    hf, tt_ = t // 2, t % 2
                    ppp = py[hf]
                    o0 = tt_ * 256
                    if not last:
                        op("pe", lambda e, hf=hf, tt_=tt_, ppp=ppp, o0=o0, Tc=Tc: e.matmul(ppp.t[:, o0:o0 + 128], NCks[hf].t[:, tt_, 1, :], Tc[hf].t[:, tt_, 0, :], start=True, stop=True),
                           reads=[NCks[hf], Tc[hf]], writes=[ppp], inc=False)
                    op("pe", lambda e, hf=hf, tt_=tt_, ppp=ppp, o0=o0, Tc=Tc: e.matmul(ppp.t[:, o0 + 128:o0 + 256], NCks[hf].t[:, tt_, 0, :], Tc[hf].t[:, tt_, 1, :], start=True, stop=True),
                       reads=[NCks[hf], Tc[hf]], writes=[ppp])
                for hf in range(2):
                    src4 = py[hf].t[:, :].rearrange("p (a b c) -> p a b c", a=2, b=2)
                    if last:
                        src, dst = src4[:, :, 1, :], Ybs[hf].t[:, :, 1, :]
                    else:
                        src, dst = src4, Ybs[hf].t[:, :, :, :]
                    if hf == 0:
                        op("act", lambda e, src=src, dst=dst: e.activation(out=dst, in_=src, func=AF.Copy), reads=[py[hf]], writes=[Ybs[hf]])
                    else:
                        op("dve", lambda e, src=src, dst=dst: e.tensor_copy(out=dst, in_=src), reads=[py[hf]], writes=[Ybs[hf]])
                pz = [nb2(), nb2()]
                for t in range(TG):
                    hf, tt_ = t // 2, t % 2
                    pzz = pz[hf]
                    o0 = tt_ * 256
                    if not last:
                        op("pe", lambda e, hf=hf, tt_=tt_, pzz=pzz, o0=o0, Tc=Tc: e.matmul(pzz.t[:, o0:o0 + 128], Tc[hf].t[:, tt_, 1, :], Ybs[hf].t[:, tt_, 0, :], start=True, stop=True),
                           reads=[Tc[hf], Ybs[hf]], writes=[pzz], inc=False)
                    op("pe", lambda e, hf=hf, tt_=tt_, pzz=pzz, o0=o0, Tc=Tc: e.matmul(pzz.t[:, o0 + 128:o0 + 256], Tc[hf].t[:, tt_, 0, :], Ybs[hf].t[:, tt_, 1, :], start=True, stop=True),
                       reads=[Tc[hf], Ybs[hf]], writes=[pzz])
                Tn = Tbs[lev % 2]
                for hf in range(2):
                    src4 = pz[hf].t[:, :].rearrange("p (a b c) -> p a b c", a=2, b=2)
                    if last:
                        op("dve", lambda e, hf=hf, src4=src4, Tn=Tn, Tc=Tc: e.tensor_tensor(
                            out=Tn[hf].t[:, :, 1, :], in0=src4[:, :, 1, :], in1=Tc[hf].t[:, :, 1, :], op=ALU.add), reads=[pz[hf], Tc[hf]], writes=[Tn[hf]])
                    else:
                        op("dve", lambda e, hf=hf, src4=src4, Tn=Tn, Tc=Tc: e.tensor_tensor(
                            out=Tn[hf].t[:, :, :, :], in0=src4, in1=Tc[hf].t[:, :, :, :], op=ALU.add), reads=[pz[hf], Tc[hf]], writes=[Tn[hf]])
                Tc = Tn
            TT = Tc
            pw_ = nb2()
            for t in range(TG):
                op("pe", lambda e, t=t: e.matmul(pw_.t[:, t * 128:(t + 1) * 128], kbg.t[:, t, :], TT[t // 2].t[:, t % 2, 1, :], start=True, stop=True),
                   reads=[kbg, TT[t // 2]], writes=[pw_], inc=(t == TG - 1))
            op("act", lambda e: e.activation(out=nwT.t[:, :, :], in_=pw_.t[:, :].rearrange("p (a b) -> p a b", a=TG), func=AF.Copy, scale=-1.0),
               reads=[pw_], writes=[nwT])
            po = nb2()
            pvns = [nb2(), nb2()]
            for t in range(TG):
                ti = g * TG + t
                tc_ = slice(t * 128, (t + 1) * 128)
                vn = vnew[t % 2]
                pvn = pvns[t % 2]
                op("pe", lambda e, t=t, pvn=pvn: e.matmul(pvn.t[:, 0:128], TT[t // 2].t[:, t % 2, 1, :], vb.t[:, t, :], start=True, stop=False),
                   reads=[TT[t // 2], vb], writes=[pvn], inc=False)
                op("pe", lambda e, t=t, pvn=pvn: e.matmul(pvn.t[:, 0:128], nwT.t[:, t, :], Sbf.t[:, :], start=False, stop=True),
                   reads=[nwT, Sbf], writes=[pvn])
                op("act", lambda e, pvn=pvn, vn=vn: e.activation(out=vn.t[:, :], in_=pvn.t[:, 0:128], func=AF.Copy), reads=[pvn], writes=[vn])
                op("pe", lambda e, tc_=tc_: e.matmul(po.t[:, tc_], Sbf.t[:, :], qgT.t[:, tc_], start=True, stop=False),
                   reads=[Sbf, qgT], writes=[po], inc=False)
                op("pe", lambda e, t=t, tc_=tc_, vn=vn: e.matmul(po.t[:, tc_], vn.t[:, :], X2.t[:, t, 1, :], start=False, stop=True),
                   reads=[vn, X2], writes=[po], inc=False)
                op("pe", lambda e, t=t, pvn=pvn, vn=vn: e.matmul(pvn.t[:, 128:256], kd.t[:, t, :], vn.t[:, :], start=True, stop=True),
                   reads=[kd, vn], writes=[pvn])
                op("dve", lambda e, pvn=pvn, ti=ti: e.scalar_tensor_tensor(out=Sbf.t[:, :], in0=S32.t[:, :], scalar=dl.t[:, ti, 8 + h:9 + h],
                                                                           in1=pvn.t[:, 128:256], op0=ALU.mult, op1=ALU.add),
                   reads=[S32, dl, pvn], writes=[Sbf])
                op("dve", lambda e, pvn=pvn, ti=ti: e.scalar_tensor_tensor(out=S32.t[:, :], in0=S32.t[:, :], scalar=dl.t[:, ti, 8 + h:9 + h],
                                                                           in1=pvn.t[:, 128:256], op0=ALU.mult, op1=ALU.add),
                   reads=[S32, dl, pvn], writes=[S32])
            op("act", lambda e: e.activation(out=o32.t[:, :], in_=po.t[:, :], func=AF.Copy), reads=[po], writes=[o32])
            op("act", lambda e: e.activation(out=og2.t[:, :], in_=o32.t[:, :], func=AF.Square), reads=[o32], writes=[og2])
            pb = nb2()
            op("pe", lambda e, pb=pb: e.matmul(pb.t[:, :], onesb.t[:, :], og2.t[:, :], start=True, stop=True), reads=[og2, onesb], writes=[pb])
            op("act", lambda e, pb=pb: e.activation(out=rn[1].t[:, :], in_=pb.t[:, :], func=AF.Ln, scale=1.0 / 128, bias=EPS), reads=[pb], writes=[rn[1]])
            op("act", lambda e: e.activation(out=rn[1].t[:, :], in_=rn[1].t[:, :], func=AF.Exp, scale=-0.5), reads=[rn[1]], writes=[rn[1]])
            op("dve", lambda e: e.tensor_tensor(out=og.t[:, :], in0=o32.t[:, :], in1=rn[1].t[:, :], op=ALU.mult), reads=[o32, rn[1]], writes=[og])
            op("dve", lambda e: e.scalar_tensor_tensor(out=og2.t[:, :], in0=og.t[:, :], scalar=P("onorm", li), in1=zs.t[:, :],
                                                       op0=ALU.mult, op1=ALU.mult), reads=[og, prm, zs], writes=[og2])
            for oc in range(8):
                pb = nb2()
                op("pe", lambda e, oc=oc, pb=pb: e.matmul(pb.t[:, :], wo.t[:, oc * 128:(oc + 1) * 128], og2.t[:, :], start=True, stop=True),
                   reads=[wo, og2], writes=[pb])
                resid_add(pb, l, 16, oc, sl, cols)


        wcur = [None]
        k.replay(k.record(lambda: stage1(0)))
        for u in range(U):
            ra = k.record(lambda: stage2(u))
            rb = k.record(lambda: stage1(u + 1)) if u + 1 < U else []
            k.replay(k.interleave(ra, rb) if INTERLEAVE else (ra + rb))


def moe_layer(k, nc, env):
    op, dma = k.op, k.dma
    xT, hnT, modT, prm, cst = env["xT"], env["hnT"], env["modT"], env["prm"], env["cst"]
    nb, nw, resid_add, P, make_hn = env["nb"], env["nw"], env["resid_add"], env["P"], env["make_hn"]
    l, sl, S, NG, NT = env["l"], env["sl"], env["S"], env["NG"], env["NT"]
    moer_d, moeup_d, moedn_d = env["moer_d"], env["moeup_d"], env["moedn_d"]
    ones, ident = env["ones"], env["ident"]
    wpool = env["wpool"]
    TG = GS // 128
    with ExitStack() as pes:
        GT = k.sb([32, S], F32, es=pes, name="GT")
        with ExitStack() as res:
            def sbt(shape, dt, name):
                return k.sb(shape, dt, es=res, name=name)
            env["alloc_scr"](res)
            h32 = sbt([128, KD, GS], F32, "hn32")
            wr = sbt([128, KD, 36], F32, "wr")
            lg = sbt([128, 36], F32, "lg")
            sm = {n: sbt([128, 36], F32, "sm_" + n) for n in ("mx", "oh", "pen", "ml", "m1", "k1", "ml2", "m2", "k2", "ex", "sm", "gp", "r", "den", "w1", "w2", "G", "nmx")}
            dma("sp", wr.t[:, :, :], moer_d[l].rearrange("(k p) n -> p k n", p=128), writes=[wr])

            def dv(fn, reads, writes):
                op("dve", fn, reads=reads, writes=writes)

            for g in range(NG):
                make_hn(l, 1, sl, g, hn32=h32)
                for t in range(TG):
                    ti = g * TG + t
                    pb = nb()
                    for kk in range(KD):
                        op("pe", lambda e, kk=kk, t=t, pb=pb: e.matmul(pb.t[:, 0:36], h32.t[:, kk, t * 128:(t + 1) * 128], wr.t[:, kk, :],
                                                                     start=(kk == 0), stop=(kk == KD - 1)),
                           reads=[h32, wr], writes=[pb], inc=(kk == KD - 1))
                    dv(lambda e, pb=pb: e.tensor_tensor(out=lg.t[:, :], in0=pb.t[:, 0:36], in1=P("rbias", l * 36, 36), op=ALU.add), [pb, prm], [lg])
                    dv(lambda e: e.tensor_reduce(out=sm["mx"].t[:, 0:1], in_=lg.t[:, 0:4], axis=mybir.AxisListType.X, op=ALU.max), [lg], [sm["mx"]])
                    dv(lambda e: e.tensor_scalar(out=sm["oh"].t[:, 0:4], in0=lg.t[:, 0:4], scalar1=sm["mx"].t[:, 0:1], scalar2=None, op0=ALU.is_ge), [lg, sm["mx"]], [sm["oh"]])
                    dv(lambda e: e.tensor_scalar_mul(out=sm["nmx"].t[:, 0:1], in0=sm["mx"].t[:, 0:1], scalar1=-1.0), [sm["mx"]], [sm["nmx"]])
                    op("act", lambda e: e.activation(out=sm["ex"].t[:, 0:4], in_=lg.t[:, 0:4], func=AF.Exp, bias=sm["nmx"].t[:, 0:1]), reads=[lg, sm["nmx"]], writes=[sm["ex"]])
                    dv(lambda e: e.tensor_reduce(out=sm["sm"].t[:, 0:1], in_=sm["ex"].t[:, 0:4], axis=mybir.AxisListType.X, op=ALU.add), [sm["ex"]], [sm["sm"]])
                    dv(lambda e: e.reciprocal(out=sm["gp"].t[:, 0:1], in_=sm["sm"].t[:, 0:1]), [sm["sm"]], [sm["gp"]])
                    dv(lambda e: e.tensor_scalar(out=sm["pen"].t[:, 0:4], in0=sm["oh"].t[:, 0:4], scalar1=-1.0, scalar2=1.0e4, op0=ALU.add, op1=ALU.mult), [sm["oh"]], [sm["pen"]])
                    dv(lambda e: e.tensor_tensor(out=sm["ml"].t[:, 0:32].rearrange("p (a b) -> p a b", a=4), in0=lg.t[:, 4:36].rearrange("p (a b) -> p a b", a=4),
                                                 in1=sm["pen"].t[:, 0:4].unsqueeze(2).to_broadcast([128, 4, 8]), op=ALU.add), [lg, sm["pen"]], [sm["ml"]])
                    dv(lambda e: e.tensor_reduce(out=sm["m1"].t[:, 0:1], in_=sm["ml"].t[:, 0:32], axis=mybir.AxisListType.X, op=ALU.max), [sm["ml"]], [sm["m1"]])
                    dv(lambda e: e.tensor_scalar(out=sm["k1"].t[:, 0:32], in0=sm["ml"].t[:, 0:32], scalar1=sm["m1"].t[:, 0:1], scalar2=None, op0=ALU.is_ge), [sm["ml"], sm["m1"]], [sm["k1"]])
                    dv(lambda e: e.scalar_tensor_tensor(out=sm["ml2"].t[:, 0:32], in0=sm["k1"].t[:, 0:32], scalar=-1.0e4, in1=sm["ml"].t[:, 0:32], op0=ALU.mult, op1=ALU.add),
                       [sm["k1"], sm["ml"]], [sm["ml2"]])
                    dv(lambda e: e.tensor_reduce(out=sm["m2"].t[:, 0:1], in_=sm["ml2"].t[:, 0:32], axis=mybir.AxisListType.X, op=ALU.max), [sm["ml2"]], [sm["m2"]])
                    dv(lambda e: e.tensor_scalar(out=sm["k2"].t[:, 0:32], in0=sm["ml2"].t[:, 0:32], scalar1=sm["m2"].t[:, 0:1], scalar2=None, op0=ALU.is_ge), [sm["ml2"], sm["m2"]], [sm["k2"]])
                    dv(lambda e: e.tensor_tensor(out=sm["r"].t[:, 0:1], in0=sm["m2"].t[:, 0:1], in1=sm["m1"].t[:, 0:1], op=ALU.subtract), [sm["m2"], sm["m1"]], [sm["r"]])
                    op("act", lambda e: e.activation(out=sm["r"].t[:, 0:1], in_=sm["r"].t[:, 0:1], func=AF.Exp), reads=[sm["r"]], writes=[sm["r"]])
                    dv(lambda e: e.tensor_scalar_add(out=sm["den"].t[:, 0:1], in0=sm["r"].t[:, 0:1], scalar1=1.0), [sm["r"]], [sm["den"]])
                    dv(lambda e: e.reciprocal(out=sm["den"].t[:, 0:1], in_=sm["den"].t[:, 0:1]), [sm["den"]], [sm["den"]])
                    dv(lambda e: e.tensor_tensor(out=sm["w1"].t[:, 0:1], in0=sm["gp"].t[:, 0:1], in1=sm["den"].t[:, 0:1], op=ALU.mult), [sm["gp"], sm["den"]], [sm["w1"]])
                    dv(lambda e: e.tensor_tensor(out=sm["w2"].t[:, 0:1], in0=sm["w1"].t[:, 0:1], in1=sm["r"].t[:, 0:1], op=ALU.mult), [sm["w1"], sm["r"]], [sm["w2"]])
                    dv(lambda e: e.tensor_scalar_mul(out=sm["G"].t[:, 0:32], in0=sm["k1"].t[:, 0:32], scalar1=sm["w1"].t[:, 0:1]), [sm["k1"], sm["w1"]], [sm["G"]])
                    dv(lambda e: e.scalar_tensor_tensor(out=sm["G"].t[:, 0:32], in0=sm["k2"].t[:, 0:32], scalar=sm["w2"].t[:, 0:1], in1=sm["G"].t[:, 0:32], op0=ALU.mult, op1=ALU.add),
                       [sm["k2"], sm["w2"], sm["G"]], [sm["G"]])
                    pt = nb()
                    op("pe", lambda e, pt=pt: e.transpose(pt.t[0:32, 0:128], sm["G"].t[:, 0:32], ident), reads=[sm["G"], cst], writes=[pt])
                    op("act", lambda e, pt=pt, ti=ti: e.activation(out=GT.t[:, ti * 128:(ti + 1) * 128], in_=pt.t[0:32, 0:128], func=AF.Copy), reads=[pt], writes=[GT])
            k.barrier()

        def sbt(shape, dt, name):
            return k.sb(shape, dt, es=pes, name=name)
        selall = sbt([32, NEXP // 2, 128], F32, "selall")
        wup = list(wpool) + [sbt([128, KD, 512], BF16, "wupx%d" % i) for i in range(2)]
        wdn = [sbt([128, 2, D], BF16, "wdn%d" % i) for i in range(4)]
        gsb = [sbt([128, GS], F32, "gsb%d" % i) for i in range(2)]
        sgb = [sbt([128, GS], F32, "sgb%d" % i) for i in range(2)]
        tb = sgb
        hb = [[sbt([128, 2, GS], BF16, "hb%d_%d" % (i, j)) for j in range(2)] for i in range(2)]
        def build_sel(half):
            for ex_ in range(NEXP // 2):
                exg = half * (NEXP // 2) + ex_
                op("dve", lambda e, ex_=ex_, exg=exg: e.tensor_copy(out=selall.t[:, ex_, :], in_=ident[0:32, exg:exg + 1].to_broadcast([32, 128])),
                   reads=[cst], writes=[selall])

        build_sel(0)
        def fetch_pair(ep_):
            for ei_ in range(2):
                ex_ = 2 * ep_ + ei_
                wu_ = wup[ex_ % 4]
                dma("pool", wu_.t[:, :, :], moeup_d[l, ex_].rearrange("(k p) n -> p k n", p=128), writes=[wu_])
                wd_ = wdn[ex_ % 4]
                dma("pool", wd_.t[:, :, :], moedn_d[l, ex_].rearrange("(k p) n -> p k n", p=128), writes=[wd_])

        fetch_pair(0)
        for ep in range(NEXP // 2):
            if ep + 1 < NEXP // 2:
                fetch_pair(ep + 1)
            if ep == NEXP // 4:
                build_sel(1)
            wus = [wup[(2 * ep + ei) % 4] for ei in range(2)]
            wds = [wdn[(2 * ep + ei) % 4] for ei in range(2)]
            def phase1(g):
                cols = slice(g * GS, (g + 1) * GS)
                for ei in range(2):
                    ex = 2 * ep + ei
                    wu = wus[ei]
                    pg_ = nb()
                    op("pe", lambda e, ex=ex, pg_=pg_: e.matmul(pg_.t[:, :], selall.t[:, ex % (NEXP // 2), :], GT.t[:, cols], start=True, stop=True),
                       reads=[selall, GT], writes=[pg_])
                    gs_ = gsb[ei]
                    op("dve", lambda e, pg_=pg_, gs_=gs_: e.tensor_copy(out=gs_.t[:, :], in_=pg_.t[:, :]), reads=[pg_], writes=[gs_])
                    hh = hb[ei][g % 2]
                    for j in range(2):
                        pgt = nb()
                        put = nb()
                        for kk in range(KD):
                            op("pe", lambda e, kk=kk, j=j, pgt=pgt, wu=wu: e.matmul(pgt.t[:, :], wu.t[:, kk, j * 128:(j + 1) * 128], hnT.t[:, kk, cols],
                                                                                 start=(kk == 0), stop=(kk == KD - 1)), reads=[wu, hnT], writes=[pgt], inc=(kk == KD - 1))
                        for kk in range(KD):
                            op("pe", lambda e, kk=kk, j=j, put=put, wu=wu: e.matmul(put.t[:, :], wu.t[:, kk, FE + j * 128:FE + (j + 1) * 128], hnT.t[:, kk, cols],
                                                                                 start=(kk == 0), stop=(kk == KD - 1)), reads=[wu, hnT], writes=[put], inc=(kk == KD - 1))
                        sg = sgb[j]
                        tt = tb[j]
                        op("act", lambda e, pgt=pgt, sg=sg: e.activation(out=sg.t[:, :], in_=pgt.t[:, :], func=AF.Silu), reads=[pgt], writes=[sg])
                        op("dve", lambda e, put=put, sg=sg, tt=tt: e.tensor_tensor(out=tt.t[:, :], in0=put.t[:, :], in1=sg.t[:, :], op=ALU.mult),
                           reads=[put, sg], writes=[tt])
                        op("pool", lambda e, j=j, tt=tt, hh=hh, gs_=gs_: e.tensor_tensor(out=hh.t[:, j, :], in0=tt.t[:, :], in1=gs_.t[:, :], op=ALU.mult),
                           reads=[tt, gs_], writes=[hh])
            def phase2(g):
                cols = slice(g * GS, (g + 1) * GS)
                for oc in range(8):
                    py = nb()
                    n = 0
                    for ei in range(2):
                        hh = hb[ei][g % 2]
                        wd = wds[ei]
                        for j in range(2):
                            op("pe", lambda e, j=j, oc=oc, py=py, wd=wd, hh=hh, n=n: e.matmul(py.t[:, :], wd.t[:, j, oc * 128:(oc + 1) * 128], hh.t[:, j, :],
                                                                                          start=(n == 0), stop=(n == 3)), reads=[wd, hh], writes=[py], inc=(n == 3))
                            n += 1
                    resid_add(py, l, 40, oc, sl, cols)

            for g in range(NG):
                phase1(g)
                if g > 0:
                    phase2(g - 1)
            phase2(NG - 1)


_CACHE = {}


def prepare_inputs(inputs, cfg):
    NSEQ, S, L, NCO = cfg["NSEQ"], cfg["S"], cfg["DEPTH"], cfg["NCORES"]
    f = lambda a: np.ascontiguousarray(np.asarray(a, np.float32))
    prm, _ = _build_prm(inputs, L)
    cst = _build_cst()
    dnin = f(inputs["dn_w_in"])
    qkvz = dnin[:, :, :4096].reshape(2, D, 4, 8, 128).transpose(0, 1, 3, 2, 4).reshape(2, D, 4096)
    shared = {
        "cst": cst, "prm": prm, "mk": _build_mk(),
        "mod_w": f(inputs["mod_w"])[:L], "ab_w_in": f(inputs["ab_w_in"]), "pool_w": f(inputs["pool_w"]),
        "ab_w_out": f(inputs["ab_w_out"]), "dn_w_in_h": np.ascontiguousarray(qkvz),
        "dn_w_bg": np.ascontiguousarray(dnin[:, :, 4096:4112]), "dn_w_out": f(inputs["dn_w_out"]),
        "moe_w_r": np.ascontiguousarray(np.concatenate([f(inputs["moe_w_grp"])[:L], f(inputs["moe_w_exp"])[:L]], axis=2)),
        "moe_w_up": f(inputs["moe_w_up"])[:L], "moe_w_down": f(inputs["moe_w_down"])[:L],
    }
    x = f(inputs["x"])
    c = f(inputs["c"])
    maps = []
    for core in range(NCO):
        xs = x[core * NSEQ:(core + 1) * NSEQ].reshape(NSEQ * S, D)
        cs = c[core * NSEQ:(core + 1) * NSEQ]
        cT = cs.reshape(NSEQ, 8, 128).transpose(2, 1, 0).reshape(128, 8 * NSEQ)
        m = dict(shared)
        m["x"] = np.ascontiguousarray(xs)
        m["cT"] = np.ascontiguousarray(cT)
        maps.append(m)
    return maps


def kernel(**inputs):
    cfg = CFG
    key = tuple(sorted(cfg.items()))
    if key not in _CACHE:
        _CACHE[key] = build_program(cfg)[0]
    nc = _CACHE[key]
    maps = prepare_inputs(inputs, cfg)
    res = run_bass_kernel_spmd(nc, maps, core_ids=list(range(cfg["NCORES"])))
    outs = [np.asarray(r["out"], np.float32).reshape(cfg["NSEQ"], cfg["S"], D) for r in res.results]
    return np.concatenate(outs, axis=0)
```

```python
import numpy as np
from contextlib import ExitStack
import concourse.bass as bass
import concourse.mybir as mybir
from concourse.bass_utils import run_bass_kernel_spmd

F32 = mybir.dt.float32
BF16 = mybir.dt.bfloat16
AF = mybir.ActivationFunctionType
ALU = mybir.AluOpType

D = 1024
KD = 8
EPS = 1e-6
NEXP = 32
FE = 256
NDSEM = 8
GS = 512
NEG = -30000.0
INTERLEAVE = True

CFG = dict(NSEQ=4, S=2048, DEPTH=4, NCORES=8)


class Buf:
    __slots__ = ("t", "w", "r", "name")

    def __init__(self, t, name):
        self.t = t
        self.w = {}
        self.r = {}
        self.name = name

    def __getitem__(self, k):
        return self.t[k]


class _Proxy:
    def __init__(self):
        self.call = None

    def __getattr__(self, name):
        def f(*a, **kw):
            self.call = (name, a, kw)
            return None
        return f


class K:
    def __init__(self, nc, es):
        self.nc = nc
        self.es = es
        self.E = {"pe": nc.tensor, "act": nc.scalar, "dve": nc.vector, "pool": nc.gpsimd, "sp": nc.sync}
        self.sem = {e: es.enter_context(nc.semaphore("s_" + e)) for e in self.E}
        self.cnt = {e: 0 for e in self.E}
        self.waited = {e: {} for e in self.E}
        self.dq = {}
        for q in ("sp", "pool"):
            self.dq[q] = [[es.enter_context(nc.semaphore("d_%s%d" % (q, i))), 0, "d_%s%d" % (q, i)] for i in range(NDSEM)]
        self.dqi = {"sp": 0, "pool": 0}
        self.uid = 0
        self.nins = 0
        self.rec = None

    def sb(self, shape, dt, es=None, name=None):
        self.uid += 1
        name = (name or "t") + "_%d" % self.uid
        t = (es or self.es).enter_context(self.nc.sbuf_tensor(name, list(shape), dt))
        return Buf(t, name)

    def psb(self, shape, dt, name):
        t = self.es.enter_context(self.nc.psum_tensor(name, list(shape), dt))
        return Buf(t, name)

    def _wait(self, eng, tok):
        key, sem, val, peng = tok
        if peng == "pe" and eng == "pe":
            return
        if self.waited[eng].get(key, 0) >= val:
            return
        self.E[eng].wait_ge(sem, val)
        self.nins += 1
        self.waited[eng][key] = val

    def _deps(self, eng, reads, writes):
        for b in reads:
            for tok in b.w.values():
                self._wait(eng, tok)
        for b in writes:
            for tok in b.w.values():
                self._wait(eng, tok)
            for tok in b.r.values():
                self._wait(eng, tok)

    def _mark(self, tok, reads, writes):
        for b in reads:
            b.r[tok[0]] = tok
        for b in writes:
            b.w = {tok[0]: tok}
            b.r = {}

    def op(self, eng, fn, reads=(), writes=(), inc=True):
        if self.rec is not None:
            pr = _Proxy()
            fn(pr)
            self.rec.append(("op", eng, pr.call, tuple(reads), tuple(writes)))
            return
        self._deps(eng, reads, writes)
        ins = fn(self.E[eng])
        self.nins += 1
        if inc:
            self.cnt[eng] += 1
            ins.then_inc(self.sem[eng], 1)
            tok = (eng, self.sem[eng], self.cnt[eng], eng)
        else:
            tok = (eng, self.sem[eng], self.cnt[eng] + 1, eng)
        self._mark(tok, reads, writes)

    def dma(self, q, out, in_, reads=(), writes=()):
        if self.rec is not None:
            self.rec.append(("dma", q, (out, in_), tuple(reads), tuple(writes)))
            return
        self._deps(q, reads, writes)
        slot = self.dq[q][self.dqi[q] % NDSEM]
        self.dqi[q] += 1
        if slot[1] > 0:
            self._wait(q, (slot[2], slot[0], slot[1], "dma"))
        ins = self.E[q].dma_start(out=out, in_=in_)
        self.nins += 1
        slot[1] += 16
        ins.then_inc(slot[0], 16)
        tok = (slot[2], slot[0], slot[1], "dma")
        self._mark(tok, reads, writes)

    def record(self, fn):
        self.rec = []
        fn()
        r, self.rec = self.rec, None
        return r

    def replay(self, items):
        for it in items:
            if it[0] == "op":
                _, eng, call, reads, writes = it
                self.op(eng, lambda e, c=call: getattr(e, c[0])(*c[1], **c[2]), reads=reads, writes=writes)
            else:
                _, q, (out, in_), reads, writes = it
                self.dma(q, out, in_, reads=reads, writes=writes)

    @staticmethod
    def interleave(a, b):
        out = []
        i = j = 0
        while i < len(a) or j < len(b):
            if j >= len(b) or (i < len(a) and i * len(b) <= j * len(a)):
                out.append(a[i])
                i += 1
            else:
                out.append(b[j])
                j += 1
        return out

    def barrier(self, engines=None):
        engs = list(self.E) if engines is None else engines
        for e in engs:
            for f in self.E:
                if self.cnt[f] > 0:
                    self._wait(e, (f, self.sem[f], self.cnt[f], "x"))
            for q in self.dq:
                for slot in self.dq[q]:
                    if slot[1] > 0:
                        self._wait(e, (slot[2], slot[0], slot[1], "dma"))


def _prm_layout(L):
    off = {}
    n = 0

    def add(name, cnt):
        nonlocal n
        off[name] = n
        n += cnt

    add("nm", L * 8)
    add("nf", L * 8)
    add("fn", 8)
    add("modb", L * 48)
    add("pscale", 2 * 4)
    add("convw", 2 * 4 * 31)
    add("convb", 2 * 4)
    add("lng", 2 * 4)
    add("lnb", 2 * 4)
    add("dnconv", 2 * 24 * 4)
    add("onorm", 2)
    add("alog", 2)
    add("dtb", 2)
    add("mlo", 1)
    add("mhi", 1)
    add("rbias", L * 36)
    return off, n


def _fm(v):
    v = np.asarray(v, np.float32)
    lead = v.shape[:-1]
    n = v.shape[-1] // 128
    a = v.reshape(lead + (n, 128))
    a = np.moveaxis(a, -1, 0)
    return np.ascontiguousarray(a.reshape(128, -1))


def _build_prm(inp, L):
    off, n = _prm_layout(L)
    P = np.zeros((128, n), np.float32)

    def put(name, arr):
        arr = np.asarray(arr, np.float32)
        P[:arr.shape[0], off[name]:off[name] + arr.shape[1]] = arr

    put("nm", _fm(inp["norm_mix"][:L]))
    put("nf", _fm(inp["norm_ffn"][:L]))
    put("fn", _fm(inp["final_norm"]))
    put("modb", _fm(inp["mod_b"][:L]))
    put("pscale", _fm(inp["pool_scale"]))
    cw = np.asarray(inp["conv_w"], np.float32)
    cw = cw.reshape(2, 31, 4, 128).transpose(3, 0, 2, 1)
    put("convw", cw.reshape(128, -1))
    put("convb", _fm(inp["conv_b"]))
    put("lng", _fm(inp["conv_ln_g"]))
    put("lnb", _fm(inp["conv_ln_b"]))
    dcw = np.asarray(inp["dn_conv_w"], np.float32)
    dcw = dcw.reshape(2, 4, 24, 128).transpose(3, 0, 2, 1)
    put("dnconv", dcw.reshape(128, -1))
    put("onorm", np.asarray(inp["dn_onorm"], np.float32).T)
    al = np.zeros((16, 2), np.float32)
    al[8:16, :] = np.asarray(inp["dn_a_log"], np.float32).T
    put("alog", al)
    db = np.zeros((16, 2), np.float32)
    db[8:16, :] = np.asarray(inp["dn_dt_bias"], np.float32).T
    put("dtb", db)
    mlo = np.zeros((16, 1), np.float32)
    mlo[0:8] = 1.0
    put("mlo", mlo)
    put("mhi", 1.0 - mlo)
    rb = np.concatenate([np.asarray(inp["moe_b_grp"], np.float32)[:L], np.asarray(inp["moe_b_exp"], np.float32)[:L]], axis=1)
    put("rbias", np.broadcast_to(rb.reshape(1, -1), (128, L * 36)))
    return P, off


def _build_cst():
    c = np.zeros((128, 640), np.float32)
    c[:, 0:128] = np.eye(128, dtype=np.float32)
    c[:, 128:256] = 1.0
    c[127, 256:384] = 1.0
    m = np.arange(128)[:, None]
    cc = np.arange(128)[None, :]
    c[:, 384:512] = np.where(cc > m, 0.0, NEG)
    c[:, 512:640] = np.where(cc >= m, 0.0, NEG)
    return c


def _build_mk():
    mk = np.zeros((128, 7, 2, 128), np.float32)
    r = np.arange(128)[:, None]
    c = np.arange(128)[None, :]
    for k_ in range(7):
        b = 1 << k_
        same = (r // (2 * b)) == (c // (2 * b))
        mk[:, k_, 0, :] = same & ((r % (2 * b)) >= b) & ((c % (2 * b)) < b)
        mk[:, k_, 1, :] = same & ((c % (2 * b)) >= b) & ((r % (2 * b)) < b)
    return np.ascontiguousarray(mk.reshape(128, -1))


def build_program(cfg):
    NSEQ, S, L = cfg["NSEQ"], cfg["S"], cfg["DEPTH"]
    NG = S // GS
    NT = S // 128
    off, NP = _prm_layout(L)
    nc = bass.Bass("TRN2", target_bir_lowering=False)

    def din(name, shape):
        return nc.dram_tensor(name, list(shape), F32, kind="ExternalInput").ap()

    x_d = din("x", [NSEQ * S, D])
    cT_d = din("cT", [128, 8 * NSEQ])
    cst_d = din("cst", [128, 640])
    prm_d = din("prm", [128, NP])
    mk_d = din("mk", [128, 7 * 2 * 128])
    modw_d = din("mod_w", [L, D, 6 * D])
    abin_d = din("ab_w_in", [2, D, 1536])
    poolw_d = din("pool_w", [2, 4, 128, 128])
    about_d = din("ab_w_out", [2, D, D])
    dnin_d = din("dn_w_in_h", [2, D, 4096])
    dnbg_d = din("dn_w_bg", [2, D, 16])
    dnout_d = din("dn_w_out", [2, D, D])
    moer_d = din("moe_w_r", [L, D, 36])
    moeup_d = din("moe_w_up", [L, NEXP, D, 2 * FE])
    moedn_d = din("moe_w_down", [L, NEXP, FE, D])
    out_d = nc.dram_tensor("out", [NSEQ * S, D], F32, kind="ExternalOutput").ap()

    es = ExitStack()
    k = K(nc, es)
    op, dma = k.op, k.dma

    xT = k.sb([128, KD, S], F32, name="xT")
    hnT = k.sb([128, KD, S], BF16, name="hnT")
    cst = k.sb([128, 640], F32, name="cst")
    identb = k.sb([128, 128], BF16, name="identb")
    onesb = k.sb([128, 128], BF16, name="onesb")
    prm = k.sb([128, NP], F32, name="prm")
    cact = k.sb([128, 8 * NSEQ], F32, name="cact")
    modT = k.sb([128, L, 48, NSEQ], F32, name="modT")
    modA = k.sb([128, L, 2, 8, NSEQ], F32, name="modA")
    wpool = [k.sb([128, KD, 512], BF16, name="wp%d" % i) for i in range(2)]
    wpi = [0]
    banks = [k.psb([128, 512], F32, "bank%d" % i) for i in range(7)]
    bankb = k.psb([128, 1024], BF16, "bankb")
    bi = [0]

    def nb():
        b = banks[bi[0] % 7]
        bi[0] += 1
        return b

    def nw():
        w = wpool[wpi[0] % 2]
        wpi[0] += 1
        return w

    ident = cst.t[:, 0:128]
    ones = cst.t[:, 128:256]
    sel127 = cst.t[:, 256:384]
    negm2 = cst.t[:, 384:640]

    def P(name, i=0, n=1, rows=128):
        o = off[name] + i
        return prm.t[0:rows, o:o + n]

    dma("sp", cst.t[:, :], cst_d[:, :], writes=[cst])
    dma("sp", prm.t[:, :], prm_d[:, :], writes=[prm])
    dma("sp", cact.t[:, :], cT_d[:, :], writes=[cact])
    dma("pool", identb.t[:, :], cst_d[:, 0:128], writes=[identb])
    dma("pool", onesb.t[:, :], cst_d[:, 128:256], writes=[onesb])
    op("act", lambda e: e.activation(out=cact.t[:, :], in_=cact.t[:, :], func=AF.Silu), reads=[cact], writes=[cact])

    with ExitStack() as pes:
        mw = [k.sb([128, KD, 512], F32, es=pes, name="mw%d" % i) for i in range(2)]
        mi = 0
        for l in range(L):
            for pc in range(12):
                w = mw[mi % 2]
                mi += 1
                dma("sp", w.t[:, :, :], modw_d[l, :, pc * 512:(pc + 1) * 512].rearrange("(k p) n -> p k n", p=128), writes=[w])
                pb = nb()
                for c4 in range(4):
                    for kk in range(KD):
                        op("pe", lambda e, c4=c4, kk=kk, w=w, pb=pb: e.matmul(
                            pb.t[:, c4 * NSEQ:(c4 + 1) * NSEQ], w.t[:, kk, c4 * 128:(c4 + 1) * 128],
                            cact.t[:, kk * NSEQ:(kk + 1) * NSEQ], start=(kk == 0), stop=(kk == KD - 1)),
                           reads=[w, cact], writes=[pb], inc=(kk == KD - 1))
                for c4 in range(4):
                    ch = pc * 4 + c4
                    op("dve", lambda e, c4=c4, ch=ch, pb=pb, l=l: e.tensor_scalar(
                        out=modT.t[:, l, ch, :], in0=pb.t[:, c4 * NSEQ:(c4 + 1) * NSEQ],
                        scalar1=P("modb", l * 48 + ch), scalar2=None, op0=ALU.add),
                       reads=[pb, prm], writes=[modT])
            for j, (nname, cbase) in enumerate((("nm", 8), ("nf", 32))):
                for kk in range(KD):
                    op("dve", lambda e, j=j, kk=kk, nname=nname, cbase=cbase, l=l: e.tensor_scalar(
                        out=modA.t[:, l, j, kk, :], in0=modT.t[:, l, cbase + kk, :],
                        scalar1=1.0, scalar2=P(nname, l * 8 + kk), op0=ALU.add, op1=ALU.mult),
                       reads=[modT, prm], writes=[modA])
    k.barrier()

    def rms_rstd(pes, g, scale_n):
        cols = slice(g * GS, (g + 1) * GS)
        pb = nb()
        for kk in range(KD):
            sq = scr["sqb"][kk % 2]
            op("act", lambda e, kk=kk, sq=sq: e.activation(out=sq.t[:, :], in_=xT.t[:, kk, cols], func=AF.Square),
               reads=[xT], writes=[sq])
            op("pe", lambda e, kk=kk, sq=sq, pb=pb: e.matmul(pb.t[:, :], ones, sq.t[:, :], start=(kk == 0), stop=(kk == KD - 1)),
               reads=[sq, cst], writes=[pb])
        rs = rsb[g % 2]
        op("act", lambda e: e.activation(out=rs.t[:, :], in_=pb.t[:, :], func=AF.Ln, scale=1.0 / scale_n, bias=EPS), reads=[pb], writes=[rs])
        op("act", lambda e: e.activation(out=rs.t[:, :], in_=rs.t[:, :], func=AF.Exp, scale=-0.5), reads=[rs], writes=[rs])
        return rs

    def make_hn(l, which, sl, g, hn32=None):
        cols = slice(g * GS, (g + 1) * GS)
        rs = rms_rstd(None, g, float(D))
        shbase = 0 if which == 0 else 24
        for kk in range(KD):
            tmp = scr["tmpb"][kk % 2]
            op("dve", lambda e, kk=kk, tmp=tmp: e.tensor_tensor(out=tmp.t[:, :], in0=xT.t[:, kk, cols], in1=rs.t[:, :], op=ALU.mult),
               reads=[xT, rs], writes=[tmp])
            op("act", lambda e, kk=kk, tmp=tmp: e.activation(
                out=hnT.t[:, kk, cols], in_=tmp.t[:, :], func=AF.Identity,
                bias=modT.t[:, l, shbase + kk, sl:sl + 1], scale=modA.t[:, l, which, kk, sl:sl + 1]),
               reads=[tmp, modT, modA], writes=[hnT])
            if hn32 is not None:
                op("act", lambda e, kk=kk, tmp=tmp: e.activation(
                    out=hn32.t[:, kk, :], in_=tmp.t[:, :], func=AF.Identity,
                    bias=modT.t[:, l, shbase + kk, sl:sl + 1], scale=modA.t[:, l, which, kk, sl:sl + 1]),
                   reads=[tmp, modT, modA], writes=[hn32])

    def load_w(src_ap, ncols=512):
        w = nw()
        dma("pool", w.t[:, :, 0:ncols], src_ap.rearrange("(k p) n -> p k n", p=128), writes=[w])
        return w

    def proj(w, wc0, g, pb, pcols=None):
        cols = slice(g * GS, (g + 1) * GS)
        for kk in range(KD):
            op("pe", lambda e, kk=kk: e.matmul(pb.t[:, :], w.t[:, kk, wc0:wc0 + 128], hnT.t[:, kk, cols],
                                               start=(kk == 0), stop=(kk == KD - 1)),
               reads=[w, hnT], writes=[pb], inc=(kk == KD - 1))

    def resid_add(pb, l, gate_chunk_base, kk, sl, cols):
        op("dve", lambda e: e.scalar_tensor_tensor(
            out=xT.t[:, kk, cols], in0=pb.t[:, :], scalar=modT.t[:, l, gate_chunk_base + kk, sl:sl + 1],
            in1=xT.t[:, kk, cols], op0=ALU.mult, op1=ALU.add), reads=[pb, modT, xT], writes=[xT])

    rsb = [k.sb([128, GS], F32, name="rs%d" % i) for i in range(2)]
    scr = {}

    def alloc_scr(es_):
        scr["sqb"] = [k.sb([128, GS], F32, es=es_, name="sq%d" % i) for i in range(2)]
        scr["tmpb"] = [k.sb([128, GS], F32, es=es_, name="tmp%d" % i) for i in range(2)]

    for sl in range(NSEQ):
        with ExitStack() as pes:
            xin = [k.sb([128, D], F32, es=pes, name="xin%d" % i) for i in range(2)]
            for i in range(NT):
                xi = xin[i % 2]
                dma("sp", xi.t[:, :], x_d[sl * S + i * 128: sl * S + (i + 1) * 128, :], writes=[xi])
                for half in range(2):
                    pb = nb()
                    for c4 in range(4):
                        kk = half * 4 + c4
                        op("pe", lambda e, kk=kk, c4=c4, pb=pb, xi=xi: e.transpose(
                            pb.t[:, c4 * 128:(c4 + 1) * 128], xi.t[:, kk * 128:(kk + 1) * 128], ident),
                           reads=[xi, cst], writes=[pb], inc=(c4 == 3))
                    eng = "act" if half == 0 else "dve"
                    if eng == "act":
                        op("act", lambda e, half=half, pb=pb, i=i: e.activation(
                            out=xT.t[:, half * 4:(half + 1) * 4, i * 128:(i + 1) * 128],
                            in_=pb.t[:, :].rearrange("p (a b) -> p a b", a=4), func=AF.Copy), reads=[pb], writes=[xT])
                    else:
                        op("dve", lambda e, half=half, pb=pb, i=i: e.tensor_copy(
                            out=xT.t[:, half * 4:(half + 1) * 4, i * 128:(i + 1) * 128],
                            in_=pb.t[:, :].rearrange("p (a b) -> p a b", a=4)), reads=[pb], writes=[xT])
        k.barrier()

        for l in range(L):
            li = l // 2
            with ExitStack() as hs:
                alloc_scr(hs)
                for g in range(NG):
                    make_hn(l, 0, sl, g)
                k.barrier()
            if l % 2 == 0:
                even_mixer(k, nc, locals())
            else:
                odd_mixer(k, nc, locals())
            k.barrier()
            moe_layer(k, nc, locals())
            k.barrier()

        with ExitStack() as pes:
            alloc_scr(pes)
            xn = [k.sb([128, KD, GS], F32, es=pes, name="xn%d" % i) for i in range(2)]
            ost = [k.sb([128, D], F32, es=pes, name="ost%d" % i) for i in range(2)]
            for g in range(NG):
                cols = slice(g * GS, (g + 1) * GS)
                rs = rms_rstd(None, g, float(D))
                xg = xn[g % 2]
                for kk in range(KD):
                    op("dve", lambda e, kk=kk: e.scalar_tensor_tensor(
                        out=xg.t[:, kk, :], in0=xT.t[:, kk, cols], scalar=P("fn", kk), in1=rs.t[:, :],
                        op0=ALU.mult, op1=ALU.mult), reads=[xT, prm, rs], writes=[xg])
                for ti in range(GS // 128):
                    i = g * (GS // 128) + ti
                    o = ost[i % 2]
                    for half in range(2):
                        pb = nb()
                        for c4 in range(4):
                            kk = half * 4 + c4
                            op("pe", lambda e, kk=kk, c4=c4, pb=pb: e.transpose(
                                pb.t[:, c4 * 128:(c4 + 1) * 128], xg.t[:, kk, ti * 128:(ti + 1) * 128], ident),
                               reads=[xg, cst], writes=[pb], inc=(c4 == 3))
                        if half == 0:
                            op("act", lambda e, pb=pb, o=o: e.activation(out=o.t[:, 0:512], in_=pb.t[:, :], func=AF.Copy),
                               reads=[pb], writes=[o])
                        else:
                            op("dve", lambda e, pb=pb, o=o: e.tensor_copy(out=o.t[:, 512:1024], in_=pb.t[:, :]),
                               reads=[pb], writes=[o])
                    dma("sp", out_d[sl * S + i * 128: sl * S + (i + 1) * 128, :], o.t[:, :], reads=[o])
        k.barrier()

    k.barrier(["sp"])
    es.close()
    return nc, k


def even_mixer(k, nc, env):
    op, dma = k.op, k.dma
    xT, hnT, modT, prm, cst = env["xT"], env["hnT"], env["modT"], env["prm"], env["cst"]
    nb, load_w, proj, resid_add, P = env["nb"], env["load_w"], env["proj"], env["resid_add"], env["P"]
    l, li, sl, S, NG = env["l"], env["li"], env["sl"], env["S"], env["NG"]
    abin_d, poolw_d, about_d = env["abin_d"], env["poolw_d"], env["about_d"]
    ones = env["ones"]
    rsb = env["rsb"]
    WIN = (2, 4, 8, 16)
    with ExitStack() as pes:
        env["alloc_scr"](pes)
        sqb, tmpb = env["scr"]["sqb"], env["scr"]["tmpb"]
        ua = [k.sb([128, 4, 16 + GS], F32, es=pes, name="ua%d" % i) for i in range(2)]
        tl = [k.sb([128, 16 + GS], F32, es=pes, name="tl%d" % i) for i in range(2)]
        glu = [k.sb([128, 4, 30 + GS], F32, es=pes, name="glu%d" % i) for i in range(2)]
        accs = [k.sb([128, 4, GS], F32, es=pes, name="cacc%d" % i) for i in range(2)]
        pooleds = [k.sb([128, 4, GS], BF16, es=pes, name="pooled%d" % i) for i in range(2)]
        pw = k.sb([128, 4, 128], BF16, es=pes, name="poolw")
        sig = [k.sb([128, GS], F32, es=pes, name="sig0")] * 2
        cts = [k.sb([128, 8, GS], BF16, es=pes, name="cat0")]
        mu, rstd = rsb[0], rsb[1]
        dma("pool", pw.t[:, :, :], poolw_d[li].rearrange("j c d -> c j d"), writes=[pw])
        def stageA(g):
            cols = slice(g * GS, (g + 1) * GS)
            u_, g_ = ua[g % 2], glu[g % 2]
            acc = accs[g % 2]
            pooled = pooleds[g % 2]
            if g == 0:
                op("pool", lambda e: e.memset(u_.t[:, :, 0:16], 0.0), writes=[u_])
                op("pool", lambda e: e.memset(g_.t[:, :, 0:30], 0.0), writes=[g_])
            else:
                up, gp = ua[(g - 1) % 2], glu[(g - 1) % 2]
                op("pool", lambda e: e.tensor_copy(out=u_.t[:, :, 0:16], in_=up.t[:, :, GS:GS + 16]), reads=[up], writes=[u_])
                op("pool", lambda e: e.tensor_copy(out=g_.t[:, :, 0:30], in_=gp.t[:, :, GS:GS + 30]), reads=[gp], writes=[g_])
            w0 = load_w(abin_d[li, :, 0:512])
            for j in range(4):
                pb = nb()
                proj(w0, j * 128, g, pb)
                op("act", lambda e, j=j, pb=pb: e.activation(out=u_.t[:, j, 16:16 + GS], in_=pb.t[:, :], func=AF.Copy), reads=[pb], writes=[u_])
            wv = load_w(abin_d[li, :, 512:1024])
            wg = load_w(abin_d[li, :, 1024:1536])
            for j in range(4):
                pg = nb()
                proj(wg, j * 128, g, pg)
                sg = sig[j % 2]
                op("act", lambda e, pg=pg, sg=sg: e.activation(out=sg.t[:, :], in_=pg.t[:, :], func=AF.Sigmoid), reads=[pg], writes=[sg])
                pv = nb()
                proj(wv, j * 128, g, pv)
                op("dve", lambda e, j=j, pv=pv, sg=sg: e.tensor_tensor(out=g_.t[:, j, 30:30 + GS], in0=pv.t[:, :], in1=sg.t[:, :], op=ALU.mult),
                   reads=[pv, sg], writes=[g_])
            for j in range(4):
                w_ = WIN[j]
                prev_ap = u_.t[:, j, :]
                prev_buf = u_
                for lev in range(j + 1):
                    sh = 1 << lev
                    c0 = (2 << lev) - 1
                    dst = tl[lev % 2]
                    op("dve", lambda e, dst=dst, prev_ap=prev_ap, sh=sh, c0=c0: e.tensor_tensor(
                        out=dst.t[:, c0:16 + GS], in0=prev_ap[:, c0:16 + GS], in1=prev_ap[:, c0 - sh:16 + GS - sh], op=ALU.add),
                       reads=[prev_buf], writes=[dst])
                    prev_ap = dst.t[:, :]
                    prev_buf = dst
                if g == 0:
                    for t in range(w_ - 1):
                        op("dve", lambda e, t=t, prev_ap=prev_ap, w_=w_: e.tensor_scalar_mul(
                            out=prev_ap[:, 16 + t:17 + t], in0=prev_ap[:, 16 + t:17 + t], scalar1=float(w_) / (t + 1)),
                           reads=[prev_buf], writes=[prev_buf])
                op("dve", lambda e, j=j, prev_ap=prev_ap, w_=w_: e.scalar_tensor_tensor(
                    out=pooled.t[:, j, :], in0=prev_ap[:, 16:16 + GS], scalar=1.0 / w_, in1=u_.t[:, j, 16:16 + GS],
                    op0=ALU.mult, op1=ALU.subtract), reads=[prev_buf, u_], writes=[pooled])
            for j in range(4):
                eng = "dve"
                for tap in range(31):
                    wcol = P("convw", (li * 4 + j) * 31 + tap)
                    srcv = g_.t[:, j, tap:tap + GS]
                    if tap == 0:
                        op(eng, lambda e, j=j, srcv=srcv, wcol=wcol: e.tensor_scalar(
                            out=acc.t[:, j, :], in0=srcv, scalar1=wcol, scalar2=P("convb", li * 4 + j),
                            op0=ALU.mult, op1=ALU.add), reads=[g_, prm], writes=[acc])
                    else:
                        op(eng, lambda e, j=j, srcv=srcv, wcol=wcol: e.scalar_tensor_tensor(
                            out=acc.t[:, j, :], in0=srcv, scalar=wcol, in1=acc.t[:, j, :],
                            op0=ALU.mult, op1=ALU.add), reads=[g_, prm, acc], writes=[acc])
        def stageB(g):
            cols = slice(g * GS, (g + 1) * GS)
            acc, ct = accs[g % 2], cts[0]
            pooled = pooleds[g % 2]
            for j in range(4):
                pb = nb()
                op("pe", lambda e, j=j, pb=pb: e.matmul(pb.t[:, :], pw.t[:, j, :], pooled.t[:, j, :], start=True, stop=True),
                   reads=[pw, pooled], writes=[pb])
                op("act", lambda e, j=j, pb=pb: e.activation(out=ct.t[:, j, :], in_=pb.t[:, :], func=AF.Copy,
                                                             scale=P("pscale", li * 4 + j)), reads=[pb, prm], writes=[ct])
            pm = nb()
            pq = nb()
            for j in range(4):
                s2 = sqb[j % 2]
                op("act", lambda e, j=j, s2=s2: e.activation(out=s2.t[:, :], in_=acc.t[:, j, :], func=AF.Square), reads=[acc], writes=[s2])
                op("pe", lambda e, j=j: e.matmul(pm.t[:, :], ones, acc.t[:, j, :], start=(j == 0), stop=(j == 3)),
                   reads=[acc, cst], writes=[pm])
                op("pe", lambda e, s2=s2, j=j: e.matmul(pq.t[:, :], ones, s2.t[:, :], start=(j == 0), stop=(j == 3)),
                   reads=[s2, cst], writes=[pq])
            op("act", lambda e: e.activation(out=mu.t[:, :], in_=pm.t[:, :], func=AF.Copy, scale=1.0 / 512), reads=[pm], writes=[mu])
            op("dve", lambda e: e.tensor_tensor(out=rstd.t[:, :], in0=mu.t[:, :], in1=mu.t[:, :], op=ALU.mult), reads=[mu], writes=[rstd])
            op("dve", lambda e: e.scalar_tensor_tensor(out=rstd.t[:, :], in0=pq.t[:, :], scalar=1.0 / 512, in1=rstd.t[:, :],
                                                       op0=ALU.mult, op1=ALU.subtract), reads=[pq, rstd], writes=[rstd])
            op("act", lambda e: e.activation(out=rstd.t[:, :], in_=rstd.t[:, :], func=AF.Ln, bias=EPS), reads=[rstd], writes=[rstd])
            op("act", lambda e: e.activation(out=rstd.t[:, :], in_=rstd.t[:, :], func=AF.Exp, scale=-0.5), reads=[rstd], writes=[rstd])
            for j in range(4):
                t_ = tmpb[j % 2]
                op("dve", lambda e, j=j, t_=t_: e.tensor_tensor(out=t_.t[:, :], in0=acc.t[:, j, :], in1=mu.t[:, :], op=ALU.subtract),
                   reads=[acc, mu], writes=[t_])
                op("dve", lambda e, t_=t_: e.tensor_tensor(out=t_.t[:, :], in0=t_.t[:, :], in1=rstd.t[:, :], op=ALU.mult),
                   reads=[t_, rstd], writes=[t_])
                op("act", lambda e, j=j, t_=t_: e.activation(
                    out=ct.t[:, 4 + j, :], in_=t_.t[:, :], func=AF.Silu, bias=P("lnb", li * 4 + j), scale=P("lng", li * 4 + j)),
                   reads=[t_, prm], writes=[ct])
            wo = [load_w(about_d[li, :, h * 512:(h + 1) * 512]) for h in range(2)]
            for oc in range(8):
                pb = nb()
                w = wo[oc // 4]
                for kk in range(8):
                    op("pe", lambda e, kk=kk, oc=oc, w=w, pb=pb: e.matmul(
                        pb.t[:, :], w.t[:, kk, (oc % 4) * 128:(oc % 4 + 1) * 128], ct.t[:, kk, :], start=(kk == 0), stop=(kk == 7)),
                       reads=[w, ct], writes=[pb], inc=(kk == 7))
                resid_add(pb, l, 16, oc, sl, cols)

        stageA(0)
        for g in range(NG):
            if g + 1 < NG:
                stageA(g + 1)
            stageB(g)


def odd_mixer(k, nc, env):
    op, dma = k.op, k.dma
    xT, hnT, modT, prm, cst = env["xT"], env["hnT"], env["modT"], env["prm"], env["cst"]
    nb, load_w, proj, resid_add, P = env["nb"], env["load_w"], env["proj"], env["resid_add"], env["P"]
    l, li, sl, S, NG, NT = env["l"], env["li"], env["sl"], env["S"], env["NG"], env["NT"]
    dnin_d, dnbg_d, dnout_d = env["dnin_d"], env["dnbg_d"], env["dnout_d"]
    ones, ident, sel127, negm2, identb, bankb = env["ones"], env["ident"], env["sel127"], env["negm2"], env["identb"], env["bankb"]
    onesb = env["onesb"]
    TG = GS // 128
    with ExitStack() as pes:
        def sbt(shape, dt, name):
            return k.sb(shape, dt, es=pes, name=name)
        BG = sbt([16, S], F32, "BG")
        TM = sbt([128, NT, 16], F32, "TM")
        egc = sbt([128, NT, 16], F32, "egc")
        bexp = sbt([128, NT, 8], F32, "bexp")
        kdec = sbt([128, NT, 16], F32, "kdec")
        gl = sbt([128, NT, 16], F32, "gl")
        dl = gl
        wbg = sbt([128, KD, 16], BF16, "wbg")
        nea = sbt([16, 1], F32, "nea")
        res2 = ExitStack()
        r1 = [k.sb([16, GS], F32, es=res2, name="r1_%d" % i) for i in range(2)]
        r2 = [k.sb([16, GS], F32, es=res2, name="r2_%d" % i) for i in range(2)]
        dma("pool", wbg.t[:, :, :], dnbg_d[li].rearrange("(k p) n -> p k n", p=128), writes=[wbg])
        op("act", lambda e: e.activation(out=nea.t[:, :], in_=P("alog", li, rows=16), func=AF.Exp), reads=[prm], writes=[nea])
        op("dve", lambda e: e.tensor_scalar_mul(out=nea.t[:, :], in0=nea.t[:, :], scalar1=-1.0), reads=[nea], writes=[nea])
        for g in range(NG):
            cols = slice(g * GS, (g + 1) * GS)
            pb = nb()
            for kk in range(KD):
                op("pe", lambda e, kk=kk, pb=pb: e.matmul(pb.t[0:16, :], wbg.t[:, kk, :], hnT.t[:, kk, cols],
                                                        start=(kk == 0), stop=(kk == KD - 1)),
                   reads=[wbg, hnT], writes=[pb], inc=(kk == KD - 1))
            a, b = r1[0], r1[1]
            c_, d_ = r2[0], r2[1]
            op("act", lambda e, pb=pb: e.activation(out=a.t[:, :], in_=pb.t[0:16, :], func=AF.Sigmoid), reads=[pb], writes=[a])
            op("act", lambda e, pb=pb: e.activation(out=b.t[:, :], in_=pb.t[0:16, :], func=AF.Exp, bias=P("dtb", li, rows=16)),
               reads=[pb, prm], writes=[b])
            op("act", lambda e: e.activation(out=b.t[:, :], in_=b.t[:, :], func=AF.Ln, bias=1.0), reads=[b], writes=[b])
            op("dve", lambda e: e.tensor_scalar_mul(out=b.t[:, :], in0=b.t[:, :], scalar1=nea.t[:, 0:1]), reads=[b, nea], writes=[b])
            src, dst = b, c_
            for lev in range(7):
                sh = 1 << lev
                sv = src.t[:, :].rearrange("p (a t) -> p a t", t=128)
                dv = dst.t[:, :].rearrange("p (a t) -> p a t", t=128)
                op("dve", lambda e, sv=sv, dv=dv, sh=sh: e.tensor_tensor(out=dv[:, :, sh:128], in0=sv[:, :, sh:128],
                                                                         in1=sv[:, :, 0:128 - sh], op=ALU.add),
                   reads=[src], writes=[dst])
                op("dve", lambda e, sv=sv, dv=dv, sh=sh: e.tensor_copy(out=dv[:, :, 0:sh], in_=sv[:, :, 0:sh]),
                   reads=[src], writes=[dst])
                src, dst = dst, src
            op("dve", lambda e: e.tensor_scalar_mul(out=d_.t[:, :], in0=a.t[:, :], scalar1=P("mlo", rows=16)), reads=[a, prm], writes=[d_])
            op("dve", lambda e, src=src: e.scalar_tensor_tensor(out=BG.t[:, cols], in0=src.t[:, :], scalar=P("mhi", rows=16), in1=d_.t[:, :],
                                                                op0=ALU.mult, op1=ALU.add), reads=[src, d_, prm], writes=[BG])
        pb = nb()
        for i in range(NT):
            op("pe", lambda e, i=i, pb=pb: e.transpose(pb.t[:, i * 16:(i + 1) * 16], BG.t[:, i * 128:(i + 1) * 128], ident[0:16, 0:16]),
               reads=[BG, cst], writes=[pb], inc=(i == NT - 1))
        op("act", lambda e, pb=pb: e.activation(out=TM.t[:, :, :], in_=pb.t[:, 0:NT * 16].rearrange("p (a b) -> p a b", b=16), func=AF.Copy),
           reads=[pb], writes=[TM])
        pb2 = nb()
        op("pe", lambda e: e.matmul(pb2.t[:, 0:NT * 16], sel127, TM.t[:, :, :].rearrange("p a b -> p (a b)"), start=True, stop=True),
           reads=[TM, cst], writes=[pb2])
        op("act", lambda e: e.activation(out=gl.t[:, :, :], in_=pb2.t[:, 0:NT * 16].rearrange("p (a b) -> p a b", b=16), func=AF.Copy),
           reads=[pb2], writes=[gl])
        op("act", lambda e: e.activation(out=egc.t[:, :, :], in_=TM.t[:, :, :], func=AF.Exp), reads=[TM], writes=[egc])
        op("dve", lambda e: e.tensor_tensor(out=bexp.t[:, :, :], in0=TM.t[:, :, 0:8], in1=egc.t[:, :, 8:16], op=ALU.mult),
           reads=[TM, egc], writes=[bexp])
        op("dve", lambda e: e.tensor_tensor(out=kdec.t[:, :, :], in0=gl.t[:, :, :], in1=TM.t[:, :, :], op=ALU.subtract),
           reads=[gl, TM], writes=[kdec])
        op("act", lambda e: e.activation(out=kdec.t[:, :, :], in_=kdec.t[:, :, :], func=AF.Exp), reads=[kdec], writes=[kdec])
        op("act", lambda e: e.activation(out=dl.t[:, :, :], in_=gl.t[:, :, :], func=AF.Exp), reads=[gl], writes=[dl])

        k.barrier()
        res2.close()
        uraw = [sbt([128, 3 + GS], F32, "uraw%d" % q) for q in range(3)]
        cacc = [sbt([128, GS], F32, "cacc%d" % q) for q in range(3)]
        rn = env["rsb"]
        qn = sbt([128, GS], F32, "qn")
        kn = sbt([128, GS], F32, "kn")
        rowm = [sbt([16, GS], F32, "rowm0")] * 2
        beta_b = sbt([128, GS], F32, "beta_b")
        gc_b = sbt([128, GS], F32, "gc_b")
        eg_b = beta_b
        kT = sbt([128, GS], BF16, "kT")
        nkbT = sbt([128, GS], BF16, "nkbT")
        nkT = sbt([128, GS], BF16, "nkT")
        qT = sbt([128, GS], BF16, "qT")
        qgTs = [sbt([128, GS], BF16, "qgT%d" % i) for i in range(2)]
        zss = [sbt([128, GS], BF16, "zs%d" % i) for i in range(2)]
        tmp1 = sbt([128, TG, 128], F32, "dtmp1")
        tmp2 = sbt([128, TG, 2, 128], F32, "dtmp2")
        X2s = [sbt([128, TG, 2, 128], BF16, "X2_%d" % i) for i in range(2)]
        NLb = sbt([128, TG, 2, 128], BF16, "NLb")
        NCks = [sbt([128, 2, 2, 128], BF16, "NCk%d" % i) for i in range(2)]
        Ybs = [sbt([128, 2, 2, 128], BF16, "Yb%d" % i) for i in range(2)]
        Tbs = [[sbt([128, 2, 2, 128], BF16, "Tb%d_%d" % (i, j)) for j in range(2)] for i in range(2)]
        mkb = sbt([128, 7, 2, 128], BF16, "mkb")
        dma("pool", mkb.t[:, :, :, :], env["mk_d"][:, :].rearrange("p (a b c) -> p a b c", a=7, b=2), writes=[mkb])
        kbgs = [sbt([128, TG, 128], BF16, "kbg%d" % i) for i in range(2)]
        kds = [sbt([128, TG, 128], BF16, "kd%d" % i) for i in range(2)]
        vbs = [sbt([128, TG, 128], BF16, "vb%d" % i) for i in range(2)]
        nwT = sbt([128, TG, 128], BF16, "nwT")
        vnew = [sbt([128, 128], BF16, "vnew%d" % i) for i in range(2)]
        S32 = sbt([128, 128], F32, "S32")
        Sbf = sbt([128, 128], BF16, "Sbf")
        o32 = sbt([128, GS], F32, "o32")
        og = o32
        og2 = sbt([128, GS], BF16, "og2")
        wo = sbt([128, D], BF16, "wo_h")

        U = 8 * NG

        banks_ = env["banks"]
        bctr = [0, 0]

        def nb1():
            bctr[0] += 1
            return banks_[(bctr[0] - 1) % 4]

        def nb2():
            bctr[1] += 1
            return banks_[4 + (bctr[1] - 1) % 3]

        def stage1(u):
            h, g = divmod(u, NG)
            p = u % 2
            X2, kbg, kd, vb, qgT, zs = X2s[p], kbgs[p], kds[p], vbs[p], qgTs[p], zss[p]
            if g == 0:
                wcur[0] = load_w(dnin_d[li, :, h * 512:(h + 1) * 512])
            w = wcur[0]
            cols = slice(g * GS, (g + 1) * GS)
            for q in range(3):
                ur = uraw[q]
                if g == 0:
                    op("pool", lambda e, ur=ur: e.memset(ur.t[:, 0:3], 0.0), writes=[ur])
                else:
                    op("pool", lambda e, ur=ur: e.tensor_copy(out=ur.t[:, 0:3], in_=ur.t[:, GS:GS + 3]), reads=[ur], writes=[ur])
                pb = nb1()
                proj(w, q * 128, g, pb)
                op("act", lambda e, ur=ur, pb=pb: e.activation(out=ur.t[:, 3:3 + GS], in_=pb.t[:, :], func=AF.Copy), reads=[pb], writes=[ur])
            pb = nb1()
            proj(w, 3 * 128, g, pb)
            op("act", lambda e, pb=pb: e.activation(out=zs.t[:, :], in_=pb.t[:, :], func=AF.Silu), reads=[pb], writes=[zs])
            for q in range(3):
                ur = uraw[q]
                ca = cacc[q]
                cb = (li * 24 + q * 8 + h) * 4
                op("dve", lambda e, ur=ur, ca=ca, cb=cb: e.tensor_scalar_mul(out=ca.t[:, :], in0=ur.t[:, 3:3 + GS], scalar1=P("dnconv", cb + 3)),
                   reads=[ur, prm], writes=[ca])
                for tap in range(3):
                    op("dve", lambda e, ur=ur, ca=ca, cb=cb, tap=tap: e.scalar_tensor_tensor(
                        out=ca.t[:, :], in0=ur.t[:, tap:tap + GS], scalar=P("dnconv", cb + tap), in1=ca.t[:, :],
                        op0=ALU.mult, op1=ALU.add), reads=[ur, prm, ca], writes=[ca])
                op("act", lambda e, ca=ca: e.activation(out=ca.t[:, :], in_=ca.t[:, :], func=AF.Silu), reads=[ca], writes=[ca])
            for q in range(2):
                ca = cacc[q]
                sqs = (qT, qgT)[q]
                op("act", lambda e, ca=ca, sqs=sqs: e.activation(out=sqs.t[:, :], in_=ca.t[:, :], func=AF.Square), reads=[ca], writes=[sqs])
                pb = nb1()
                op("pe", lambda e, pb=pb, sqs=sqs: e.matmul(pb.t[:, :], onesb.t[:, :], sqs.t[:, :], start=True, stop=True),
                   reads=[sqs, onesb], writes=[pb])
                op("act", lambda e, pb=pb: e.activation(out=rn[0].t[:, :], in_=pb.t[:, :], func=AF.Ln, bias=EPS), reads=[pb], writes=[rn[0]])
                op("act", lambda e: e.activation(out=rn[0].t[:, :], in_=rn[0].t[:, :], func=AF.Exp, scale=-0.5), reads=[rn[0]], writes=[rn[0]])
                if q == 0:
                    op("dve", lambda e: e.scalar_tensor_tensor(out=qn.t[:, :], in0=cacc[0].t[:, :], scalar=128.0 ** -0.5, in1=rn[0].t[:, :],
                                                               op0=ALU.mult, op1=ALU.mult), reads=[cacc[0], rn[0]], writes=[qn])
                else:
                    op("dve", lambda e: e.tensor_tensor(out=kn.t[:, :], in0=cacc[1].t[:, :], in1=rn[0].t[:, :], op=ALU.mult),
                       reads=[cacc[1], rn[0]], writes=[kn])
            for qi, (row, dstb) in enumerate(((h, beta_b), (8 + h, gc_b))):
                rm = rowm[qi]
                op("dve", lambda e, rm=rm, row=row: e.tensor_scalar_mul(out=rm.t[:, :], in0=BG.t[:, cols], scalar1=ident[0:16, row:row + 1]),
                   reads=[BG, cst], writes=[rm])
                pb = nb1()
                op("pe", lambda e, rm=rm, pb=pb: e.matmul(pb.t[:, :], ones[0:16, :], rm.t[:, :], start=True, stop=True),
                   reads=[rm, cst], writes=[pb])
                op("act", lambda e, pb=pb, dstb=dstb: e.activation(out=dstb.t[:, :], in_=pb.t[:, :], func=AF.Copy), reads=[pb], writes=[dstb])
            op("act", lambda e: e.activation(out=kT.t[:, :], in_=kn.t[:, :], func=AF.Copy), reads=[kn], writes=[kT])
            op("act", lambda e: e.activation(out=nkT.t[:, :], in_=kn.t[:, :], func=AF.Copy, scale=-1.0), reads=[kn], writes=[nkT])
            op("pool", lambda e: e.tensor_tensor(out=nkbT.t[:, :], in0=kn.t[:, :], in1=beta_b.t[:, :], op=ALU.mult),
               reads=[kn, beta_b], writes=[nkbT])
            op("act", lambda e: e.activation(out=eg_b.t[:, :], in_=gc_b.t[:, :], func=AF.Exp), reads=[gc_b], writes=[eg_b])
            op("act", lambda e: e.activation(out=qT.t[:, :], in_=qn.t[:, :], func=AF.Copy), reads=[qn], writes=[qT])
            op("pool", lambda e: e.tensor_tensor(out=qgT.t[:, :], in0=qn.t[:, :], in1=eg_b.t[:, :], op=ALU.mult),
               reads=[qn, eg_b], writes=[qgT])
            pk = nb1()
            pv = nb1()
            for t in range(TG):
                tc_ = slice(t * 128, (t + 1) * 128)
                op("pe", lambda e, t=t, tc_=tc_: e.transpose(pk.t[:, tc_], kn.t[:, tc_], ident), reads=[kn, cst], writes=[pk], inc=(t == TG - 1))
            for t in range(TG):
                tc_ = slice(t * 128, (t + 1) * 128)
                op("pe", lambda e, t=t, tc_=tc_: e.transpose(pv.t[:, tc_], cacc[2].t[:, tc_], ident), reads=[cacc[2], cst], writes=[pv], inc=(t == TG - 1))
            pm = [nb1(), nb1()]
            for t in range(TG):
                ti = g * TG + t
                tc_ = slice(t * 128, (t + 1) * 128)
                op("act", lambda e, t=t, ti=ti, tc_=tc_: e.activation(out=kbg.t[:, t, :], in_=pk.t[:, tc_], func=AF.Copy, scale=bexp.t[:, ti, h:h + 1]),
                   reads=[pk, bexp], writes=[kbg])
                op("act", lambda e, t=t, ti=ti, tc_=tc_: e.activation(out=kd.t[:, t, :], in_=pk.t[:, tc_], func=AF.Copy, scale=kdec.t[:, ti, 8 + h:9 + h]),
                   reads=[pk, kdec], writes=[kd])
                op("act", lambda e, t=t, ti=ti, tc_=tc_: e.activation(out=vb.t[:, t, :], in_=pv.t[:, tc_], func=AF.Copy, scale=TM.t[:, ti, h:h + 1]),
                   reads=[pv, TM], writes=[vb])
                op("dve", lambda e, t=t, ti=ti, tc_=tc_: e.tensor_scalar(out=tmp1.t[:, t, :], in0=gc_b.t[:, tc_], scalar1=TM.t[:, ti, 8 + h:9 + h],
                                                                         scalar2=0.0, op0=ALU.subtract, op1=ALU.min), reads=[gc_b, TM], writes=[tmp1])
                op("dve", lambda e, t=t: e.tensor_tensor(out=tmp2.t[:, t, :, :], in0=tmp1.t[:, t, :].unsqueeze(1).to_broadcast([128, 2, 128]),
                                                          in1=negm2.rearrange("p (a b) -> p a b", a=2), op=ALU.add), reads=[tmp1, cst], writes=[tmp2])
                pmm = pm[t // 2]
                o0 = (t % 2) * 256
                op("pe", lambda e, tc_=tc_, pmm=pmm, o0=o0: e.matmul(pmm.t[:, o0:o0 + 128], nkT.t[:, tc_], nkbT.t[:, tc_], start=True, stop=True),
                   reads=[nkT, nkbT], writes=[pmm], inc=False)
                op("pe", lambda e, tc_=tc_, pmm=pmm, o0=o0: e.matmul(pmm.t[:, o0 + 128:o0 + 256], kT.t[:, tc_], qT.t[:, tc_], start=True, stop=True),
                   reads=[kT, qT], writes=[pmm])
            op("act", lambda e: e.activation(out=tmp2.t[:, :, :, :], in_=tmp2.t[:, :, :, :], func=AF.Exp), reads=[tmp2], writes=[tmp2])
            for hf in range(2):
                op("dve", lambda e, hf=hf: e.tensor_tensor(
                    out=X2.t[:, 2 * hf:2 * hf + 2, :, :], in0=pm[hf].t[:, :].rearrange("p (a b c) -> p a b c", a=2, b=2),
                    in1=tmp2.t[:, 2 * hf:2 * hf + 2, :, :], op=ALU.mult), reads=[pm[hf], tmp2], writes=[X2])

        def stage2(u):
            h, g = divmod(u, NG)
            p = u % 2
            X2, kbg, kd, vb, qgT, zs = X2s[p], kbgs[p], kds[p], vbs[p], qgTs[p], zss[p]
            cols = slice(g * GS, (g + 1) * GS)
            if g == 0:
                dma("pool", wo.t[:, :], dnout_d[li, h * 128:(h + 1) * 128, :], writes=[wo])
                op("dve", lambda e: e.memset(S32.t[:, :], 0.0), writes=[S32])
                op("dve", lambda e: e.memset(Sbf.t[:, :], 0.0), writes=[Sbf])
            for t in range(TG):
                op("pe", lambda e, t=t: e.transpose(bankb.t[:, t * 128:(t + 1) * 128], X2.t[:, t, 0, :], identb.t[:, :]),
                   reads=[X2, identb], writes=[bankb], inc=(t == TG - 1))
            op("act", lambda e: e.activation(out=NLb.t[:, :, 0, :], in_=bankb.t[:, 0:TG * 128].rearrange("p (a b) -> p a b", a=TG), func=AF.Copy),
               reads=[bankb], writes=[NLb])
            op("act", lambda e: e.activation(out=NLb.t[:, :, 1, :], in_=X2.t[:, :, 0, :], func=AF.Copy), reads=[X2], writes=[NLb])
            for hf in range(2):
                op("dve", lambda e, hf=hf: e.tensor_tensor(out=NCks[hf].t[:, :, :, :], in0=NLb.t[:, 2 * hf:2 * hf + 2, :, :],
                                                           in1=mkb.t[:, 0, :, :].unsqueeze(1).to_broadcast([128, 2, 2, 128]), op=ALU.mult),
                   reads=[NLb, mkb], writes=[NCks[hf]])
            Tc = Tbs[0]
            for hf in range(2):
                op("pool", lambda e, hf=hf, Tc=Tc: e.tensor_tensor(out=Tc[hf].t[:, :, :, :], in0=NCks[hf].t[:, :, :, :],
                                                                   in1=identb.t[:, :].unsqueeze(1).unsqueeze(1).to_broadcast([128, 2, 2, 128]), op=ALU.add),
                   reads=[NCks[hf], identb], writes=[Tc[hf]])
            for lev in range(1, 7):
                last = (lev == 6)
                for hf in range(2):
                    op("dve", lambda e, hf=hf, lev=lev: e.tensor_tensor(out=NCks[hf].t[:, :, :, :], in0=NLb.t[:, 2 * hf:2 * hf + 2, :, :],
                                                                        in1=mkb.t[:, lev, :, :].unsqueeze(1).to_broadcast([128, 2, 2, 128]), op=ALU.mult),
                       reads=[NLb, mkb], writes=[NCks[hf]])
                py = [nb2(), nb2()]
                for t in range(TG):
                    hf, tt_ = t // 2, t % 2
                    ppp = py[hf]
                    o0 = tt_ * 256
                    if not last:
                        op("pe", lambda e, hf=hf, tt_=tt_, ppp=ppp, o0=o0, Tc=Tc: e.matmul(ppp.t[:, o0:o0 + 128], NCks[hf].t[:, tt_, 1, :], Tc[hf].t[:, tt_, 0, :], start=True, stop=True),
                           reads=[NCks[hf], Tc[hf]], writes=[ppp], inc=False)
                    op("pe", lambda e, hf=hf, tt_=tt_, ppp=ppp, o0=o0, Tc=Tc: e.matmul(ppp.t[:, o0 + 128:o0 + 256], NCks[hf].t[:, tt_, 0, :], Tc[hf].t[:, tt_, 1, :], start=True, stop=True),
                       reads=[NCks[hf], Tc[hf]], writes=[ppp])
                for hf in range(2):
                    src4 = py[hf].t[:, :].rearrange("p (a b c) -> p a b c", a=2, b=2)
                    if last:
                        src, dst = src4[:, :, 1, :], Ybs[hf].t[:, :, 1, :]
                    else:
                        src, dst = src4, Ybs[hf].t[:, :, :, :]
                    if hf == 0:
                        op("act", lambda e, src=src, dst=dst: e.activation(out=dst, in_=src, func=AF.Copy), reads=[py[hf]], writes=[Ybs[hf]])
                    else:
                        op("dve", lambda e, src=src, dst=dst: e.tensor_copy(out=dst, in_=src), reads=[py[hf]], writes=[Ybs[hf]])
                pz = [nb2(), nb2()]
                for t in range(TG):
                    hf, tt_ = t // 2, t % 2
                    pzz = pz[hf]
                    o0 = tt_ * 256
                    if not last:
                        op("pe", lambda e, hf=hf, tt_=tt_, pzz=pzz, o0=o0, Tc=Tc: e.matmul(pzz.t[:, o0:o0 + 128], Tc[hf].t[:, tt_, 1, :], Ybs[hf].t[:, tt_, 0, :], start=True, stop=True),
                           reads=[Tc[hf], Ybs[hf]], writes=[pzz], inc=False)
                    op("pe", lambda e, hf=hf, tt_=tt_, pzz=pzz, o0=o0, Tc=Tc: e.matmul(pzz.t[:, o0 + 128:o0 + 256], Tc[hf].t[:, tt_, 0, :], Ybs[hf].t[:, tt_, 1, :], start=True, stop=True),
                       reads=[Tc[hf], Ybs[hf]], writes=[pzz])
                Tn = Tbs[lev % 2]
                for hf in range(2):
                    src4 = pz[hf].t[:, :].rearrange("p (a b c) -> p a b c", a=2, b=2)
                    if last:
                        op("dve", lambda e, hf=hf, src4=src4, Tn=Tn, Tc=Tc: e.tensor_tensor(
                            out=Tn[hf].t[:, :, 1, :], in0=src4[:, :, 1, :], in1=Tc[hf].t[:, :, 1, :], op=ALU.add), reads=[pz[hf], Tc[hf]], writes=[Tn[hf]])
                    else:
                        op("dve", lambda e, hf=hf, src4=src4, Tn=Tn, Tc=Tc: e.tensor_tensor(
                            out=Tn[hf].t[:, :, :, :], in0=src4, in1=Tc[hf].t[:, :, :, :], op=ALU.add), reads=[pz[hf], Tc[hf]], writes=[Tn[hf]])
                Tc = Tn
            TT = Tc
            pw_ = nb2()
            for t in range(TG):
                op("pe", lambda e, t=t: e.matmul(pw_.t[:, t * 128:(t + 1) * 128], kbg.t[:, t, :], TT[t // 2].t[:, t % 2, 1, :], start=True, stop=True),
                   reads=[kbg, TT[t // 2]], writes=[pw_], inc=(t == TG - 1))
            op("act", lambda e: e.activation(out=nwT.t[:, :, :], in_=pw_.t[:, :].rearrange("p (a b) -> p a b", a=TG), func=AF.Copy, scale=-1.0),
               reads=[pw_], writes=[nwT])
            po = nb2()
            pvns = [nb2(), nb2()]
            for t in range(TG):
                ti = g * TG + t
                tc_ = slice(t * 128, (t + 1) * 128)
                vn = vnew[t % 2]
                pvn = pvns[t % 2]
                op("pe", lambda e, t=t, pvn=pvn: e.matmul(pvn.t[:, 0:128], TT[t // 2].t[:, t % 2, 1, :], vb.t[:, t, :], start=True, stop=False),
                   reads=[TT[t // 2], vb], writes=[pvn], inc=False)
                op("pe", lambda e, t=t, pvn=pvn: e.matmul(pvn.t[:, 0:128], nwT.t[:, t, :], Sbf.t[:, :], start=False, stop=True),
                   reads=[nwT, Sbf], writes=[pvn])
                op("act", lambda e, pvn=pvn, vn=vn: e.activation(out=vn.t[:, :], in_=pvn.t[:, 0:128], func=AF.Copy), reads=[pvn], writes=[vn])
                op("pe", lambda e, tc_=tc_: e.matmul(po.t[:, tc_], Sbf.t[:, :], qgT.t[:, tc_], start=True, stop=False),
                   reads=[Sbf, qgT], writes=[po], inc=False)
                op("pe", lambda e, t=t, tc_=tc_, vn=vn: e.matmul(po.t[:, tc_], vn.t[:, :], X2.t[:, t, 1, :], start=False, stop=True),
                   reads=[vn, X2], writes=[po], inc=False)
                op("pe", lambda e, t=t, pvn=pvn, vn=vn: e.matmul(pvn.t[:, 128:256], kd.t[:, t, :], vn.t[:, :], start=True, stop=True),
                   reads=[kd, vn], writes=[pvn])
                op("dve", lambda e, pvn=pvn, ti=ti: e.scalar_tensor_tensor(out=Sbf.t[:, :], in0=S32.t[:, :], scalar=dl.t[:, ti, 8 + h:9 + h],
                                                                           in1=pvn.t[:, 128:256], op0=ALU.mult, op1=ALU.add),
                   reads=[S32, dl, pvn], writes=[Sbf])
                op("dve", lambda e, pvn=pvn, ti=ti: e.scalar_tensor_tensor(out=S32.t[:, :], in0=S32.t[:, :], scalar=dl.t[:, ti, 8 + h:9 + h],
                                                                           in1=pvn.t[:, 128:256], op0=ALU.mult, op1=ALU.add),
                   reads=[S32, dl, pvn], writes=[S32])
            op("act", lambda e: e.activation(out=o32.t[:, :], in_=po.t[:, :], func=AF.Copy), reads=[po], writes=[o32])
            op("act", lambda e: e.activation(out=og2.t[:, :], in_=o32.t[:, :], func=AF.Square), reads=[o32], writes=[og2])
            pb = nb2()
            op("pe", lambda e, pb=pb: e.matmul(pb.t[:, :], onesb.t[:, :], og2.t[:, :], start=True, stop=True), reads=[og2, onesb], writes=[pb])
            op("act", lambda e, pb=pb: e.activation(out=rn[1].t[:, :], in_=pb.t[:, :], func=AF.Ln, scale=1.0 / 128, bias=EPS), reads=[pb], writes=[rn[1]])
            op("act", lambda e: e.activation(out=rn[1].t[:, :], in_=rn[1].t[:, :], func=AF.Exp, scale=-0.5), reads=[rn[1]], writes=[rn[1]])
            op("dve", lambda e: e.tensor_tensor(out=og.t[:, :], in0=o32.t[:, :], in1=rn[1].t[:, :], op=ALU.mult), reads=[o32, rn[1]], writes=[og])
            op("dve", lambda e: e.scalar_tensor_tensor(out=og2.t[:, :], in0=og.t[:, :], scalar=P("onorm", li), in1=zs.t[:, :],
                                                       op0=ALU.mult, op1=ALU.mult), reads=[og, prm, zs], writes=[og2])
            for oc in range(8):
                pb = nb2()
                op("pe", lambda e, oc=oc, pb=pb: e.matmul(pb.t[:, :], wo.t[:, oc * 128:(oc + 1) * 128], og2.t[:, :], start=True, stop=True),
                   reads=[wo, og2], writes=[pb])
                resid_add(pb, l, 16, oc, sl, cols)


        wcur = [None]
        k.replay(k.record(lambda: stage1(0)))
        for u in range(U):
            ra = k.record(lambda: stage2(u))
            rb = k.record(lambda: stage1(u + 1)) if u + 1 < U else []
            k.replay(k.interleave(ra, rb) if INTERLEAVE else (ra + rb))


def moe_layer(k, nc, env):
    op, dma = k.op, k.dma
    xT, hnT, modT, prm, cst = env["xT"], env["hnT"], env["modT"], env["prm"], env["cst"]
    nb, nw, resid_add, P, make_hn = env["nb"], env["nw"], env["resid_add"], env["P"], env["make_hn"]
    l, sl, S, NG, NT = env["l"], env["sl"], env["S"], env["NG"], env["NT"]
    moer_d, moeup_d, moedn_d = env["moer_d"], env["moeup_d"], env["moedn_d"]
    ones, ident = env["ones"], env["ident"]
    wpool = env["wpool"]
    TG = GS // 128
    with ExitStack() as pes:
        GTh = k.sb([32, S], BF16, es=pes, name="GTh")
        GTl = k.sb([32, S], BF16, es=pes, name="GTl")
        with ExitStack() as res:
            def sbt(shape, dt, name):
                return k.sb(shape, dt, es=res, name=name)
            env["alloc_scr"](res)
            GT = sbt([32, S], F32, "GT")
            h32 = sbt([128, KD, GS], F32, "hn32")
            wr = sbt([128, KD, 36], F32, "wr")
            lg = sbt([128, 36], F32, "lg")
            sm = {n: sbt([128, 36], F32, "sm_" + n) for n in ("mx", "oh", "pen", "ml", "m1", "k1", "ml2", "m2", "k2", "ex", "sm", "gp", "r", "den", "w1", "w2", "G", "nmx")}
            dma("sp", wr.t[:, :, :], moer_d[l].rearrange("(k p) n -> p k n", p=128), writes=[wr])

            def dv(fn, reads, writes):
                op("dve", fn, reads=reads, writes=writes)

            for g in range(NG):
                make_hn(l, 1, sl, g, hn32=h32)
                for t in range(TG):
                    ti = g * TG + t
                    pb = nb()
                    for kk in range(KD):
                        op("pe", lambda e, kk=kk, t=t, pb=pb: e.matmul(pb.t[:, 0:36], h32.t[:, kk, t * 128:(t + 1) * 128], wr.t[:, kk, :],
                                                                     start=(kk == 0), stop=(kk == KD - 1)),
                           reads=[h32, wr], writes=[pb], inc=(kk == KD - 1))
                    dv(lambda e, pb=pb: e.tensor_tensor(out=lg.t[:, :], in0=pb.t[:, 0:36], in1=P("rbias", l * 36, 36), op=ALU.add), [pb, prm], [lg])
                    dv(lambda e: e.tensor_reduce(out=sm["mx"].t[:, 0:1], in_=lg.t[:, 0:4], axis=mybir.AxisListType.X, op=ALU.max), [lg], [sm["mx"]])
                    dv(lambda e: e.tensor_scalar(out=sm["oh"].t[:, 0:4], in0=lg.t[:, 0:4], scalar1=sm["mx"].t[:, 0:1], scalar2=None, op0=ALU.is_ge), [lg, sm["mx"]], [sm["oh"]])
                    dv(lambda e: e.tensor_scalar_mul(out=sm["nmx"].t[:, 0:1], in0=sm["mx"].t[:, 0:1], scalar1=-1.0), [sm["mx"]], [sm["nmx"]])
                    op("act", lambda e: e.activation(out=sm["ex"].t[:, 0:4], in_=lg.t[:, 0:4], func=AF.Exp, bias=sm["nmx"].t[:, 0:1]), reads=[lg, sm["nmx"]], writes=[sm["ex"]])
                    dv(lambda e: e.tensor_reduce(out=sm["sm"].t[:, 0:1], in_=sm["ex"].t[:, 0:4], axis=mybir.AxisListType.X, op=ALU.add), [sm["ex"]], [sm["sm"]])
                    dv(lambda e: e.reciprocal(out=sm["gp"].t[:, 0:1], in_=sm["sm"].t[:, 0:1]), [sm["sm"]], [sm["gp"]])
                    dv(lambda e: e.tensor_scalar(out=sm["pen"].t[:, 0:4], in0=sm["oh"].t[:, 0:4], scalar1=-1.0, scalar2=1.0e4, op0=ALU.add, op1=ALU.mult), [sm["oh"]], [sm["pen"]])
                    dv(lambda e: e.tensor_tensor(out=sm["ml"].t[:, 0:32].rearrange("p (a b) -> p a b", a=4), in0=lg.t[:, 4:36].rearrange("p (a b) -> p a b", a=4),
                                                 in1=sm["pen"].t[:, 0:4].unsqueeze(2).to_broadcast([128, 4, 8]), op=ALU.add), [lg, sm["pen"]], [sm["ml"]])
                    dv(lambda e: e.tensor_reduce(out=sm["m1"].t[:, 0:1], in_=sm["ml"].t[:, 0:32], axis=mybir.AxisListType.X, op=ALU.max), [sm["ml"]], [sm["m1"]])
                    dv(lambda e: e.tensor_scalar(out=sm["k1"].t[:, 0:32], in0=sm["ml"].t[:, 0:32], scalar1=sm["m1"].t[:, 0:1], scalar2=None, op0=ALU.is_ge), [sm["ml"], sm["m1"]], [sm["k1"]])
                    dv(lambda e: e.scalar_tensor_tensor(out=sm["ml2"].t[:, 0:32], in0=sm["k1"].t[:, 0:32], scalar=-1.0e4, in1=sm["ml"].t[:, 0:32], op0=ALU.mult, op1=ALU.add),
                       [sm["k1"], sm["ml"]], [sm["ml2"]])
                    dv(lambda e: e.tensor_reduce(out=sm["m2"].t[:, 0:1], in_=sm["ml2"].t[:, 0:32], axis=mybir.AxisListType.X, op=ALU.max), [sm["ml2"]], [sm["m2"]])
                    dv(lambda e: e.tensor_scalar(out=sm["k2"].t[:, 0:32], in0=sm["ml2"].t[:, 0:32], scalar1=sm["m2"].t[:, 0:1], scalar2=None, op0=ALU.is_ge), [sm["ml2"], sm["m2"]], [sm["k2"]])
                    dv(lambda e: e.tensor_tensor(out=sm["r"].t[:, 0:1], in0=sm["m2"].t[:, 0:1], in1=sm["m1"].t[:, 0:1], op=ALU.subtract), [sm["m2"], sm["m1"]], [sm["r"]])
                    op("act", lambda e: e.activation(out=sm["r"].t[:, 0:1], in_=sm["r"].t[:, 0:1], func=AF.Exp), reads=[sm["r"]], writes=[sm["r"]])
                    dv(lambda e: e.tensor_scalar_add(out=sm["den"].t[:, 0:1], in0=sm["r"].t[:, 0:1], scalar1=1.0), [sm["r"]], [sm["den"]])
                    dv(lambda e: e.reciprocal(out=sm["den"].t[:, 0:1], in_=sm["den"].t[:, 0:1]), [sm["den"]], [sm["den"]])
                    dv(lambda e: e.tensor_tensor(out=sm["w1"].t[:, 0:1], in0=sm["gp"].t[:, 0:1], in1=sm["den"].t[:, 0:1], op=ALU.mult), [sm["gp"], sm["den"]], [sm["w1"]])
                    dv(lambda e: e.tensor_tensor(out=sm["w2"].t[:, 0:1], in0=sm["w1"].t[:, 0:1], in1=sm["r"].t[:, 0:1], op=ALU.mult), [sm["w1"], sm["r"]], [sm["w2"]])
                    dv(lambda e: e.tensor_scalar_mul(out=sm["G"].t[:, 0:32], in0=sm["k1"].t[:, 0:32], scalar1=sm["w1"].t[:, 0:1]), [sm["k1"], sm["w1"]], [sm["G"]])
                    dv(lambda e: e.scalar_tensor_tensor(out=sm["G"].t[:, 0:32], in0=sm["k2"].t[:, 0:32], scalar=sm["w2"].t[:, 0:1], in1=sm["G"].t[:, 0:32], op0=ALU.mult, op1=ALU.add),
                       [sm["k2"], sm["w2"], sm["G"]], [sm["G"]])
                    pt = nb()
                    op("pe", lambda e, pt=pt: e.transpose(pt.t[0:32, 0:128], sm["G"].t[:, 0:32], ident), reads=[sm["G"], cst], writes=[pt])
                    op("act", lambda e, pt=pt, ti=ti: e.activation(out=GT.t[:, ti * 128:(ti + 1) * 128], in_=pt.t[0:32, 0:128], func=AF.Copy), reads=[pt], writes=[GT])
            op("act", lambda e: e.activation(out=GTh.t[:, :], in_=GT.t[:, :], func=AF.Copy), reads=[GT], writes=[GTh])
            op("dve", lambda e: e.tensor_tensor(out=GTl.t[:, :], in0=GT.t[:, :], in1=GTh.t[:, :], op=ALU.subtract), reads=[GT, GTh], writes=[GTl])
            k.barrier()

        def sbt(shape, dt, name):
            return k.sb(shape, dt, es=pes, name=name)
        selall = sbt([32, NEXP, 128], BF16, "selall")
        wup = list(wpool) + [sbt([128, KD, 512], BF16, "wupx%d" % i) for i in range(2)]
        wdn = [sbt([128, 2, D], BF16, "wdn%d" % i) for i in range(4)]
        gsb = [sbt([128, GS], F32, "gsb%d" % i) for i in range(2)]
        sgb = [sbt([128, GS], F32, "sgb%d" % i) for i in range(2)]
        tb = sgb
        hb = [[sbt([128, 2, GS], BF16, "hb%d_%d" % (i, j)) for j in range(2)] for i in range(2)]
        for ex_ in range(NEXP):
            op("dve", lambda e, ex_=ex_: e.tensor_copy(out=selall.t[:, ex_, :], in_=ident[0:32, ex_:ex_ + 1].to_broadcast([32, 128])),
               reads=[cst], writes=[selall])

        def fetch_pair(ep_):
            for ei_ in range(2):
                ex_ = 2 * ep_ + ei_
                wu_ = wup[ex_ % 4]
                dma("pool", wu_.t[:, :, :], moeup_d[l, ex_].rearrange("(k p) n -> p k n", p=128), writes=[wu_])
                wd_ = wdn[ex_ % 4]
                dma("pool", wd_.t[:, :, :], moedn_d[l, ex_].rearrange("(k p) n -> p k n", p=128), writes=[wd_])

        fetch_pair(0)
        for ep in range(NEXP // 2):
            if ep + 1 < NEXP // 2:
                fetch_pair(ep + 1)
            wus = [wup[(2 * ep + ei) % 4] for ei in range(2)]
            wds = [wdn[(2 * ep + ei) % 4] for ei in range(2)]
            def phase1(g):
                cols = slice(g * GS, (g + 1) * GS)
                for ei in range(2):
                    ex = 2 * ep + ei
                    wu = wus[ei]
                    pg_ = nb()
                    op("pe", lambda e, ex=ex, pg_=pg_: e.matmul(pg_.t[:, :], selall.t[:, ex, :], GTh.t[:, cols], start=True, stop=False),
                       reads=[selall, GTh], writes=[pg_], inc=False)
                    op("pe", lambda e, ex=ex, pg_=pg_: e.matmul(pg_.t[:, :], selall.t[:, ex, :], GTl.t[:, cols], start=False, stop=True),
                       reads=[selall, GTl], writes=[pg_])
                    gs_ = gsb[ei]
                    op("dve", lambda e, pg_=pg_, gs_=gs_: e.tensor_copy(out=gs_.t[:, :], in_=pg_.t[:, :]), reads=[pg_], writes=[gs_])
                    hh = hb[ei][g % 2]
                    for j in range(2):
                        pgt = nb()
                        put = nb()
                        for kk in range(KD):
                            op("pe", lambda e, kk=kk, j=j, pgt=pgt, wu=wu: e.matmul(pgt.t[:, :], wu.t[:, kk, j * 128:(j + 1) * 128], hnT.t[:, kk, cols],
                                                                                 start=(kk == 0), stop=(kk == KD - 1)), reads=[wu, hnT], writes=[pgt], inc=(kk == KD - 1))
                        for kk in range(KD):
                            op("pe", lambda e, kk=kk, j=j, put=put, wu=wu: e.matmul(put.t[:, :], wu.t[:, kk, FE + j * 128:FE + (j + 1) * 128], hnT.t[:, kk, cols],
                                                                                 start=(kk == 0), stop=(kk == KD - 1)), reads=[wu, hnT], writes=[put], inc=(kk == KD - 1))
                        sg = sgb[j]
                        tt = tb[j]
                        op("act", lambda e, pgt=pgt, sg=sg: e.activation(out=sg.t[:, :], in_=pgt.t[:, :], func=AF.Silu), reads=[pgt], writes=[sg])
                        op("dve", lambda e, put=put, sg=sg, tt=tt: e.tensor_tensor(out=tt.t[:, :], in0=put.t[:, :], in1=sg.t[:, :], op=ALU.mult),
                           reads=[put, sg], writes=[tt])
                        op("pool", lambda e, j=j, tt=tt, hh=hh, gs_=gs_: e.tensor_tensor(out=hh.t[:, j, :], in0=tt.t[:, :], in1=gs_.t[:, :], op=ALU.mult),
                           reads=[tt, gs_], writes=[hh])
            def phase2(g):
                cols = slice(g * GS, (g + 1) * GS)
                for oc in range(8):
                    py = nb()
                    n = 0
                    for ei in range(2):
                        hh = hb[ei][g % 2]
                        wd = wds[ei]
                        for j in range(2):
                            op("pe", lambda e, j=j, oc=oc, py=py, wd=wd, hh=hh, n=n: e.matmul(py.t[:, :], wd.t[:, j, oc * 128:(oc + 1) * 128], hh.t[:, j, :],
                                                                                          start=(n == 0), stop=(n == 3)), reads=[wd, hh], writes=[py], inc=(n == 3))
                            n += 1
                    resid_add(py, l, 40, oc, sl, cols)

            for g in range(NG):
                phase1(g)
                if g > 0:
                    phase2(g - 1)
            phase2(NG - 1)


_CACHE = {}


def prepare_inputs(inputs, cfg):
    NSEQ, S, L, NCO = cfg["NSEQ"], cfg["S"], cfg["DEPTH"], cfg["NCORES"]
    f = lambda a: np.ascontiguousarray(np.asarray(a, np.float32))
    prm, _ = _build_prm(inputs, L)
    cst = _build_cst()
    dnin = f(inputs["dn_w_in"])
    qkvz = dnin[:, :, :4096].reshape(2, D, 4, 8, 128).transpose(0, 1, 3, 2, 4).reshape(2, D, 4096)
    shared = {
        "cst": cst, "prm": prm, "mk": _build_mk(),
        "mod_w": f(inputs["mod_w"])[:L], "ab_w_in": f(inputs["ab_w_in"]), "pool_w": f(inputs["pool_w"]),
        "ab_w_out": f(inputs["ab_w_out"]), "dn_w_in_h": np.ascontiguousarray(qkvz),
        "dn_w_bg": np.ascontiguousarray(dnin[:, :, 4096:4112]), "dn_w_out": f(inputs["dn_w_out"]),
        "moe_w_r": np.ascontiguousarray(np.concatenate([f(inputs["moe_w_grp"])[:L], f(inputs["moe_w_exp"])[:L]], axis=2)),
        "moe_w_up": f(inputs["moe_w_up"])[:L], "moe_w_down": f(inputs["moe_w_down"])[:L],
    }
    x = f(inputs["x"])
    c = f(inputs["c"])
    maps = []
    for core in range(NCO):
        xs = x[core * NSEQ:(core + 1) * NSEQ].reshape(NSEQ * S, D)
        cs = c[core * NSEQ:(core + 1) * NSEQ]
        cT = cs.reshape(NSEQ, 8, 128).transpose(2, 1, 0).reshape(128, 8 * NSEQ)
        m = dict(shared)
        m["x"] = np.ascontiguousarray(xs)
        m["cT"] = np.ascontiguousarray(cT)
        maps.append(m)
    return maps


def kernel(**inputs):
    cfg = CFG
    key = tuple(sorted(cfg.items()))
    if key not in _CACHE:
        _CACHE[key] = build_program(cfg)[0]
    nc = _CACHE[key]
    maps = prepare_inputs(inputs, cfg)
    res = run_bass_kernel_spmd(nc, maps, core_ids=list(range(cfg["NCORES"])))
    outs = [np.asarray(r["out"], np.float32).reshape(cfg["NSEQ"], cfg["S"], D) for r in res.results]
    return np.concatenate(outs, axis=0)
```

```python
import numpy as np
from contextlib import ExitStack
import concourse.bass as bass
import concourse.mybir as mybir
from concourse.bass_utils import run_bass_kernel_spmd

F32 = mybir.dt.float32
BF16 = mybir.dt.bfloat16
AF = mybir.ActivationFunctionType
ALU = mybir.AluOpType

D = 1024
KD = 8
EPS = 1e-6
NEXP = 32
FE = 256
NDSEM = 8
GS = 512
NEG = -30000.0
INTERLEAVE = True

CFG = dict(NSEQ=4, S=2048, DEPTH=4, NCORES=8)


class Buf:
    __slots__ = ("t", "w", "r", "name")

    def __init__(self, t, name):
        self.t = t
        self.w = {}
        self.r = {}
        self.name = name

    def __getitem__(self, k):
        return self.t[k]


class _Proxy:
    def __init__(self):
        self.call = None

    def __getattr__(self, name):
        def f(*a, **kw):
            self.call = (name, a, kw)
            return None
        return f


class K:
    def __init__(self, nc, es):
        self.nc = nc
        self.es = es
        self.E = {"pe": nc.tensor, "act": nc.scalar, "dve": nc.vector, "pool": nc.gpsimd, "sp": nc.sync}
        self.sem = {e: es.enter_context(nc.semaphore("s_" + e)) for e in self.E}
        self.cnt = {e: 0 for e in self.E}
        self.waited = {e: {} for e in self.E}
        self.dq = {}
        for q in ("sp", "pool"):
            self.dq[q] = [[es.enter_context(nc.semaphore("d_%s%d" % (q, i))), 0, "d_%s%d" % (q, i)] for i in range(NDSEM)]
        self.dqi = {"sp": 0, "pool": 0}
        self.uid = 0
        self.nins = 0
        self.rec = None

    def sb(self, shape, dt, es=None, name=None):
        self.uid += 1
        name = (name or "t") + "_%d" % self.uid
        t = (es or self.es).enter_context(self.nc.sbuf_tensor(name, list(shape), dt))
        return Buf(t, name)

    def psb(self, shape, dt, name):
        t = self.es.enter_context(self.nc.psum_tensor(name, list(shape), dt))
        return Buf(t, name)

    def _wait(self, eng, tok):
        key, sem, val, peng = tok
        if peng == "pe" and eng == "pe":
            return
        if self.waited[eng].get(key, 0) >= val:
            return
        self.E[eng].wait_ge(sem, val)
        self.nins += 1
        self.waited[eng][key] = val

    def _deps(self, eng, reads, writes):
        for b in reads:
            for tok in b.w.values():
                self._wait(eng, tok)
        for b in writes:
            for tok in b.w.values():
                self._wait(eng, tok)
            for tok in b.r.values():
                self._wait(eng, tok)

    def _mark(self, tok, reads, writes):
        for b in reads:
            b.r[tok[0]] = tok
        for b in writes:
            b.w = {tok[0]: tok}
            b.r = {}

    def op(self, eng, fn, reads=(), writes=(), inc=True):
        if self.rec is not None:
            pr = _Proxy()
            fn(pr)
            self.rec.append(("op", eng, pr.call, tuple(reads), tuple(writes)))
            return
        self._deps(eng, reads, writes)
        ins = fn(self.E[eng])
        self.nins += 1
        if inc:
            self.cnt[eng] += 1
            ins.then_inc(self.sem[eng], 1)
            tok = (eng, self.sem[eng], self.cnt[eng], eng)
        else:
            tok = (eng, self.sem[eng], self.cnt[eng] + 1, eng)
        self._mark(tok, reads, writes)

    def dma(self, q, out, in_, reads=(), writes=()):
        if self.rec is not None:
            self.rec.append(("dma", q, (out, in_), tuple(reads), tuple(writes)))
            return
        self._deps(q, reads, writes)
        slot = self.dq[q][self.dqi[q] % NDSEM]
        self.dqi[q] += 1
        if slot[1] > 0:
            self._wait(q, (slot[2], slot[0], slot[1], "dma"))
        ins = self.E[q].dma_start(out=out, in_=in_)
        self.nins += 1
        slot[1] += 16
        ins.then_inc(slot[0], 16)
        tok = (slot[2], slot[0], slot[1], "dma")
        self._mark(tok, reads, writes)

    def record(self, fn):
        self.rec = []
        fn()
        r, self.rec = self.rec, None
        return r

    def replay(self, items):
        for it in items:
            if it[0] == "op":
                _, eng, call, reads, writes = it
                self.op(eng, lambda e, c=call: getattr(e, c[0])(*c[1], **c[2]), reads=reads, writes=writes)
            else:
                _, q, (out, in_), reads, writes = it
                self.dma(q, out, in_, reads=reads, writes=writes)

    @staticmethod
    def interleave(a, b):
        out = []
        i = j = 0
        while i < len(a) or j < len(b):
            if j >= len(b) or (i < len(a) and i * len(b) <= j * len(a)):
                out.append(a[i])
                i += 1
            else:
                out.append(b[j])
                j += 1
        return out

    def barrier(self, engines=None):
        engs = list(self.E) if engines is None else engines
        for e in engs:
            for f in self.E:
                if self.cnt[f] > 0:
                    self._wait(e, (f, self.sem[f], self.cnt[f], "x"))
            for q in self.dq:
                for slot in self.dq[q]:
                    if slot[1] > 0:
                        self._wait(e, (slot[2], slot[0], slot[1], "dma"))


def _prm_layout(L):
    off = {}
    n = 0

    def add(name, cnt):
        nonlocal n
        off[name] = n
        n += cnt

    add("nm", L * 8)
    add("nf", L * 8)
    add("fn", 8)
    add("modb", L * 48)
    add("pscale", 2 * 4)
    add("convw", 2 * 4 * 31)
    add("convb", 2 * 4)
    add("lng", 2 * 4)
    add("lnb", 2 * 4)
    add("dnconv", 2 * 24 * 4)
    add("onorm", 2)
    add("alog", 2)
    add("dtb", 2)
    add("mlo", 1)
    add("mhi", 1)
    add("rbias", L * 36)
    return off, n


def _fm(v):
    v = np.asarray(v, np.float32)
    lead = v.shape[:-1]
    n = v.shape[-1] // 128
    a = v.reshape(lead + (n, 128))
    a = np.moveaxis(a, -1, 0)
    return np.ascontiguousarray(a.reshape(128, -1))


def _build_prm(inp, L):
    off, n = _prm_layout(L)
    P = np.zeros((128, n), np.float32)

    def put(name, arr):
        arr = np.asarray(arr, np.float32)
        P[:arr.shape[0], off[name]:off[name] + arr.shape[1]] = arr

    put("nm", _fm(inp["norm_mix"][:L]))
    put("nf", _fm(inp["norm_ffn"][:L]))
    put("fn", _fm(inp["final_norm"]))
    put("modb", _fm(inp["mod_b"][:L]))
    put("pscale", _fm(inp["pool_scale"]))
    cw = np.asarray(inp["conv_w"], np.float32)
    cw = cw.reshape(2, 31, 4, 128).transpose(3, 0, 2, 1)
    put("convw", cw.reshape(128, -1))
    put("convb", _fm(inp["conv_b"]))
    put("lng", _fm(inp["conv_ln_g"]))
    put("lnb", _fm(inp["conv_ln_b"]))
    dcw = np.asarray(inp["dn_conv_w"], np.float32)
    dcw = dcw.reshape(2, 4, 24, 128).transpose(3, 0, 2, 1)
    put("dnconv", dcw.reshape(128, -1))
    put("onorm", np.asarray(inp["dn_onorm"], np.float32).T)
    al = np.zeros((16, 2), np.float32)
    al[8:16, :] = np.asarray(inp["dn_a_log"], np.float32).T
    put("alog", al)
    db = np.zeros((16, 2), np.float32)
    db[8:16, :] = np.asarray(inp["dn_dt_bias"], np.float32).T
    put("dtb", db)
    mlo = np.zeros((16, 1), np.float32)
    mlo[0:8] = 1.0
    put("mlo", mlo)
    put("mhi", 1.0 - mlo)
    rb = np.concatenate([np.asarray(inp["moe_b_grp"], np.float32)[:L], np.asarray(inp["moe_b_exp"], np.float32)[:L]], axis=1)
    put("rbias", np.broadcast_to(rb.reshape(1, -1), (128, L * 36)))
    return P, off


def _build_cst():
    c = np.zeros((128, 640), np.float32)
    c[:, 0:128] = np.eye(128, dtype=np.float32)
    c[:, 128:256] = 1.0
    c[127, 256:384] = 1.0
    m = np.arange(128)[:, None]
    cc = np.arange(128)[None, :]
    c[:, 384:512] = np.where(cc > m, 0.0, NEG)
    c[:, 512:640] = np.where(cc >= m, 0.0, NEG)
    return c


def _build_mk():
    mk = np.zeros((128, 7, 2, 128), np.float32)
    r = np.arange(128)[:, None]
    c = np.arange(128)[None, :]
    for k_ in range(7):
        b = 1 << k_
        same = (r // (2 * b)) == (c // (2 * b))
        mk[:, k_, 0, :] = same & ((r % (2 * b)) >= b) & ((c % (2 * b)) < b)
        mk[:, k_, 1, :] = same & ((c % (2 * b)) >= b) & ((r % (2 * b)) < b)
    return np.ascontiguousarray(mk.reshape(128, -1))


def build_program(cfg):
    NSEQ, S, L = cfg["NSEQ"], cfg["S"], cfg["DEPTH"]
    NG = S // GS
    NT = S // 128
    off, NP = _prm_layout(L)
    nc = bass.Bass("TRN2", target_bir_lowering=False)

    def din(name, shape):
        return nc.dram_tensor(name, list(shape), F32, kind="ExternalInput").ap()

    x_d = din("x", [NSEQ * S, D])
    cT_d = din("cT", [128, 8 * NSEQ])
    cst_d = din("cst", [128, 640])
    prm_d = din("prm", [128, NP])
    mk_d = din("mk", [128, 7 * 2 * 128])
    modw_d = din("mod_w", [L, D, 6 * D])
    abin_d = din("ab_w_in", [2, D, 1536])
    poolw_d = din("pool_w", [2, 4, 128, 128])
    about_d = din("ab_w_out", [2, D, D])
    dnin_d = din("dn_w_in_h", [2, D, 4096])
    dnbg_d = din("dn_w_bg", [2, D, 16])
    dnout_d = din("dn_w_out", [2, D, D])
    moer_d = din("moe_w_r", [L, D, 36])
    moeup_d = din("moe_w_up", [L, NEXP, D, 2 * FE])
    moedn_d = din("moe_w_down", [L, NEXP, FE, D])
    out_d = nc.dram_tensor("out", [NSEQ * S, D], F32, kind="ExternalOutput").ap()

    es = ExitStack()
    k = K(nc, es)
    op, dma = k.op, k.dma

    xT = k.sb([128, KD, S], F32, name="xT")
    hnT = k.sb([128, KD, S], BF16, name="hnT")
    cst = k.sb([128, 640], F32, name="cst")
    identb = k.sb([128, 128], BF16, name="identb")
    onesb = k.sb([128, 128], BF16, name="onesb")
    prm = k.sb([128, NP], F32, name="prm")
    cact = k.sb([128, 8 * NSEQ], F32, name="cact")
    modT = k.sb([128, L, 48, NSEQ], F32, name="modT")
    modA = k.sb([128, L, 2, 8, NSEQ], F32, name="modA")
    wpool = [k.sb([128, KD, 512], BF16, name="wp%d" % i) for i in range(2)]
    wpi = [0]
    banks = [k.psb([128, 512], F32, "bank%d" % i) for i in range(7)]
    bankb = k.psb([128, 1024], BF16, "bankb")
    bi = [0]

    def nb():
        b = banks[bi[0] % 7]
        bi[0] += 1
        return b

    def nw():
        w = wpool[wpi[0] % 2]
        wpi[0] += 1
        return w

    ident = cst.t[:, 0:128]
    ones = cst.t[:, 128:256]
    sel127 = cst.t[:, 256:384]
    negm2 = cst.t[:, 384:640]

    def P(name, i=0, n=1, rows=128):
        o = off[name] + i
        return prm.t[0:rows, o:o + n]

    dma("sp", cst.t[:, :], cst_d[:, :], writes=[cst])
    dma("sp", prm.t[:, :], prm_d[:, :], writes=[prm])
    dma("sp", cact.t[:, :], cT_d[:, :], writes=[cact])
    dma("pool", identb.t[:, :], cst_d[:, 0:128], writes=[identb])
    dma("pool", onesb.t[:, :], cst_d[:, 128:256], writes=[onesb])
    op("act", lambda e: e.activation(out=cact.t[:, :], in_=cact.t[:, :], func=AF.Silu), reads=[cact], writes=[cact])

    with ExitStack() as pes:
        mw = [k.sb([128, KD, 512], F32, es=pes, name="mw%d" % i) for i in range(2)]
        mi = 0
        for l in range(L):
            for pc in range(12):
                w = mw[mi % 2]
                mi += 1
                dma("sp", w.t[:, :, :], modw_d[l, :, pc * 512:(pc + 1) * 512].rearrange("(k p) n -> p k n", p=128), writes=[w])
                pb = nb()
                for c4 in range(4):
                    for kk in range(KD):
                        op("pe", lambda e, c4=c4, kk=kk, w=w, pb=pb: e.matmul(
                            pb.t[:, c4 * NSEQ:(c4 + 1) * NSEQ], w.t[:, kk, c4 * 128:(c4 + 1) * 128],
                            cact.t[:, kk * NSEQ:(kk + 1) * NSEQ], start=(kk == 0), stop=(kk == KD - 1)),
                           reads=[w, cact], writes=[pb], inc=(kk == KD - 1))
                for c4 in range(4):
                    ch = pc * 4 + c4
                    op("dve", lambda e, c4=c4, ch=ch, pb=pb, l=l: e.tensor_scalar(
                        out=modT.t[:, l, ch, :], in0=pb.t[:, c4 * NSEQ:(c4 + 1) * NSEQ],
                        scalar1=P("modb", l * 48 + ch), scalar2=None, op0=ALU.add),
                       reads=[pb, prm], writes=[modT])
            for j, (nname, cbase) in enumerate((("nm", 8), ("nf", 32))):
                for kk in range(KD):
                    op("dve", lambda e, j=j, kk=kk, nname=nname, cbase=cbase, l=l: e.tensor_scalar(
                        out=modA.t[:, l, j, kk, :], in0=modT.t[:, l, cbase + kk, :],
                        scalar1=1.0, scalar2=P(nname, l * 8 + kk), op0=ALU.add, op1=ALU.mult),
                       reads=[modT, prm], writes=[modA])
    k.barrier()

    def rms_rstd(pes, g, scale_n):
        cols = slice(g * GS, (g + 1) * GS)
        pb = nb()
        for kk in range(KD):
            op("act", lambda e, kk=kk: e.activation(out=hnT.t[:, kk, cols], in_=xT.t[:, kk, cols], func=AF.Square),
               reads=[xT], writes=[hnT])
        for kk in range(KD):
            op("pe", lambda e, kk=kk, pb=pb: e.matmul(pb.t[:, :], onesb.t[:, :], hnT.t[:, kk, cols], start=(kk == 0), stop=(kk == KD - 1)),
               reads=[hnT, onesb], writes=[pb], inc=(kk == KD - 1))
        rs = rsb[g % 2]
        op("act", lambda e: e.activation(out=rs.t[:, :], in_=pb.t[:, :], func=AF.Ln, scale=1.0 / scale_n, bias=EPS), reads=[pb], writes=[rs])
        op("act", lambda e: e.activation(out=rs.t[:, :], in_=rs.t[:, :], func=AF.Exp, scale=-0.5), reads=[rs], writes=[rs])
        return rs

    def make_hn(l, which, sl, g, hn32=None):
        cols = slice(g * GS, (g + 1) * GS)
        rs = rms_rstd(None, g, float(D))
        shbase = 0 if which == 0 else 24
        for kk in range(KD):
            tmp = scr["tmpb"][kk % 2]
            op("dve", lambda e, kk=kk, tmp=tmp: e.tensor_tensor(out=tmp.t[:, :], in0=xT.t[:, kk, cols], in1=rs.t[:, :], op=ALU.mult),
               reads=[xT, rs], writes=[tmp])
            op("act", lambda e, kk=kk, tmp=tmp: e.activation(
                out=hnT.t[:, kk, cols], in_=tmp.t[:, :], func=AF.Identity,
                bias=modT.t[:, l, shbase + kk, sl:sl + 1], scale=modA.t[:, l, which, kk, sl:sl + 1]),
               reads=[tmp, modT, modA], writes=[hnT])
            if hn32 is not None:
                op("act", lambda e, kk=kk, tmp=tmp: e.activation(
                    out=hn32.t[:, kk, :], in_=tmp.t[:, :], func=AF.Identity,
                    bias=modT.t[:, l, shbase + kk, sl:sl + 1], scale=modA.t[:, l, which, kk, sl:sl + 1]),
                   reads=[tmp, modT, modA], writes=[hn32])

    def load_w(src_ap, ncols=512):
        w = nw()
        dma("pool", w.t[:, :, 0:ncols], src_ap.rearrange("(k p) n -> p k n", p=128), writes=[w])
        return w

    def proj(w, wc0, g, pb, pcols=None):
        cols = slice(g * GS, (g + 1) * GS)
        for kk in range(KD):
            op("pe", lambda e, kk=kk: e.matmul(pb.t[:, :], w.t[:, kk, wc0:wc0 + 128], hnT.t[:, kk, cols],
                                               start=(kk == 0), stop=(kk == KD - 1)),
               reads=[w, hnT], writes=[pb], inc=(kk == KD - 1))

    def resid_add(pb, l, gate_chunk_base, kk, sl, cols):
        op("dve", lambda e: e.scalar_tensor_tensor(
            out=xT.t[:, kk, cols], in0=pb.t[:, :], scalar=modT.t[:, l, gate_chunk_base + kk, sl:sl + 1],
            in1=xT.t[:, kk, cols], op0=ALU.mult, op1=ALU.add), reads=[pb, modT, xT], writes=[xT])

    rsb = [k.sb([128, GS], F32, name="rs%d" % i) for i in range(2)]
    scr = {}

    def alloc_scr(es_):
        scr["sqb"] = [k.sb([128, GS], F32, es=es_, name="sq%d" % i) for i in range(2)]
        scr["tmpb"] = [k.sb([128, GS], F32, es=es_, name="tmp%d" % i) for i in range(2)]

    for sl in range(NSEQ):
        with ExitStack() as pes:
            xin = [k.sb([128, D], F32, es=pes, name="xin%d" % i) for i in range(2)]
            for i in range(NT):
                xi = xin[i % 2]
                dma("sp", xi.t[:, :], x_d[sl * S + i * 128: sl * S + (i + 1) * 128, :], writes=[xi])
                for half in range(2):
                    pb = nb()
                    for c4 in range(4):
                        kk = half * 4 + c4
                        op("pe", lambda e, kk=kk, c4=c4, pb=pb, xi=xi: e.transpose(
                            pb.t[:, c4 * 128:(c4 + 1) * 128], xi.t[:, kk * 128:(kk + 1) * 128], ident),
                           reads=[xi, cst], writes=[pb], inc=(c4 == 3))
                    eng = "act" if half == 0 else "dve"
                    if eng == "act":
                        op("act", lambda e, half=half, pb=pb, i=i: e.activation(
                            out=xT.t[:, half * 4:(half + 1) * 4, i * 128:(i + 1) * 128],
                            in_=pb.t[:, :].rearrange("p (a b) -> p a b", a=4), func=AF.Copy), reads=[pb], writes=[xT])
                    else:
                        op("dve", lambda e, half=half, pb=pb, i=i: e.tensor_copy(
                            out=xT.t[:, half * 4:(half + 1) * 4, i * 128:(i + 1) * 128],
                            in_=pb.t[:, :].rearrange("p (a b) -> p a b", a=4)), reads=[pb], writes=[xT])
        k.barrier()

        for l in range(L):
            li = l // 2
            with ExitStack() as hs:
                alloc_scr(hs)
                for g in range(NG):
                    make_hn(l, 0, sl, g)
                k.barrier()
            if l % 2 == 0:
                even_mixer(k, nc, locals())
            else:
                odd_mixer(k, nc, locals())
            k.barrier()
            moe_layer(k, nc, locals())
            k.barrier()

        with ExitStack() as pes:
            alloc_scr(pes)
            xn = [k.sb([128, KD, GS], F32, es=pes, name="xn%d" % i) for i in range(2)]
            ost = [k.sb([128, D], F32, es=pes, name="ost%d" % i) for i in range(2)]
            for g in range(NG):
                cols = slice(g * GS, (g + 1) * GS)
                rs = rms_rstd(None, g, float(D))
                xg = xn[g % 2]
                for kk in range(KD):
                    op("dve", lambda e, kk=kk: e.scalar_tensor_tensor(
                        out=xg.t[:, kk, :], in0=xT.t[:, kk, cols], scalar=P("fn", kk), in1=rs.t[:, :],
                        op0=ALU.mult, op1=ALU.mult), reads=[xT, prm, rs], writes=[xg])
                for ti in range(GS // 128):
                    i = g * (GS // 128) + ti
                    o = ost[i % 2]
                    for half in range(2):
                        pb = nb()
                        for c4 in range(4):
                            kk = half * 4 + c4
                            op("pe", lambda e, kk=kk, c4=c4, pb=pb: e.transpose(
                                pb.t[:, c4 * 128:(c4 + 1) * 128], xg.t[:, kk, ti * 128:(ti + 1) * 128], ident),
                               reads=[xg, cst], writes=[pb], inc=(c4 == 3))
                        if half == 0:
                            op("act", lambda e, pb=pb, o=o: e.activation(out=o.t[:, 0:512], in_=pb.t[:, :], func=AF.Copy),
                               reads=[pb], writes=[o])
                        else:
                            op("dve", lambda e, pb=pb, o=o: e.tensor_copy(out=o.t[:, 512:1024], in_=pb.t[:, :]),
                               reads=[pb], writes=[o])
                    dma("sp", out_d[sl * S + i * 128: sl * S + (i + 1) * 128, :], o.t[:, :], reads=[o])
        k.barrier()

    k.barrier(["sp"])
    es.close()
    return nc, k


def even_mixer(k, nc, env):
    op, dma = k.op, k.dma
    xT, hnT, modT, prm, cst = env["xT"], env["hnT"], env["modT"], env["prm"], env["cst"]
    nb, load_w, proj, resid_add, P = env["nb"], env["load_w"], env["proj"], env["resid_add"], env["P"]
    l, li, sl, S, NG = env["l"], env["li"], env["sl"], env["S"], env["NG"]
    abin_d, poolw_d, about_d = env["abin_d"], env["poolw_d"], env["about_d"]
    ones = env["ones"]
    rsb = env["rsb"]
    WIN = (2, 4, 8, 16)
    with ExitStack() as pes:
        env["alloc_scr"](pes)
        sqb, tmpb = env["scr"]["sqb"], env["scr"]["tmpb"]
        ua = [k.sb([128, 4, 16 + GS], F32, es=pes, name="ua%d" % i) for i in range(2)]
        tl = [k.sb([128, 16 + GS], F32, es=pes, name="tl%d" % i) for i in range(2)]
        glu = [k.sb([128, 4, 30 + GS], F32, es=pes, name="glu%d" % i) for i in range(2)]
        accs = [k.sb([128, 4, GS], F32, es=pes, name="cacc%d" % i) for i in range(2)]
        pooleds = [k.sb([128, 4, GS], BF16, es=pes, name="pooled%d" % i) for i in range(2)]
        pw = k.sb([128, 4, 128], BF16, es=pes, name="poolw")
        sig = [k.sb([128, GS], F32, es=pes, name="sig0")] * 2
        cts = [k.sb([128, 8, GS], BF16, es=pes, name="cat0")]
        mu, rstd = rsb[0], rsb[1]
        dma("pool", pw.t[:, :, :], poolw_d[li].rearrange("j c d -> c j d"), writes=[pw])
        def stageA(g):
            cols = slice(g * GS, (g + 1) * GS)
            u_, g_ = ua[g % 2], glu[g % 2]
            acc = accs[g % 2]
            pooled = pooleds[g % 2]
            if g == 0:
                op("pool", lambda e: e.memset(u_.t[:, :, 0:16], 0.0), writes=[u_])
                op("pool", lambda e: e.memset(g_.t[:, :, 0:30], 0.0), writes=[g_])
            else:
                up, gp = ua[(g - 1) % 2], glu[(g - 1) % 2]
                op("pool", lambda e: e.tensor_copy(out=u_.t[:, :, 0:16], in_=up.t[:, :, GS:GS + 16]), reads=[up], writes=[u_])
                op("pool", lambda e: e.tensor_copy(out=g_.t[:, :, 0:30], in_=gp.t[:, :, GS:GS + 30]), reads=[gp], writes=[g_])
            w0 = load_w(abin_d[li, :, 0:512])
            for j in range(4):
                pb = nb()
                proj(w0, j * 128, g, pb)
                op("act", lambda e, j=j, pb=pb: e.activation(out=u_.t[:, j, 16:16 + GS], in_=pb.t[:, :], func=AF.Copy), reads=[pb], writes=[u_])
            wv = load_w(abin_d[li, :, 512:1024])
            wg = load_w(abin_d[li, :, 1024:1536])
            for j in range(4):
                pg = nb()
                proj(wg, j * 128, g, pg)
                sg = sig[j % 2]
                op("act", lambda e, pg=pg, sg=sg: e.activation(out=sg.t[:, :], in_=pg.t[:, :], func=AF.Sigmoid), reads=[pg], writes=[sg])
                pv = nb()
                proj(wv, j * 128, g, pv)
                op("dve", lambda e, j=j, pv=pv, sg=sg: e.tensor_tensor(out=g_.t[:, j, 30:30 + GS], in0=pv.t[:, :], in1=sg.t[:, :], op=ALU.mult),
                   reads=[pv, sg], writes=[g_])
            for j in range(4):
                w_ = WIN[j]
                prev_ap = u_.t[:, j, :]
                prev_buf = u_
                for lev in range(j + 1):
                    sh = 1 << lev
                    c0 = (2 << lev) - 1
                    dst = tl[lev % 2]
                    op("dve", lambda e, dst=dst, prev_ap=prev_ap, sh=sh, c0=c0: e.tensor_tensor(
                        out=dst.t[:, c0:16 + GS], in0=prev_ap[:, c0:16 + GS], in1=prev_ap[:, c0 - sh:16 + GS - sh], op=ALU.add),
                       reads=[prev_buf], writes=[dst])
                    prev_ap = dst.t[:, :]
                    prev_buf = dst
                if g == 0:
                    for t in range(w_ - 1):
                        op("dve", lambda e, t=t, prev_ap=prev_ap, w_=w_: e.tensor_scalar_mul(
                            out=prev_ap[:, 16 + t:17 + t], in0=prev_ap[:, 16 + t:17 + t], scalar1=float(w_) / (t + 1)),
                           reads=[prev_buf], writes=[prev_buf])
                op("dve", lambda e, j=j, prev_ap=prev_ap, w_=w_: e.scalar_tensor_tensor(
                    out=pooled.t[:, j, :], in0=prev_ap[:, 16:16 + GS], scalar=1.0 / w_, in1=u_.t[:, j, 16:16 + GS],
                    op0=ALU.mult, op1=ALU.subtract), reads=[prev_buf, u_], writes=[pooled])
            for j in range(4):
                eng = "dve"
                for tap in range(31):
                    wcol = P("convw", (li * 4 + j) * 31 + tap)
                    srcv = g_.t[:, j, tap:tap + GS]
                    if tap == 0:
                        op(eng, lambda e, j=j, srcv=srcv, wcol=wcol: e.tensor_scalar(
                            out=acc.t[:, j, :], in0=srcv, scalar1=wcol, scalar2=P("convb", li * 4 + j),
                            op0=ALU.mult, op1=ALU.add), reads=[g_, prm], writes=[acc])
                    else:
                        op(eng, lambda e, j=j, srcv=srcv, wcol=wcol: e.scalar_tensor_tensor(
                            out=acc.t[:, j, :], in0=srcv, scalar=wcol, in1=acc.t[:, j, :],
                            op0=ALU.mult, op1=ALU.add), reads=[g_, prm, acc], writes=[acc])
        def stageB(g):
            cols = slice(g * GS, (g + 1) * GS)
            acc, ct = accs[g % 2], cts[0]
            pooled = pooleds[g % 2]
            for j in range(4):
                pb = nb()
                op("pe", lambda e, j=j, pb=pb: e.matmul(pb.t[:, :], pw.t[:, j, :], pooled.t[:, j, :], start=True, stop=True),
                   reads=[pw, pooled], writes=[pb])
                op("act", lambda e, j=j, pb=pb: e.activation(out=ct.t[:, j, :], in_=pb.t[:, :], func=AF.Copy,
                                                             scale=P("pscale", li * 4 + j)), reads=[pb, prm], writes=[ct])
            pm = nb()
            pq = nb()
            for j in range(4):
                s2 = sqb[j % 2]
                op("act", lambda e, j=j, s2=s2: e.activation(out=s2.t[:, :], in_=acc.t[:, j, :], func=AF.Square), reads=[acc], writes=[s2])
                op("pe", lambda e, j=j: e.matmul(pm.t[:, :], ones, acc.t[:, j, :], start=(j == 0), stop=(j == 3)),
                   reads=[acc, cst], writes=[pm])
                op("pe", lambda e, s2=s2, j=j: e.matmul(pq.t[:, :], ones, s2.t[:, :], start=(j == 0), stop=(j == 3)),
                   reads=[s2, cst], writes=[pq])
            op("act", lambda e: e.activation(out=mu.t[:, :], in_=pm.t[:, :], func=AF.Copy, scale=1.0 / 512), reads=[pm], writes=[mu])
            op("dve", lambda e: e.tensor_tensor(out=rstd.t[:, :], in0=mu.t[:, :], in1=mu.t[:, :], op=ALU.mult), reads=[mu], writes=[rstd])
            op("dve", lambda e: e.scalar_tensor_tensor(out=rstd.t[:, :], in0=pq.t[:, :], scalar=1.0 / 512, in1=rstd.t[:, :],
                                                       op0=ALU.mult, op1=ALU.subtract), reads=[pq, rstd], writes=[rstd])
            op("act", lambda e: e.activation(out=rstd.t[:, :], in_=rstd.t[:, :], func=AF.Ln, bias=EPS), reads=[rstd], writes=[rstd])
            op("act", lambda e: e.activation(out=rstd.t[:, :], in_=rstd.t[:, :], func=AF.Exp, scale=-0.5), reads=[rstd], writes=[rstd])
            for j in range(4):
                t_ = tmpb[j % 2]
                op("dve", lambda e, j=j, t_=t_: e.tensor_tensor(out=t_.t[:, :], in0=acc.t[:, j, :], in1=mu.t[:, :], op=ALU.subtract),
                   reads=[acc, mu], writes=[t_])
                op("dve", lambda e, t_=t_: e.tensor_tensor(out=t_.t[:, :], in0=t_.t[:, :], in1=rstd.t[:, :], op=ALU.mult),
                   reads=[t_, rstd], writes=[t_])
                op("act", lambda e, j=j, t_=t_: e.activation(
                    out=ct.t[:, 4 + j, :], in_=t_.t[:, :], func=AF.Silu, bias=P("lnb", li * 4 + j), scale=P("lng", li * 4 + j)),
                   reads=[t_, prm], writes=[ct])
            wo = [load_w(about_d[li, :, h * 512:(h + 1) * 512]) for h in range(2)]
            for oc in range(8):
                pb = nb()
                w = wo[oc // 4]
                for kk in range(8):
                    op("pe", lambda e, kk=kk, oc=oc, w=w, pb=pb: e.matmul(
                        pb.t[:, :], w.t[:, kk, (oc % 4) * 128:(oc % 4 + 1) * 128], ct.t[:, kk, :], start=(kk == 0), stop=(kk == 7)),
                       reads=[w, ct], writes=[pb], inc=(kk == 7))
                resid_add(pb, l, 16, oc, sl, cols)

        stageA(0)
        for g in range(NG):
            if g + 1 < NG:
                stageA(g + 1)
            stageB(g)


def odd_mixer(k, nc, env):
    op, dma = k.op, k.dma
    xT, hnT, modT, prm, cst = env["xT"], env["hnT"], env["modT"], env["prm"], env["cst"]
    nb, load_w, proj, resid_add, P = env["nb"], env["load_w"], env["proj"], env["resid_add"], env["P"]
    l, li, sl, S, NG, NT = env["l"], env["li"], env["sl"], env["S"], env["NG"], env["NT"]
    dnin_d, dnbg_d, dnout_d = env["dnin_d"], env["dnbg_d"], env["dnout_d"]
    ones, ident, sel127, negm2, identb, bankb = env["ones"], env["ident"], env["sel127"], env["negm2"], env["identb"], env["bankb"]
    onesb = env["onesb"]
    TG = GS // 128
    with ExitStack() as pes:
        def sbt(shape, dt, name):
            return k.sb(shape, dt, es=pes, name=name)
        BG = sbt([16, S], F32, "BG")
        TM = sbt([128, NT, 16], F32, "TM")
        egc = sbt([128, NT, 16], F32, "egc")
        bexp = sbt([128, NT, 8], F32, "bexp")
        kdec = sbt([128, NT, 16], F32, "kdec")
        gl = sbt([128, NT, 16], F32, "gl")
        dl = gl
        wbg = sbt([128, KD, 16], BF16, "wbg")
        nea = sbt([16, 1], F32, "nea")
        res2 = ExitStack()
        r1 = [k.sb([16, GS], F32, es=res2, name="r1_%d" % i) for i in range(2)]
        r2 = [k.sb([16, GS], F32, es=res2, name="r2_%d" % i) for i in range(2)]
        dma("pool", wbg.t[:, :, :], dnbg_d[li].rearrange("(k p) n -> p k n", p=128), writes=[wbg])
        op("act", lambda e: e.activation(out=nea.t[:, :], in_=P("alog", li, rows=16), func=AF.Exp), reads=[prm], writes=[nea])
        op("dve", lambda e: e.tensor_scalar_mul(out=nea.t[:, :], in0=nea.t[:, :], scalar1=-1.0), reads=[nea], writes=[nea])
        for g in range(NG):
            cols = slice(g * GS, (g + 1) * GS)
            pb = nb()
            for kk in range(KD):
                op("pe", lambda e, kk=kk, pb=pb: e.matmul(pb.t[0:16, :], wbg.t[:, kk, :], hnT.t[:, kk, cols],
                                                        start=(kk == 0), stop=(kk == KD - 1)),
                   reads=[wbg, hnT], writes=[pb], inc=(kk == KD - 1))
            a, b = r1[0], r1[1]
            c_, d_ = r2[0], r2[1]
            op("act", lambda e, pb=pb: e.activation(out=a.t[:, :], in_=pb.t[0:16, :], func=AF.Sigmoid), reads=[pb], writes=[a])
            op("act", lambda e, pb=pb: e.activation(out=b.t[:, :], in_=pb.t[0:16, :], func=AF.Exp, bias=P("dtb", li, rows=16)),
               reads=[pb, prm], writes=[b])
            op("act", lambda e: e.activation(out=b.t[:, :], in_=b.t[:, :], func=AF.Ln, bias=1.0), reads=[b], writes=[b])
            op("dve", lambda e: e.tensor_scalar_mul(out=b.t[:, :], in0=b.t[:, :], scalar1=nea.t[:, 0:1]), reads=[b, nea], writes=[b])
            src, dst = b, c_
            for lev in range(7):
                sh = 1 << lev
                sv = src.t[:, :].rearrange("p (a t) -> p a t", t=128)
                dv = dst.t[:, :].rearrange("p (a t) -> p a t", t=128)
                op("dve", lambda e, sv=sv, dv=dv, sh=sh: e.tensor_tensor(out=dv[:, :, sh:128], in0=sv[:, :, sh:128],
                                                                         in1=sv[:, :, 0:128 - sh], op=ALU.add),
                   reads=[src], writes=[dst])
                op("dve", lambda e, sv=sv, dv=dv, sh=sh: e.tensor_copy(out=dv[:, :, 0:sh], in_=sv[:, :, 0:sh]),
                   reads=[src], writes=[dst])
                src, dst = dst, src
            op("dve", lambda e: e.tensor_scalar_mul(out=d_.t[:, :], in0=a.t[:, :], scalar1=P("mlo", rows=16)), reads=[a, prm], writes=[d_])
            op("dve", lambda e, src=src: e.scalar_tensor_tensor(out=BG.t[:, cols], in0=src.t[:, :], scalar=P("mhi", rows=16), in1=d_.t[:, :],
                                                                op0=ALU.mult, op1=ALU.add), reads=[src, d_, prm], writes=[BG])
        pb = nb()
        for i in range(NT):
            op("pe", lambda e, i=i, pb=pb: e.transpose(pb.t[:, i * 16:(i + 1) * 16], BG.t[:, i * 128:(i + 1) * 128], ident[0:16, 0:16]),
               reads=[BG, cst], writes=[pb], inc=(i == NT - 1))
        op("act", lambda e, pb=pb: e.activation(out=TM.t[:, :, :], in_=pb.t[:, 0:NT * 16].rearrange("p (a b) -> p a b", b=16), func=AF.Copy),
           reads=[pb], writes=[TM])
        pb2 = nb()
        op("pe", lambda e: e.matmul(pb2.t[:, 0:NT * 16], sel127, TM.t[:, :, :].rearrange("p a b -> p (a b)"), start=True, stop=True),
           reads=[TM, cst], writes=[pb2])
        op("act", lambda e: e.activation(out=gl.t[:, :, :], in_=pb2.t[:, 0:NT * 16].rearrange("p (a b) -> p a b", b=16), func=AF.Copy),
           reads=[pb2], writes=[gl])
        op("act", lambda e: e.activation(out=egc.t[:, :, :], in_=TM.t[:, :, :], func=AF.Exp), reads=[TM], writes=[egc])
        op("dve", lambda e: e.tensor_tensor(out=bexp.t[:, :, :], in0=TM.t[:, :, 0:8], in1=egc.t[:, :, 8:16], op=ALU.mult),
           reads=[TM, egc], writes=[bexp])
        op("dve", lambda e: e.tensor_tensor(out=kdec.t[:, :, :], in0=gl.t[:, :, :], in1=TM.t[:, :, :], op=ALU.subtract),
           reads=[gl, TM], writes=[kdec])
        op("act", lambda e: e.activation(out=kdec.t[:, :, :], in_=kdec.t[:, :, :], func=AF.Exp), reads=[kdec], writes=[kdec])
        op("act", lambda e: e.activation(out=dl.t[:, :, :], in_=gl.t[:, :, :], func=AF.Exp), reads=[gl], writes=[dl])

        k.barrier()
        res2.close()
        uraw = [sbt([128, 3 + GS], F32, "uraw%d" % q) for q in range(3)]
        cacc = [sbt([128, GS], F32, "cacc%d" % q) for q in range(3)]
        rn = env["rsb"]
        qn = sbt([128, GS], F32, "qn")
        kn = sbt([128, GS], F32, "kn")
        rowm = [sbt([16, GS], F32, "rowm0")] * 2
        beta_b = sbt([128, GS], F32, "beta_b")
        gc_b = sbt([128, GS], F32, "gc_b")
        eg_b = beta_b
        kT = sbt([128, GS], BF16, "kT")
        nkbT = sbt([128, GS], BF16, "nkbT")
        nkT = sbt([128, GS], BF16, "nkT")
        qT = sbt([128, GS], BF16, "qT")
        qgTs = [sbt([128, GS], BF16, "qgT%d" % i) for i in range(2)]
        zss = [sbt([128, GS], BF16, "zs%d" % i) for i in range(2)]
        tmp1 = sbt([128, TG, 128], F32, "dtmp1")
        tmp2 = sbt([128, TG, 2, 128], F32, "dtmp2")
        X2s = [sbt([128, TG, 2, 128], BF16, "X2_%d" % i) for i in range(2)]
        NLb = sbt([128, TG, 2, 128], BF16, "NLb")
        NCks = [sbt([128, 2, 2, 128], BF16, "NCk%d" % i) for i in range(2)]
        Ybs = [sbt([128, 2, 2, 128], BF16, "Yb%d" % i) for i in range(2)]
        Tbs = [[sbt([128, 2, 2, 128], BF16, "Tb%d_%d" % (i, j)) for j in range(2)] for i in range(2)]
        mkb = sbt([128, 7, 2, 128], BF16, "mkb")
        dma("pool", mkb.t[:, :, :, :], env["mk_d"][:, :].rearrange("p (a b c) -> p a b c", a=7, b=2), writes=[mkb])
        kbgs = [sbt([128, TG, 128], BF16, "kbg%d" % i) for i in range(2)]
        kds = [sbt([128, TG, 128], BF16, "kd%d" % i) for i in range(2)]
        vbs = [sbt([128, TG, 128], BF16, "vb%d" % i) for i in range(2)]
        nwT = sbt([128, TG, 128], BF16, "nwT")
        vnew = [sbt([128, 128], BF16, "vnew%d" % i) for i in range(2)]
        S32 = sbt([128, 128], F32, "S32")
        Sbf = sbt([128, 128], BF16, "Sbf")
        o32 = sbt([128, GS], F32, "o32")
        og = o32
        og2 = sbt([128, GS], BF16, "og2")
        wo = sbt([128, D], BF16, "wo_h")

        U = 8 * NG

        banks_ = env["banks"]
        bctr = [0, 0]

        def nb1():
            bctr[0] += 1
            return banks_[(bctr[0] - 1) % 4]

        def nb2():
            bctr[1] += 1
            return banks_[4 + (bctr[1] - 1) % 3]

        def stage1(u):
            h, g = divmod(u, NG)
            p = u % 2
            X2, kbg, kd, vb, qgT, zs = X2s[p], kbgs[p], kds[p], vbs[p], qgTs[p], zss[p]
            if g == 0:
                wcur[0] = load_w(dnin_d[li, :, h * 512:(h + 1) * 512])
            w = wcur[0]
            cols = slice(g * GS, (g + 1) * GS)
            for q in range(3):
                ur = uraw[q]
                if g == 0:
                    op("pool", lambda e, ur=ur: e.memset(ur.t[:, 0:3], 0.0), writes=[ur])
                else:
                    op("pool", lambda e, ur=ur: e.tensor_copy(out=ur.t[:, 0:3], in_=ur.t[:, GS:GS + 3]), reads=[ur], writes=[ur])
                pb = nb1()
                proj(w, q * 128, g, pb)
                op("act", lambda e, ur=ur, pb=pb: e.activation(out=ur.t[:, 3:3 + GS], in_=pb.t[:, :], func=AF.Copy), reads=[pb], writes=[ur])
            pb = nb1()
            proj(w, 3 * 128, g, pb)
            op("act", lambda e, pb=pb: e.activation(out=zs.t[:, :], in_=pb.t[:, :], func=AF.Silu), reads=[pb], writes=[zs])
            for q in range(3):
                ur = uraw[q]
                ca = cacc[q]
                cb = (li * 24 + q * 8 + h) * 4
                op("dve", lambda e, ur=ur, ca=ca, cb=cb: e.tensor_scalar_mul(out=ca.t[:, :], in0=ur.t[:, 3:3 + GS], scalar1=P("dnconv", cb + 3)),
                   reads=[ur, prm], writes=[ca])
                for tap in range(3):
                    op("dve", lambda e, ur=ur, ca=ca, cb=cb, tap=tap: e.scalar_tensor_tensor(
                        out=ca.t[:, :], in0=ur.t[:, tap:tap + GS], scalar=P("dnconv", cb + tap), in1=ca.t[:, :],
                        op0=ALU.mult, op1=ALU.add), reads=[ur, prm, ca], writes=[ca])
                op("act", lambda e, ca=ca: e.activation(out=ca.t[:, :], in_=ca.t[:, :], func=AF.Silu), reads=[ca], writes=[ca])
            for q in range(2):
                ca = cacc[q]
                sqs = (qT, qgT)[q]
                op("act", lambda e, ca=ca, sqs=sqs: e.activation(out=sqs.t[:, :], in_=ca.t[:, :], func=AF.Square), reads=[ca], writes=[sqs])
                pb = nb1()
                op("pe", lambda e, pb=pb, sqs=sqs: e.matmul(pb.t[:, :], onesb.t[:, :], sqs.t[:, :], start=True, stop=True),
                   reads=[sqs, onesb], writes=[pb])
                op("act", lambda e, pb=pb: e.activation(out=rn[0].t[:, :], in_=pb.t[:, :], func=AF.Ln, bias=EPS), reads=[pb], writes=[rn[0]])
                op("act", lambda e: e.activation(out=rn[0].t[:, :], in_=rn[0].t[:, :], func=AF.Exp, scale=-0.5), reads=[rn[0]], writes=[rn[0]])
                if q == 0:
                    op("dve", lambda e: e.scalar_tensor_tensor(out=qn.t[:, :], in0=cacc[0].t[:, :], scalar=128.0 ** -0.5, in1=rn[0].t[:, :],
                                                               op0=ALU.mult, op1=ALU.mult), reads=[cacc[0], rn[0]], writes=[qn])
                else:
                    op("dve", lambda e: e.tensor_tensor(out=kn.t[:, :], in0=cacc[1].t[:, :], in1=rn[0].t[:, :], op=ALU.mult),
                       reads=[cacc[1], rn[0]], writes=[kn])
            for qi, (row, dstb) in enumerate(((h, beta_b), (8 + h, gc_b))):
                rm = rowm[qi]
                op("dve", lambda e, rm=rm, row=row: e.tensor_scalar_mul(out=rm.t[:, :], in0=BG.t[:, cols], scalar1=ident[0:16, row:row + 1]),
                   reads=[BG, cst], writes=[rm])
                pb = nb1()
                op("pe", lambda e, rm=rm, pb=pb: e.matmul(pb.t[:, :], ones[0:16, :], rm.t[:, :], start=True, stop=True),
                   reads=[rm, cst], writes=[pb])
                op("act", lambda e, pb=pb, dstb=dstb: e.activation(out=dstb.t[:, :], in_=pb.t[:, :], func=AF.Copy), reads=[pb], writes=[dstb])
            op("act", lambda e: e.activation(out=kT.t[:, :], in_=kn.t[:, :], func=AF.Copy), reads=[kn], writes=[kT])
            op("act", lambda e: e.activation(out=nkT.t[:, :], in_=kn.t[:, :], func=AF.Copy, scale=-1.0), reads=[kn], writes=[nkT])
            op("pool", lambda e: e.tensor_tensor(out=nkbT.t[:, :], in0=kn.t[:, :], in1=beta_b.t[:, :], op=ALU.mult),
               reads=[kn, beta_b], writes=[nkbT])
            op("act", lambda e: e.activation(out=eg_b.t[:, :], in_=gc_b.t[:, :], func=AF.Exp), reads=[gc_b], writes=[eg_b])
            op("act", lambda e: e.activation(out=qT.t[:, :], in_=qn.t[:, :], func=AF.Copy), reads=[qn], writes=[qT])
            op("pool", lambda e: e.tensor_tensor(out=qgT.t[:, :], in0=qn.t[:, :], in1=eg_b.t[:, :], op=ALU.mult),
               reads=[qn, eg_b], writes=[qgT])
            pk = nb1()
            pv = nb1()
            for t in range(TG):
                tc_ = slice(t * 128, (t + 1) * 128)
                op("pe", lambda e, t=t, tc_=tc_: e.transpose(pk.t[:, tc_], kn.t[:, tc_], ident), reads=[kn, cst], writes=[pk], inc=(t == TG - 1))
            for t in range(TG):
                tc_ = slice(t * 128, (t + 1) * 128)
                op("pe", lambda e, t=t, tc_=tc_: e.transpose(pv.t[:, tc_], cacc[2].t[:, tc_], ident), reads=[cacc[2], cst], writes=[pv], inc=(t == TG - 1))
            pm = [nb1(), nb1()]
            for t in range(TG):
                ti = g * TG + t
                tc_ = slice(t * 128, (t + 1) * 128)
                op("act", lambda e, t=t, ti=ti, tc_=tc_: e.activation(out=kbg.t[:, t, :], in_=pk.t[:, tc_], func=AF.Copy, scale=bexp.t[:, ti, h:h + 1]),
                   reads=[pk, bexp], writes=[kbg])
                op("act", lambda e, t=t, ti=ti, tc_=tc_: e.activation(out=kd.t[:, t, :], in_=pk.t[:, tc_], func=AF.Copy, scale=kdec.t[:, ti, 8 + h:9 + h]),
                   reads=[pk, kdec], writes=[kd])
                op("act", lambda e, t=t, ti=ti, tc_=tc_: e.activation(out=vb.t[:, t, :], in_=pv.t[:, tc_], func=AF.Copy, scale=TM.t[:, ti, h:h + 1]),
                   reads=[pv, TM], writes=[vb])
                op("dve", lambda e, t=t, ti=ti, tc_=tc_: e.tensor_scalar(out=tmp1.t[:, t, :], in0=gc_b.t[:, tc_], scalar1=TM.t[:, ti, 8 + h:9 + h],
                                                                         scalar2=0.0, op0=ALU.subtract, op1=ALU.min), reads=[gc_b, TM], writes=[tmp1])
                op("dve", lambda e, t=t: e.tensor_tensor(out=tmp2.t[:, t, :, :], in0=tmp1.t[:, t, :].unsqueeze(1).to_broadcast([128, 2, 128]),
                                                          in1=negm2.rearrange("p (a b) -> p a b", a=2), op=ALU.add), reads=[tmp1, cst], writes=[tmp2])
                pmm = pm[t // 2]
                o0 = (t % 2) * 256
                op("pe", lambda e, tc_=tc_, pmm=pmm, o0=o0: e.matmul(pmm.t[:, o0:o0 + 128], nkT.t[:, tc_], nkbT.t[:, tc_], start=True, stop=True),
                   reads=[nkT, nkbT], writes=[pmm], inc=False)
                op("pe", lambda e, tc_=tc_, pmm=pmm, o0=o0: e.matmul(pmm.t[:, o0 + 128:o0 + 256], kT.t[:, tc_], qT.t[:, tc_], start=True, stop=True),
                   reads=[kT, qT], writes=[pmm])
            op("act", lambda e: e.activation(out=tmp2.t[:, :, :, :], in_=tmp2.t[:, :, :, :], func=AF.Exp), reads=[tmp2], writes=[tmp2])
            for hf in range(2):
                op("dve", lambda e, hf=hf: e.tensor_tensor(
                    out=X2.t[:, 2 * hf:2 * hf + 2, :, :], in0=pm[hf].t[:, :].rearrange("p (a b c) -> p a b c", a=2, b=2),
                    in1=tmp2.t[:, 2 * hf:2 * hf + 2, :, :], op=ALU.mult), reads=[pm[hf], tmp2], writes=[X2])

        def stage2(u):
            h, g = divmod(u, NG)
            p = u % 2
            X2, kbg, kd, vb, qgT, zs = X2s[p], kbgs[p], kds[p], vbs[p], qgTs[p], zss[p]
            cols = slice(g * GS, (g + 1) * GS)
            if g == 0:
                dma("pool", wo.t[:, :], dnout_d[li, h * 128:(h + 1) * 128, :], writes=[wo])
                op("dve", lambda e: e.memset(S32.t[:, :], 0.0), writes=[S32])
                op("dve", lambda e: e.memset(Sbf.t[:, :], 0.0), writes=[Sbf])
            for t in range(TG):
                op("pe", lambda e, t=t: e.transpose(bankb.t[:, t * 128:(t + 1) * 128], X2.t[:, t, 0, :], identb.t[:, :]),
                   reads=[X2, identb], writes=[bankb], inc=(t == TG - 1))
            op("act", lambda e: e.activation(out=NLb.t[:, :, 0, :], in_=bankb.t[:, 0:TG * 128].rearrange("p (a b) -> p a b", a=TG), func=AF.Copy),
               reads=[bankb], writes=[NLb])
            op("act", lambda e: e.activation(out=NLb.t[:, :, 1, :], in_=X2.t[:, :, 0, :], func=AF.Copy), reads=[X2], writes=[NLb])
            for hf in range(2):
                op("dve", lambda e, hf=hf: e.tensor_tensor(out=NCks[hf].t[:, :, :, :], in0=NLb.t[:, 2 * hf:2 * hf + 2, :, :],
                                                           in1=mkb.t[:, 0, :, :].unsqueeze(1).to_broadcast([128, 2, 2, 128]), op=ALU.mult),
                   reads=[NLb, mkb], writes=[NCks[hf]])
            Tc = Tbs[0]
            for hf in range(2):
                op("pool", lambda e, hf=hf, Tc=Tc: e.tensor_tensor(out=Tc[hf].t[:, :, :, :], in0=NCks[hf].t[:, :, :, :],
                                                                   in1=identb.t[:, :].unsqueeze(1).unsqueeze(1).to_broadcast([128, 2, 2, 128]), op=ALU.add),
                   reads=[NCks[hf], identb], writes=[Tc[hf]])
            for lev in range(1, 7):
                last = (lev == 6)
                for hf in range(2):
                    op("dve", lambda e, hf=hf, lev=lev: e.tensor_tensor(out=NCks[hf].t[:, :, :, :], in0=NLb.t[:, 2 * hf:2 * hf + 2, :, :],
                                                                        in1=mkb.t[:, lev, :, :].unsqueeze(1).to_broadcast([128, 2, 2, 128]), op=ALU.mult),
                       reads=[NLb, mkb], writes=[NCks[hf]])
                py = [nb2(), nb2()]
                for t in range(TG):
                    hf, tt_ = t // 2, t % 2
                    ppp = py[hf]
                    o0 = tt_ * 256
                    if not last:
                        op("pe", lambda e, hf=hf, tt_=tt_, ppp=ppp, o0=o0, Tc=Tc: e.matmul(ppp.t[:, o0:o0 + 128], NCks[hf].t[:, tt_, 1, :], Tc[hf].t[:, tt_, 0, :], start=True, stop=True),
                           reads=[NCks[hf], Tc[hf]], writes=[ppp], inc=False)
                    op("pe", lambda e, hf=hf, tt_=tt_, ppp=ppp, o0=o0, Tc=Tc: e.matmul(ppp.t[:, o0 + 128:o0 + 256], NCks[hf].t[:, tt_, 0, :], Tc[hf].t[:, tt_, 1, :], start=True, stop=True),
                       reads=[NCks[hf], Tc[hf]], writes=[ppp])
                for hf in range(2):
                    src4 = py[hf].t[:, :].rearrange("p (a b c) -> p a b c", a=2, b=2)
                    if last:
                        src, dst = src4[:, :, 1, :], Ybs[hf].t[:, :, 1, :]
                    else:
                        src, dst = src4, Ybs[hf].t[:, :, :, :]
                    if hf == 0:
                        op("act", lambda e, src=src, dst=dst: e.activation(out=dst, in_=src, func=AF.Copy), reads=[py[hf]], writes=[Ybs[hf]])
                    else:
                        op("dve", lambda e, src=src, dst=dst: e.tensor_copy(out=dst, in_=src), reads=[py[hf]], writes=[Ybs[hf]])
                pz = [nb2(), nb2()]
                for t in range(TG):
                    hf, tt_ = t // 2, t % 2
                    pzz = pz[hf]
                    o0 = tt_ * 256
                    if not last:
                        op("pe", lambda e, hf=hf, tt_=tt_, pzz=pzz, o0=o0, Tc=Tc: e.matmul(pzz.t[:, o0:o0 + 128], Tc[hf].t[:, tt_, 1, :], Ybs[hf].t[:, tt_, 0, :], start=True, stop=True),
                           reads=[Tc[hf], Ybs[hf]], writes=[pzz], inc=False)
                    op("pe", lambda e, hf=hf, tt_=tt_, pzz=pzz, o0=o0, Tc=Tc: e.matmul(pzz.t[:, o0 + 128:o0 + 256], Tc[hf].t[:, tt_, 0, :], Ybs[hf].t[:, tt_, 1, :], start=True, stop=True),
                       reads=[Tc[hf], Ybs[hf]], writes=[pzz])
                Tn = Tbs[lev % 2]
                for hf in range(2):
                    src4 = pz[hf].t[:, :].rearrange("p (a b c) -> p a b c", a=2, b=2)
                    if last:
                        op("dve", lambda e, hf=hf, src4=src4, Tn=Tn, Tc=Tc: e.tensor_tensor(
                            out=Tn[hf].t[:, :, 1, :], in0=src4[:, :, 1, :], in1=Tc[hf].t[:, :, 1, :], op=ALU.add), reads=[pz[hf], Tc[hf]], writes=[Tn[hf]])
                    else:
                        op("dve", lambda e, hf=hf, src4=src4, Tn=Tn, Tc=Tc: e.tensor_tensor(
                            out=Tn[hf].t[:, :, :, :], in0=src4, in1=Tc[hf].t[:, :, :, :], op=ALU.add), reads=[pz[hf], Tc[hf]], writes=[Tn[hf]])
                Tc = Tn
            TT = Tc
            pw_ = nb2()
            for t in range(TG):
                op("pe", lambda e, t=t: e.matmul(pw_.t[:, t * 128:(t + 1) * 128], kbg.t[:, t, :], TT[t // 2].t[:, t % 2, 1, :], start=True, stop=True),
                   reads=[kbg, TT[t // 2]], writes=[pw_], inc=(t == TG - 1))
            op("act", lambda e: e.activation(out=nwT.t[:, :, :], in_=pw_.t[:, :].rearrange("p (a b) -> p a b", a=TG), func=AF.Copy, scale=-1.0),
               reads=[pw_], writes=[nwT])
            po = nb2()
            pvns = [nb2(), nb2()]
            for t in range(TG):
                ti = g * TG + t
                tc_ = slice(t * 128, (t + 1) * 128)
                vn = vnew[t % 2]
                pvn = pvns[t % 2]
                op("pe", lambda e, t=t, pvn=pvn: e.matmul(pvn.t[:, 0:128], TT[t // 2].t[:, t % 2, 1, :], vb.t[:, t, :], start=True, stop=False),
                   reads=[TT[t // 2], vb], writes=[pvn], inc=False)
                op("pe", lambda e, t=t, pvn=pvn: e.matmul(pvn.t[:, 0:128], nwT.t[:, t, :], Sbf.t[:, :], start=False, stop=True),
                   reads=[nwT, Sbf], writes=[pvn])
                op("act", lambda e, pvn=pvn, vn=vn: e.activation(out=vn.t[:, :], in_=pvn.t[:, 0:128], func=AF.Copy), reads=[pvn], writes=[vn])
                op("pe", lambda e, tc_=tc_: e.matmul(po.t[:, tc_], Sbf.t[:, :], qgT.t[:, tc_], start=True, stop=False),
                   reads=[Sbf, qgT], writes=[po], inc=False)
                op("pe", lambda e, t=t, tc_=tc_, vn=vn: e.matmul(po.t[:, tc_], vn.t[:, :], X2.t[:, t, 1, :], start=False, stop=True),
                   reads=[vn, X2], writes=[po], inc=False)
                op("pe", lambda e, t=t, pvn=pvn, vn=vn: e.matmul(pvn.t[:, 128:256], kd.t[:, t, :], vn.t[:, :], start=True, stop=True),
                   reads=[kd, vn], writes=[pvn])
                op("dve", lambda e, pvn=pvn, ti=ti: e.scalar_tensor_tensor(out=Sbf.t[:, :], in0=S32.t[:, :], scalar=dl.t[:, ti, 8 + h:9 + h],
                                                                           in1=pvn.t[:, 128:256], op0=ALU.mult, op1=ALU.add),
                   reads=[S32, dl, pvn], writes=[Sbf])
                op("dve", lambda e, pvn=pvn, ti=ti: e.scalar_tensor_tensor(out=S32.t[:, :], in0=S32.t[:, :], scalar=dl.t[:, ti, 8 + h:9 + h],
                                                                           in1=pvn.t[:, 128:256], op0=ALU.mult, op1=ALU.add),
                   reads=[S32, dl, pvn], writes=[S32])
            op("act", lambda e: e.activation(out=o32.t[:, :], in_=po.t[:, :], func=AF.Copy), reads=[po], writes=[o32])
            op("act", lambda e: e.activation(out=og2.t[:, :], in_=o32.t[:, :], func=AF.Square), reads=[o32], writes=[og2])
            pb = nb2()
            op("pe", lambda e, pb=pb: e.matmul(pb.t[:, :], onesb.t[:, :], og2.t[:, :], start=True, stop=True), reads=[og2, onesb], writes=[pb])
            op("act", lambda e, pb=pb: e.activation(out=rn[1].t[:, :], in_=pb.t[:, :], func=AF.Ln, scale=1.0 / 128, bias=EPS), reads=[pb], writes=[rn[1]])
            op("act", lambda e: e.activation(out=rn[1].t[:, :], in_=rn[1].t[:, :], func=AF.Exp, scale=-0.5), reads=[rn[1]], writes=[rn[1]])
            op("dve", lambda e: e.tensor_tensor(out=og.t[:, :], in0=o32.t[:, :], in1=rn[1].t[:, :], op=ALU.mult), reads=[o32, rn[1]], writes=[og])
            op("dve", lambda e: e.scalar_tensor_tensor(out=og2.t[:, :], in0=og.t[:, :], scalar=P("onorm", li), in1=zs.t[:, :],
                                                       op0=ALU.mult, op1=ALU.mult), reads=[og, prm, zs], writes=[og2])
            for oc in range(8):
                pb = nb2()
                op("pe", lambda e, oc=oc, pb=pb: e.matmul(pb.t[:, :], wo.t[:, oc * 128:(oc + 1) * 128], og2.t[:, :], start=True, stop=True),
                   reads=[wo, og2], writes=[pb])
                resid_add(pb, l, 16, oc, sl, cols)


        wcur = [None]
        k.replay(k.record(lambda: stage1(0)))
        for u in range(U):
            ra = k.record(lambda: stage2(u))
            rb = k.record(lambda: stage1(u + 1)) if u + 1 < U else []
            k.replay(k.interleave(ra, rb) if INTERLEAVE else (ra + rb))


def moe_layer(k, nc, env):
    op, dma = k.op, k.dma
    xT, hnT, modT, prm, cst = env["xT"], env["hnT"], env["modT"], env["prm"], env["cst"]
    nb, nw, resid_add, P, make_hn = env["nb"], env["nw"], env["resid_add"], env["P"], env["make_hn"]
    l, sl, S, NG, NT = env["l"], env["sl"], env["S"], env["NG"], env["NT"]
    moer_d, moeup_d, moedn_d = env["moer_d"], env["moeup_d"], env["moedn_d"]
    ones, ident = env["ones"], env["ident"]
    wpool = env["wpool"]
    TG = GS // 128
    with ExitStack() as pes:
        GTh = k.sb([32, S], BF16, es=pes, name="GTh")
        GTl = k.sb([32, S], BF16, es=pes, name="GTl")
        with ExitStack() as res:
            def sbt(shape, dt, name):
                return k.sb(shape, dt, es=res, name=name)
            env["alloc_scr"](res)
            GT = sbt([32, S], F32, "GT")
            h32 = sbt([128, KD, GS], F32, "hn32")
            wr = sbt([128, KD, 36], F32, "wr")
            lg = sbt([128, 36], F32, "lg")
            sm = {n: sbt([128, 36], F32, "sm_" + n) for n in ("mx", "oh", "pen", "ml", "m1", "k1", "ml2", "m2", "k2", "ex", "sm", "gp", "r", "den", "w1", "w2", "G", "nmx")}
            dma("sp", wr.t[:, :, :], moer_d[l].rearrange("(k p) n -> p k n", p=128), writes=[wr])

            def dv(fn, reads, writes):
                op("dve", fn, reads=reads, writes=writes)

            for g in range(NG):
                make_hn(l, 1, sl, g, hn32=h32)
                for t in range(TG):
                    ti = g * TG + t
                    pb = nb()
                    for kk in range(KD):
                        op("pe", lambda e, kk=kk, t=t, pb=pb: e.matmul(pb.t[:, 0:36], h32.t[:, kk, t * 128:(t + 1) * 128], wr.t[:, kk, :],
                                                                     start=(kk == 0), stop=(kk == KD - 1)),
                           reads=[h32, wr], writes=[pb], inc=(kk == KD - 1))
                    dv(lambda e, pb=pb: e.tensor_tensor(out=lg.t[:, :], in0=pb.t[:, 0:36], in1=P("rbias", l * 36, 36), op=ALU.add), [pb, prm], [lg])
                    dv(lambda e: e.tensor_reduce(out=sm["mx"].t[:, 0:1], in_=lg.t[:, 0:4], axis=mybir.AxisListType.X, op=ALU.max), [lg], [sm["mx"]])
                    dv(lambda e: e.tensor_scalar(out=sm["oh"].t[:, 0:4], in0=lg.t[:, 0:4], scalar1=sm["mx"].t[:, 0:1], scalar2=None, op0=ALU.is_ge), [lg, sm["mx"]], [sm["oh"]])
                    dv(lambda e: e.tensor_scalar_mul(out=sm["nmx"].t[:, 0:1], in0=sm["mx"].t[:, 0:1], scalar1=-1.0), [sm["mx"]], [sm["nmx"]])
                    op("act", lambda e: e.activation(out=sm["ex"].t[:, 0:4], in_=lg.t[:, 0:4], func=AF.Exp, bias=sm["nmx"].t[:, 0:1]), reads=[lg, sm["nmx"]], writes=[sm["ex"]])
                    dv(lambda e: e.tensor_reduce(out=sm["sm"].t[:, 0:1], in_=sm["ex"].t[:, 0:4], axis=mybir.AxisListType.X, op=ALU.add), [sm["ex"]], [sm["sm"]])
                    dv(lambda e: e.reciprocal(out=sm["gp"].t[:, 0:1], in_=sm["sm"].t[:, 0:1]), [sm["sm"]], [sm["gp"]])
                    dv(lambda e: e.tensor_scalar(out=sm["pen"].t[:, 0:4], in0=sm["oh"].t[:, 0:4], scalar1=-1.0, scalar2=1.0e4, op0=ALU.add, op1=ALU.mult), [sm["oh"]], [sm["pen"]])
                    dv(lambda e: e.tensor_tensor(out=sm["ml"].t[:, 0:32].rearrange("p (a b) -> p a b", a=4), in0=lg.t[:, 4:36].rearrange("p (a b) -> p a b", a=4),
                                                 in1=sm["pen"].t[:, 0:4].unsqueeze(2).to_broadcast([128, 4, 8]), op=ALU.add), [lg, sm["pen"]], [sm["ml"]])
                    dv(lambda e: e.tensor_reduce(out=sm["m1"].t[:, 0:1], in_=sm["ml"].t[:, 0:32], axis=mybir.AxisListType.X, op=ALU.max), [sm["ml"]], [sm["m1"]])
                    dv(lambda e: e.tensor_scalar(out=sm["k1"].t[:, 0:32], in0=sm["ml"].t[:, 0:32], scalar1=sm["m1"].t[:, 0:1], scalar2=None, op0=ALU.is_ge), [sm["ml"], sm["m1"]], [sm["k1"]])
                    dv(lambda e: e.scalar_tensor_tensor(out=sm["ml2"].t[:, 0:32], in0=sm["k1"].t[:, 0:32], scalar=-1.0e4, in1=sm["ml"].t[:, 0:32], op0=ALU.mult, op1=ALU.add),
                       [sm["k1"], sm["ml"]], [sm["ml2"]])
                    dv(lambda e: e.tensor_reduce(out=sm["m2"].t[:, 0:1], in_=sm["ml2"].t[:, 0:32], axis=mybir.AxisListType.X, op=ALU.max), [sm["ml2"]], [sm["m2"]])
                    dv(lambda e: e.tensor_scalar(out=sm["k2"].t[:, 0:32], in0=sm["ml2"].t[:, 0:32], scalar1=sm["m2"].t[:, 0:1], scalar2=None, op0=ALU.is_ge), [sm["ml2"], sm["m2"]], [sm["k2"]])
                    dv(lambda e: e.tensor_tensor(out=sm["r"].t[:, 0:1], in0=sm["m2"].t[:, 0:1], in1=sm["m1"].t[:, 0:1], op=ALU.subtract), [sm["m2"], sm["m1"]], [sm["r"]])
                    op("act", lambda e: e.activation(out=sm["r"].t[:, 0:1], in_=sm["r"].t[:, 0:1], func=AF.Exp), reads=[sm["r"]], writes=[sm["r"]])
                    dv(lambda e: e.tensor_scalar_add(out=sm["den"].t[:, 0:1], in0=sm["r"].t[:, 0:1], scalar1=1.0), [sm["r"]], [sm["den"]])
                    dv(lambda e: e.reciprocal(out=sm["den"].t[:, 0:1], in_=sm["den"].t[:, 0:1]), [sm["den"]], [sm["den"]])
                    dv(lambda e: e.tensor_tensor(out=sm["w1"].t[:, 0:1], in0=sm["gp"].t[:, 0:1], in1=sm["den"].t[:, 0:1], op=ALU.mult), [sm["gp"], sm["den"]], [sm["w1"]])
                    dv(lambda e: e.tensor_tensor(out=sm["w2"].t[:, 0:1], in0=sm["w1"].t[:, 0:1], in1=sm["r"].t[:, 0:1], op=ALU.mult), [sm["w1"], sm["r"]], [sm["w2"]])
                    dv(lambda e: e.tensor_scalar_mul(out=sm["G"].t[:, 0:32], in0=sm["k1"].t[:, 0:32], scalar1=sm["w1"].t[:, 0:1]), [sm["k1"], sm["w1"]], [sm["G"]])
                    dv(lambda e: e.scalar_tensor_tensor(out=sm["G"].t[:, 0:32], in0=sm["k2"].t[:, 0:32], scalar=sm["w2"].t[:, 0:1], in1=sm["G"].t[:, 0:32], op0=ALU.mult, op1=ALU.add),
                       [sm["k2"], sm["w2"], sm["G"]], [sm["G"]])
                    pt = nb()
                    op("pe", lambda e, pt=pt: e.transpose(pt.t[0:32, 0:128], sm["G"].t[:, 0:32], ident), reads=[sm["G"], cst], writes=[pt])
                    op("act", lambda e, pt=pt, ti=ti: e.activation(out=GT.t[:, ti * 128:(ti + 1) * 128], in_=pt.t[0:32, 0:128], func=AF.Copy), reads=[pt], writes=[GT])
            op("act", lambda e: e.activation(out=GTh.t[:, :], in_=GT.t[:, :], func=AF.Copy), reads=[GT], writes=[GTh])
            op("dve", lambda e: e.tensor_tensor(out=GTl.t[:, :], in0=GT.t[:, :], in1=GTh.t[:, :], op=ALU.subtract), reads=[GT, GTh], writes=[GTl])
            k.barrier()

        def sbt(shape, dt, name):
            return k.sb(shape, dt, es=pes, name=name)
        selall = sbt([32, NEXP, 128], BF16, "selall")
        wup = list(wpool) + [sbt([128, KD, 512], BF16, "wupx%d" % i) for i in range(2)]
        wdn = [sbt([128, 2, D], BF16, "wdn%d" % i) for i in range(4)]
        gsb = [sbt([128, GS], F32, "gsb%d" % i) for i in range(2)]
        sgb = [sbt([128, GS], F32, "sgb%d" % i) for i in range(2)]
        tb = sgb
        hb = [[sbt([128, 2, GS], BF16, "hb%d_%d" % (i, j)) for j in range(2)] for i in range(2)]
        for ex_ in range(NEXP):
            op("dve", lambda e, ex_=ex_: e.tensor_copy(out=selall.t[:, ex_, :], in_=ident[0:32, ex_:ex_ + 1].to_broadcast([32, 128])),
               reads=[cst], writes=[selall])

        def fetch_pair(ep_):
            for ei_ in range(2):
                ex_ = 2 * ep_ + ei_
                wu_ = wup[ex_ % 4]
                dma("pool", wu_.t[:, :, :], moeup_d[l, ex_].rearrange("(k p) n -> p k n", p=128), writes=[wu_])
                wd_ = wdn[ex_ % 4]
                dma("pool", wd_.t[:, :, :], moedn_d[l, ex_].rearrange("(k p) n -> p k n", p=128), writes=[wd_])

        fetch_pair(0)
        for ep in range(NEXP // 2):
            if ep + 1 < NEXP // 2:
                fetch_pair(ep + 1)
            wus = [wup[(2 * ep + ei) % 4] for ei in range(2)]
            wds = [wdn[(2 * ep + ei) % 4] for ei in range(2)]
            def phase1(g):
                cols = slice(g * GS, (g + 1) * GS)
                for ei in range(2):
                    ex = 2 * ep + ei
                    wu = wus[ei]
                    pg_ = nb()
                    op("pe", lambda e, ex=ex, pg_=pg_: e.matmul(pg_.t[:, :], selall.t[:, ex, :], GTh.t[:, cols], start=True, stop=False),
                       reads=[selall, GTh], writes=[pg_], inc=False)
                    op("pe", lambda e, ex=ex, pg_=pg_: e.matmul(pg_.t[:, :], selall.t[:, ex, :], GTl.t[:, cols], start=False, stop=True),
                       reads=[selall, GTl], writes=[pg_])
                    gs_ = gsb[ei]
                    op("dve", lambda e, pg_=pg_, gs_=gs_: e.tensor_copy(out=gs_.t[:, :], in_=pg_.t[:, :]), reads=[pg_], writes=[gs_])
                    hh = hb[ei][g % 2]
                    for j in range(2):
                        pgt = nb()
                        put = nb()
                        for kk in range(KD):
                            op("pe", lambda e, kk=kk, j=j, pgt=pgt, wu=wu: e.matmul(pgt.t[:, :], wu.t[:, kk, j * 128:(j + 1) * 128], hnT.t[:, kk, cols],
                                                                                 start=(kk == 0), stop=(kk == KD - 1)), reads=[wu, hnT], writes=[pgt], inc=(kk == KD - 1))
                        for kk in range(KD):
                            op("pe", lambda e, kk=kk, j=j, put=put, wu=wu: e.matmul(put.t[:, :], wu.t[:, kk, FE + j * 128:FE + (j + 1) * 128], hnT.t[:, kk, cols],
                                                                                 start=(kk == 0), stop=(kk == KD - 1)), reads=[wu, hnT], writes=[put], inc=(kk == KD - 1))
                        sg = sgb[j]
                        tt = tb[j]
                        op("act", lambda e, pgt=pgt, sg=sg: e.activation(out=sg.t[:, :], in_=pgt.t[:, :], func=AF.Silu), reads=[pgt], writes=[sg])
                        op("dve", lambda e, put=put, sg=sg, tt=tt: e.tensor_tensor(out=tt.t[:, :], in0=put.t[:, :], in1=sg.t[:, :], op=ALU.mult),
                           reads=[put, sg], writes=[tt])
                        op("pool", lambda e, j=j, tt=tt, hh=hh, gs_=gs_: e.tensor_tensor(out=hh.t[:, j, :], in0=tt.t[:, :], in1=gs_.t[:, :], op=ALU.mult),
                           reads=[tt, gs_], writes=[hh])
            def phase2(g):
                cols = slice(g * GS, (g + 1) * GS)
                for oc in range(8):
                    py = nb()
                    n = 0
                    for ei in range(2):
                        hh = hb[ei][g % 2]
                        wd = wds[ei]
                        for j in range(2):
                            op("pe", lambda e, j=j, oc=oc, py=py, wd=wd, hh=hh, n=n: e.matmul(py.t[:, :], wd.t[:, j, oc * 128:(oc + 1) * 128], hh.t[:, j, :],
                                                                                          start=(n == 0), stop=(n == 3)), reads=[wd, hh], writes=[py], inc=(n == 3))
                            n += 1
                    resid_add(py, l, 40, oc, sl, cols)

            for g in range(NG):
                phase1(g)
                if g > 0:
                    phase2(g - 1)
            phase2(NG - 1)


_CACHE = {}


def prepare_inputs(inputs, cfg):
    NSEQ, S, L, NCO = cfg["NSEQ"], cfg["S"], cfg["DEPTH"], cfg["NCORES"]
    f = lambda a: np.ascontiguousarray(np.asarray(a, np.float32))
    prm, _ = _build_prm(inputs, L)
    cst = _build_cst()
    dnin = f(inputs["dn_w_in"])
    qkvz = dnin[:, :, :4096].reshape(2, D, 4, 8, 128).transpose(0, 1, 3, 2, 4).reshape(2, D, 4096)
    shared = {
        "cst": cst, "prm": prm, "mk": _build_mk(),
        "mod_w": f(inputs["mod_w"])[:L], "ab_w_in": f(inputs["ab_w_in"]), "pool_w": f(inputs["pool_w"]),
        "ab_w_out": f(inputs["ab_w_out"]), "dn_w_in_h": np.ascontiguousarray(qkvz),
        "dn_w_bg": np.ascontiguousarray(dnin[:, :, 4096:4112]), "dn_w_out": f(inputs["dn_w_out"]),
        "moe_w_r": np.ascontiguousarray(np.concatenate([f(inputs["moe_w_grp"])[:L], f(inputs["moe_w_exp"])[:L]], axis=2)),
        "moe_w_up": f(inputs["moe_w_up"])[:L], "moe_w_down": f(inputs["moe_w_down"])[:L],
    }
    x = f(inputs["x"])
    c = f(inputs["c"])
    maps = []
    for core in range(NCO):
        xs = x[core * NSEQ:(core + 1) * NSEQ].reshape(NSEQ * S, D)
        cs = c[core * NSEQ:(core + 1) * NSEQ]
        cT = cs.reshape(NSEQ, 8, 128).transpose(2, 1, 0).reshape(128, 8 * NSEQ)
        m = dict(shared)
        m["x"] = np.ascontiguousarray(xs)
        m["cT"] = np.ascontiguousarray(cT)
        maps.append(m)
    return maps


def kernel(**inputs):
    cfg = CFG
    key = tuple(sorted(cfg.items()))
    if key not in _CACHE:
        _CACHE[key] = build_program(cfg)[0]
    nc = _CACHE[key]
    maps = prepare_inputs(inputs, cfg)
    res = run_bass_kernel_spmd(nc, maps, core_ids=list(range(cfg["NCORES"])))
    outs = [np.asarray(r["out"], np.float32).reshape(cfg["NSEQ"], cfg["S"], D) for r in res.results]
    return np.concatenate(outs, axis=0)
```
